# Optimizing a Trainium2 kernel written in Bass

```python
import math
import jax, jax.numpy as jnp
from jax import lax
import numpy as np

D_MODEL = 1024
BATCH = 8
SEQ = 2048
DEPTH = 2

N_HEADS = 8
HEAD_DIM = 64
ATTN_WIDTH = N_HEADS * HEAD_DIM
CONV_WIDTH = 512
CONV_K = 31
DILATED_GROUPS = ((128, 1), (512, 4), (2048, 16))
BLK = 128
ROPE_THETA = 10000.0
N_EXPERT_GROUPS = 4
EXPERTS_PER_GROUP = 8
N_EXPERTS = N_EXPERT_GROUPS * EXPERTS_PER_GROUP
EXPERT_FF = 512
TOP_K_IN_GROUP = 2
MOE_BLOCK = 128
PLE_DIM = 256
EPS = 1e-6
NEG_INF = -1e30

W_IN_SPLITS = (ATTN_WIDTH, 2 * ATTN_WIDTH, 3 * ATTN_WIDTH,
               3 * ATTN_WIDTH + CONV_WIDTH, 3 * ATTN_WIDTH + 2 * CONV_WIDTH,
               3 * ATTN_WIDTH + 2 * CONV_WIDTH + D_MODEL)
W_IN_COLS = 3 * ATTN_WIDTH + 2 * CONV_WIDTH + 2 * D_MODEL

kernel_name = "hybrid_dilated_attn_conformer_hmoe_ple"


def _rms_norm(t, g):
    tf = t.astype(jnp.float32)
    y = tf * lax.rsqrt(jnp.mean(tf * tf, axis=-1, keepdims=True) + EPS)
    return (y * g.astype(jnp.float32)).astype(t.dtype)


def _layer_norm(t, g, b):
    tf = t.astype(jnp.float32)
    mu = jnp.mean(tf, axis=-1, keepdims=True)
    var = jnp.mean(jnp.square(tf - mu), axis=-1, keepdims=True)
    y = (tf - mu) * lax.rsqrt(var + EPS)
    return (y * g.astype(jnp.float32) + b.astype(jnp.float32)).astype(t.dtype)


def _rope(t):
    S = t.shape[1]
    inv = jnp.power(ROPE_THETA, -jnp.arange(0, HEAD_DIM, 2, dtype=jnp.float32) / HEAD_DIM)
    ang = jnp.arange(S, dtype=jnp.float32)[:, None] * inv[None, :]
    cos = jnp.cos(ang)[None, :, None, :]
    sin = jnp.sin(ang)[None, :, None, :]
    tf = t.astype(jnp.float32)
    t1, t2 = tf[..., :HEAD_DIM // 2], tf[..., HEAD_DIM // 2:]
    return jnp.concatenate([t1 * cos - t2 * sin, t1 * sin + t2 * cos], axis=-1).astype(t.dtype)


def _to_dilated_blocks(t, dilation):
    B, S = t.shape[0], t.shape[1]
    rest = t.shape[2:]
    seg = dilation * BLK
    Sp = -(-S // seg) * seg
    t = jnp.pad(t, [(0, 0), (0, Sp - S)] + [(0, 0)] * len(rest))
    t = t.reshape((B, Sp // dilation, dilation) + rest)
    t = jnp.moveaxis(t, 2, 1)
    return t.reshape((B, dilation, Sp // seg, BLK) + rest)


def _from_dilated_blocks(t, S):
    B, d, nb, blk = t.shape[:4]
    rest = t.shape[4:]
    t = t.reshape((B, d, nb * blk) + rest)
    t = jnp.moveaxis(t, 1, 2).reshape((B, nb * blk * d) + rest)
    return t[:, :S]


def _dilated_window_attention(q, k, v, window, dilation):
    band = window // dilation
    assert band <= BLK
    S = q.shape[1]
    qb, kb, vb = (_to_dilated_blocks(t, dilation) for t in (q, k, v))

    def with_prev(t):
        prev = jnp.pad(t, ((0, 0), (0, 0), (1, 0), (0, 0), (0, 0), (0, 0)))[:, :, :-1]
        return jnp.concatenate([prev, t], axis=3)

    kk, vv = with_prev(kb), with_prev(vb)
    s = jnp.einsum('brnqhe,brnkhe->brnhqk', qb, kk,
                   preferred_element_type=jnp.float32) * (HEAD_DIM ** -0.5)
    nb = qb.shape[2]
    qi = jnp.arange(BLK)[:, None] + BLK
    kj = jnp.arange(2 * BLK)[None, :]
    in_band = (kj <= qi) & (qi - kj <= band)
    not_before_start = (jnp.arange(nb)[:, None, None] > 0) | (kj >= BLK)[None]
    allowed = in_band[None] & not_before_start
    s = jnp.where(allowed[:, None], s, NEG_INF)
    m = jnp.max(s, axis=-1)
    pexp = jnp.exp(s - m[..., None])
    l = jnp.sum(pexp, axis=-1)
    acc = jnp.einsum('brnhqk,brnkhe->brnqhe', pexp.astype(v.dtype), vv,
                     preferred_element_type=jnp.float32)
    m = jnp.moveaxis(m, 3, 4)
    l = jnp.moveaxis(l, 3, 4)
    return (_from_dilated_blocks(acc, S), _from_dilated_blocks(m, S), _from_dilated_blocks(l, S))


def _dilated_mixture_attention(q, k, v):
    parts = [_dilated_window_attention(q, k, v, w, d) for (w, d) in DILATED_GROUPS]
    M = parts[0][1]
    for _, m_g, _ in parts[1:]:
        M = jnp.maximum(M, m_g)
    num = 0.0
    den = 0.0
    for acc_g, m_g, l_g in parts:
        scale = jnp.exp(m_g - M)
        num = num + acc_g * scale[..., None]
        den = den + l_g * scale
    o = num / den[..., None]
    B, S = q.shape[0], q.shape[1]
    return o.reshape(B, S, ATTN_WIDTH).astype(q.dtype)


def _conformer_conv(cu, cg, w_dw, b_dw, ln_g, ln_b):
    u = cu * jax.nn.sigmoid(cg)
    rhs = w_dw.reshape(CONV_K, 1, CONV_WIDTH).astype(u.dtype)
    y = lax.conv_general_dilated(u, rhs, window_strides=(1,), padding=[(CONV_K - 1, 0)],
                                 dimension_numbers=('NWC', 'WIO', 'NWC'),
                                 feature_group_count=CONV_WIDTH)
    y = y + b_dw
    y = _layer_norm(y, ln_g, ln_b)
    return jax.nn.silu(y)


def _hier_moe(h, w_rg, b_rg, w_re, b_re, w_gate, w_up, w_down):
    B, S, D = h.shape
    T = B * S
    hf = h.reshape(T, D)
    lg = (hf @ w_rg + b_rg).astype(jnp.float32)
    pg = jax.nn.softmax(lg, axis=-1)
    gsel = jnp.argmax(lg, axis=-1)
    le = (hf @ w_re + b_re).astype(jnp.float32).reshape(T, N_EXPERT_GROUPS, EXPERTS_PER_GROUP)
    le_sel = jnp.take_along_axis(le, gsel[:, None, None], axis=1)[:, 0]
    top_v, top_i = lax.top_k(le_sel, TOP_K_IN_GROUP)
    gate = jnp.take_along_axis(pg, gsel[:, None], axis=1) * jax.nn.softmax(top_v, axis=-1)
    eid = gsel[:, None] * EXPERTS_PER_GROUP + top_i

    A = T * TOP_K_IN_GROUP
    flat_e = eid.reshape(-1)
    order = jnp.argsort(flat_e)
    sorted_e = flat_e[order]
    counts = jnp.bincount(flat_e, length=N_EXPERTS)
    padded = ((counts + MOE_BLOCK - 1) // MOE_BLOCK) * MOE_BLOCK
    ends = jnp.cumsum(padded)
    pad_start = ends - padded
    start = jnp.cumsum(counts) - counts
    dest = pad_start[sorted_e] + (jnp.arange(A) - start[sorted_e])
    n_rows = (-(-A // MOE_BLOCK)) * MOE_BLOCK + N_EXPERTS * MOE_BLOCK
    n_blocks = n_rows // MOE_BLOCK
    tok = order // TOP_K_IN_GROUP
    buf = jnp.zeros((n_rows, D), hf.dtype).at[dest].set(hf[tok])
    block_e = jnp.minimum(jnp.searchsorted(ends, jnp.arange(n_blocks) * MOE_BLOCK, side='right'),
                          N_EXPERTS - 1)

    def expert_block(args):
        xb, e = args
        hb = jax.nn.silu(xb @ w_gate[e]) * (xb @ w_up[e])
        return hb @ w_down[e]

    yb = lax.map(expert_block, (buf.reshape(n_blocks, MOE_BLOCK, D), block_e))
    y_rows = yb.reshape(n_rows, D)[dest]
    contrib = y_rows * gate.reshape(-1)[order][:, None].astype(y_rows.dtype)
    out = jnp.zeros((T, D), hf.dtype).at[tok].add(contrib)
    return out.reshape(B, S, D)


def setup_inputs(seed: int = 0) -> dict:
    key = jax.random.key(seed)
    ks = jax.random.split(key, 24)
    L, D, f32 = DEPTH, D_MODEL, jnp.float32

    def nrm(k, shape, fan_in):
        return jax.random.normal(k, shape, f32) * (fan_in ** -0.5)

    def gain(k, shape):
        return 1.0 + 0.05 * jax.random.normal(k, shape, f32)

    def small(k, shape, s=0.02):
        return s * jax.random.normal(k, shape, f32)

    return {
        "x": jax.random.normal(ks[0], (BATCH, SEQ, D), f32),
        "p": jax.random.normal(ks[1], (DEPTH, BATCH, SEQ, PLE_DIM), f32),
        "norm_mix": gain(ks[2], (L, D)),
        "w_in": nrm(ks[3], (L, D, W_IN_COLS), D),
        "w_dw": nrm(ks[4], (L, CONV_K, CONV_WIDTH), CONV_K),
        "b_dw": small(ks[5], (L, CONV_WIDTH)),
        "ln_conv_g": gain(ks[6], (L, CONV_WIDTH)),
        "ln_conv_b": small(ks[7], (L, CONV_WIDTH)),
        "w_attn_out": nrm(ks[8], (L, ATTN_WIDTH, D), ATTN_WIDTH),
        "w_conv_out": nrm(ks[9], (L, CONV_WIDTH, D), CONV_WIDTH),
        "b_conv_out": small(ks[10], (L, D)),
        "w_out": nrm(ks[11], (L, D, D), D),
        "norm_ffn": gain(ks[12], (L, D)),
        "w_route_group": nrm(ks[13], (L, D, N_EXPERT_GROUPS), D),
        "b_route_group": small(ks[14], (L, N_EXPERT_GROUPS), 0.01),
        "w_route_expert": nrm(ks[15], (L, D, N_EXPERTS), D),
        "b_route_expert": small(ks[16], (L, N_EXPERTS), 0.01),
        "w_exp_gate": nrm(ks[17], (L, N_EXPERTS, D, EXPERT_FF), D),
        "w_exp_up": nrm(ks[18], (L, N_EXPERTS, D, EXPERT_FF), D),
        "w_exp_down": nrm(ks[19], (L, N_EXPERTS, EXPERT_FF, D), EXPERT_FF),
        "norm_ple": gain(ks[20], (L, D)),
        "w_ple_proj": nrm(ks[21], (L, PLE_DIM, D), PLE_DIM),
        "w_ple_gate": nrm(ks[22], (L, D, D), D),
        "norm_final": gain(ks[23], (D,)),
    }


def reference(x, p, norm_mix, w_in, w_dw, b_dw, ln_conv_g, ln_conv_b, w_attn_out,
              w_conv_out, b_conv_out, w_out, norm_ffn, w_route_group, b_route_group,
              w_route_expert, b_route_expert, w_exp_gate, w_exp_up, w_exp_down,
              norm_ple, w_ple_proj, w_ple_gate, norm_final):
    B, S, _ = x.shape
    for i in range(DEPTH):
        h = _rms_norm(x, norm_mix[i])
        z = h @ w_in[i]
        q, k, v, cu, cg, ga, gb = jnp.split(z, W_IN_SPLITS, axis=-1)
        q = _rope(q.reshape(B, S, N_HEADS, HEAD_DIM))
        k = _rope(k.reshape(B, S, N_HEADS, HEAD_DIM))
        v = v.reshape(B, S, N_HEADS, HEAD_DIM)
        y_a = _dilated_mixture_attention(q, k, v) @ w_attn_out[i]
        y_b = _conformer_conv(cu, cg, w_dw[i], b_dw[i], ln_conv_g[i], ln_conv_b[i]) \
            @ w_conv_out[i] + b_conv_out[i]
        merged = jax.nn.sigmoid(ga) * y_a + jax.nn.sigmoid(gb) * y_b
        x = x + merged @ w_out[i]
        h2 = _rms_norm(x, norm_ffn[i])
        x = x + _hier_moe(h2, w_route_group[i], b_route_group[i], w_route_expert[i],
                          b_route_expert[i], w_exp_gate[i], w_exp_up[i], w_exp_down[i])
        g_ple = jax.nn.sigmoid(_rms_norm(x, norm_ple[i]) @ w_ple_gate[i])
        x = x + g_ple * (p[i] @ w_ple_proj[i])
    return _rms_norm(x, norm_final)
```

```python
import contextlib
import numpy as np
import concourse.bass as bass
import concourse.mybir as mybir
from concourse.bass_utils import run_bass_kernel_spmd

F32 = mybir.dt.float32
BF16 = mybir.dt.bfloat16
ALU = mybir.AluOpType
AF = mybir.ActivationFunctionType
AX = mybir.AxisListType

S = 2048
D = 1024
L = 2
NT = 16
NE = 32
WIN_EXT = 5632
PPL = 176
EPS = 1e-6
BIG = 1.0e30

STRICT_SAME = True


class T:
    __slots__ = ("ap", "space", "lo", "hi", "w", "r", "ov", "name")

    def __init__(self, ap, space, lo, hi, name=""):
        self.ap, self.space, self.lo, self.hi, self.name = ap, space, lo, hi, name
        self.w = None
        self.r = {}
        self.ov = [self]


class Op:
    __slots__ = ("eng", "fn", "deps", "ddeps", "sig", "count", "dkey", "dval", "is_write")

    def __init__(self, eng, fn):
        self.eng, self.fn = eng, fn
        self.deps = []
        self.ddeps = {}
        self.sig = False
        self.count = None
        self.dkey = None
        self.dval = None
        self.is_write = False


class Prog:
    def __init__(self, nc):
        self.nc = nc
        self.ops = {e: [] for e in ("pe", "act", "dve", "pool", "sp")}
        self.tiles = {"sb": [], "ps": []}
        self.dma_counts = {}
        self.reg_init = {}
        self.regs = {}

    def tile(self, ap, space, lo, hi, name=""):
        t = T(ap, space, lo, hi, name)
        for o in self.tiles[space]:
            if o.lo < hi and lo < o.hi:
                o.ov.append(t)
                t.ov.append(o)
        self.tiles[space].append(t)
        return t

    def _dep_on(self, op, prod, raw=True):
        if prod is None or prod is op:
            return
        if prod.dkey is not None:
            k = prod.dkey
            if not raw and op.dkey == k and prod.dval is not None and prod.is_write:
                return
            v = self.dma_counts[k]
            if op.ddeps.get(k, 0) < v:
                op.ddeps[k] = v
            return
        if prod.eng == op.eng and op.dkey is None and (op.eng == "pe" or not STRICT_SAME):
            return
        prod.sig = True
        op.deps.append(prod)

    def op(self, eng, fn, reads=(), writes=(), dkey=None):
        o = Op(eng, fn)
        o.dkey = dkey
        o.is_write = len(writes) > 0
        for t in reads:
            for u in t.ov:
                self._dep_on(o, u.w)
        for t in writes:
            for u in t.ov:
                self._dep_on(o, u.w, raw=False)
                for rd in u.r.values():
                    self._dep_on(o, rd)
        if dkey is not None:
            self.dma_counts[dkey] = self.dma_counts.get(dkey, 0) + 16
            o.dval = self.dma_counts[dkey]
        stream = eng if dkey is None else ("dma", id(o))
        for t in reads:
            t.r[stream] = o
        for t in writes:
            t.w = o
            t.r = {}
        self.ops[eng].append(o)
        return o

    def emit(self):
        nc = self.nc
        with contextlib.ExitStack() as st:
            sems = {e: st.enter_context(nc.semaphore("s_" + e)) for e in ("pe", "act", "dve", "pool")}
            dsems = {k: st.enter_context(nc.semaphore("d_" + str(k))) for k in self.dma_counts}
            for e, lst in self.ops.items():
                c = 0
                for o in lst:
                    if o.dkey is None and o.sig:
                        c += 1
                        o.count = c
            block = st.enter_context(nc.Block())

            def run(engname, engobj):
                waited = {}
                if engname == "pool":
                    for nm, val in self.reg_init.items():
                        r = engobj.alloc_register("bnd_" + nm)
                        engobj.reg_mov(r, val)
                        self.regs[nm] = r
                for o in self.ops[engname]:
                    best = {}
                    for p in o.deps:
                        if best.get(p.eng, 0) < p.count:
                            best[p.eng] = p.count
                    for pe_, cnt in best.items():
                        if waited.get(("c", pe_), 0) < cnt:
                            engobj.wait_ge(sems[pe_], cnt)
                            waited[("c", pe_)] = cnt
                    for k, v in o.ddeps.items():
                        if waited.get(("d", k), 0) < v:
                            engobj.wait_ge(dsems[k], v)
                            waited[("d", k)] = v
                    ins = o.fn(engobj)
                    if o.dkey is not None:
                        ins.then_inc(dsems[o.dkey], 16)
                    elif o.sig:
                        ins.then_inc(sems[engname], 1)

            @block.sync
            def _(e):
                run("sp", e)

            @block.tensor
            def _(e):
                run("pe", e)

            @block.scalar
            def _(e):
                run("act", e)

            @block.vector
            def _(e):
                run("dve", e)

            @block.gpsimd
            def _(e):
                run("pool", e)


def build_program(nl=L):
    nc = bass.Bass("TRN2", target_bir_lowering=False)

    def din(name, shape):
        return nc.dram_tensor(name, list(shape), F32, kind="ExternalInput").ap()

    x_d = din("x", [S, D])
    p_d = din("p", [L, S, 256])
    win_d = din("win", [L, D, WIN_EXT])
    wao_d = din("wao", [L, 512, D])
    wco_d = din("wco", [L, 512, D])
    wout_d = din("wout", [L, D, D])
    wr_d = din("wr", [L, D, 36])
    weg_d = [nc.dram_tensor(f"weg{l}", [8192, 2048], F32, kind="ExternalInput") for l in range(L)]
    weu_d = [nc.dram_tensor(f"weu{l}", [8192, 2048], F32, kind="ExternalInput") for l in range(L)]
    wed_d = [nc.dram_tensor(f"wed{l}", [8192, 2048], F32, kind="ExternalInput") for l in range(L)]
    cst_d = din("cst", [128, 384])
    scr_d = nc.dram_tensor("moe_scr", [18432, D], F32)
    ys_d = scr_d
    sorted_d = scr_d[:, :].bitcast(BF16).rearrange("r (two c) -> (r two) c", two=2)
    wpg_d = din("wpg", [L, D, D])
    wpe_d = din("wpe", [L, 256, D])
    pp_d = din("pp", [128, L * PPL])
    bc_d = din("bc", [128, 72 + D])
    cos_d = din("cosT", [128, S])
    sin_d = din("sinT", [128, S])
    mask_d = din("maskT", [128, S])
    id_d = din("ident", [128, 128])
    out_d = nc.dram_tensor("out", [S, D], F32, kind="ExternalOutput").ap()
    dbg_d = nc.dram_tensor("dbg", [128, 512], F32, kind="ExternalOutput").ap() if _DBG == 6 else None

    NB = 212800
    with contextlib.ExitStack() as st:
        big = st.enter_context(nc.sbuf_tensor("arena", [128, NB // 4], F32))
        pbanks = [st.enter_context(nc.psum_tensor(f"pb{i}", [128, 512], F32)) for i in range(8)]
        P = Prog(nc)
        P.reg_init = {'b8191': 8191, 'b11775': 11775, 'b36351': 24576 + 11775}
        top = [0]

        def alloc(nbytes, at=None):
            nbytes = (nbytes + 31) // 32 * 32
            if at is None:
                at = top[0]
                top[0] = at + nbytes
            assert at + nbytes <= NB, ("SBUF overflow", at, nbytes)
            return at, at + nbytes

        def mk(n, dtype, name, at=None):
            es = 2 if dtype == BF16 else 4
            lo, hi = alloc(n * es, at)
            ap = big[:, lo // 4:hi // 4]
            if dtype != F32:
                ap = ap.bitcast(dtype)
            return P.tile(ap[:, 0:n], "sb", lo, hi, name)

        ps = [P.tile(pbanks[i][:, :], "ps", i * 2048, (i + 1) * 2048, f"ps{i}") for i in range(8)]
        rr = {"b": 0, "s": 0, "m": 0}

        nbank = [6]

        def bank():
            rr["b"] = (rr["b"] + 1) % nbank[0]
            return ps[rr["b"]]

        X = [[mk(512, F32, f"x{t}_{h}") for h in range(2)] for t in range(NT)]
        x0 = X[0][0].lo

        def xrow(t):
            lo = x0 + t * 4096
            return big[:, lo // 4: lo // 4 + 1024]
        ident = mk(128, F32, "ident")
        identb = mk(128, BF16, "identb")
        ones512 = mk(128, F32, "ones512")
        maskT = mk(S, BF16, "mask")
        pp = mk(L * PPL, F32, "pp")
        bcp = mk(72, F32, "bc")
        scr = [mk(512, F32, f"scr{i}") for i in range(5)]
        smalls = [mk(36, F32, f"sm{i}") for i in range(18)]
        PH0 = top[0]

        def scratch():
            rr["s"] = (rr["s"] + 1) % 5
            return scr[rr["s"]]

        def small():
            rr["m"] = (rr["m"] + 1) % 18
            return smalls[rr["m"]]

        top[0] = PH0
        kT = [[mk(512, BF16, f"kT{c}_{g}") for g in range(4)] for c in range(4)]
        vA = [mk(520, BF16, f"vA{t}") for t in range(NT)]
        hT = [mk(512, BF16, f"hT{c}") for c in range(8)]
        ring = [mk(2048, BF16, f"ring{i}") for i in range(6)]
        qT = [mk(512, BF16, f"qT{c}") for c in range(4)]
        cos2 = [mk(512, F32, f"cosg{i}") for i in range(2)]
        sin2 = [mk(512, F32, f"sing{i}") for i in range(2)]
        U = [mk(542, BF16, f"u{c}") for c in range(4)]
        dgs = [mk(128, BF16, f"dg{j}") for j in range(31)]
        junk_mix = mk(1024, BF16, "junk_mix", at=dgs[0].lo)
        Y = [mk(512, F32, f"y{c}") for c in range(4)]
        sT = [mk(512, BF16, f"sT{c}") for c in range(4)]
        pexp = [mk(512, BF16, f"pexp{i}") for i in range(3)]
        pmsk = [mk(512, BF16, f"pmsk{i}") for i in range(3)]
        otok = [mk(512, BF16, f"otok{j}") for j in range(4)]
        oT = [mk(512, BF16, f"oT{c}") for c in range(4)]
        mg = [mk(512, BF16, f"mg{c}") for c in range(8)]
        xnb = [mk(1024, BF16, f"xnb{i}", at=Y[0].lo + i * 2048) for i in range(4)]
        MIX_END = top[0]

        top[0] = PH0
        h2T = [[mk(512, BF16, f"h2T{c}_{g}") for g in range(4)] for c in range(8)]
        H2LO = h2T[0][0].lo
        xnb2 = [mk(1024, BF16, f"xnb2_{i}") for i in range(4)]
        XNLO = xnb2[0].lo
        junk_moe = mk(1024, BF16, "junk_moe")
        wrt = mk(8 * 36, BF16, "wrt")
        OH1 = [mk(32, F32, f"oh1_{t}") for t in range(NT)]
        OH2 = [mk(32, F32, f"oh2_{t}") for t in range(NT)]
        ABF = [mk(32, BF16, f"abf{t}") for t in range(NT)]
        RK = [mk(32, F32, f"rk{t}") for t in range(NT)]
        G12 = [mk(2, F32, f"g12_{t}") for t in range(NT)]
        DF = [mk(2, F32, f"df{t}") for t in range(NT)]
        DI = [mk(2, mybir.dt.int32, f"di{t}") for t in range(NT)]
        DIS = [mk(2, mybir.dt.int32, f"dis{t}") for t in range(NT)]
        cst = mk(384, F32, "cst")
        np_t = mk(32, F32, "np_t")
        oend_t = mk(32, F32, "oend")
        ops_t = mk(32, F32, "ops")
        q_t = mk(32, F32, "q_t")
        ltri = mk(128, BF16, "ltri")
        onesb = mk(128, BF16, "onesb")
        cntn = mk(32, F32, "cntn")
        nn_t = mk(32, F32, "nn")
        end_t = mk(32, F32, "end")
        psb_t = mk(32, F32, "psb")
        ebacc = mk(64, F32, "ebacc")
        idxg = mk(64, mybir.dt.int32, "idxg")
        idxd = mk(64, mybir.dt.int32, "idxd")
        GT = mk(1024, BF16, "GT")
        xs_t = [mk(1024, BF16, f"xs{i}") for i in range(2)]
        xg_t = [mk(1024, BF16, f"xg{i}") for i in range(4)]
        xbT = [mk(1024, BF16, f"xbT{i}") for i in range(2)]
        hbt = [mk(512, BF16, f"hbt{i}") for i in range(2)]
        hbT = [mk(512, BF16, f"hbT{i}") for i in range(2)]
        yst = [mk(1024, F32, f"yst{i}") for i in range(2)]
        yst.append(mk(1024, F32, "yst2", at=xs_t[0].lo))
        yst.append(mk(1024, F32, "yst3", at=OH1[0].lo))
        PH1 = top[0]
        ew = [[mk(4096, BF16, f"ew{s}_{m}", at=(H2LO + m * 8192) if s == 0 else None) for m in range(3)] for s in range(2)]
        ycmb = [mk(1024, F32, f"ycmb{i}", at=XNLO + i * 4096) for i in range(2)]
        wpe = mk(2048, BF16, "wpe")
        pT = [mk(S, BF16, f"pT{i}") for i in range(2)]
        pst = [mk(256, F32, f"pst{i}") for i in range(2)]
        MOE_END = top[0]
        top[0] = PH1
        wpg = [mk(4096, BF16, f"wpg{i}") for i in range(2)]
        ost = [mk(1024, F32, f"ost{i}") for i in range(2)]
        nfin = mk(D, F32, "nfin")
        PLE_END = top[0]
        print("SBUF bytes/partition: mixer", MIX_END, "moe", MOE_END, "ple", PLE_END, "limit", NB)

        dram_tiles = {}
        _dn = [0]
        for t_ in range(NT):
            for a_ in range(2):
                _dn[0] += 1
                dram_tiles[("sc", t_, a_)] = P.tile(None, "sb", 10 ** 9 + 10 * _dn[0], 10 ** 9 + 10 * _dn[0] + 1, "scd")
        for b_ in range(92):
            _dn[0] += 1
            dram_tiles[("ys", b_)] = P.tile(None, "sb", 10 ** 9 + 10 * _dn[0], 10 ** 9 + 10 * _dn[0] + 1, "ysd")

        dram_tiles[('gser',)] = P.tile(None, 'sb', 2 * 10 ** 9, 2 * 10 ** 9 + 1, 'gser')
        P.op("sp", lambda e: e.dma_start(out=ident.ap, in_=id_d), writes=[ident], dkey="c0")
        P.op("sp", lambda e: e.dma_start(out=pp.ap, in_=pp_d), writes=[pp], dkey="c0")
        P.op("sp", lambda e: e.dma_start(out=bcp.ap, in_=bc_d[:, 0:72]), writes=[bcp], dkey="c0")
        P.op("pool", lambda e: e.dma_start(out=maskT.ap, in_=mask_d), writes=[maskT], dkey="c1")
        P.op("pool", lambda e: e.dma_start(out=identb.ap, in_=id_d), writes=[identb], dkey="c1")
        P.op("pool", lambda e: e.memset(ones512.ap, 1.0 / 512), writes=[ones512])
        for g in range(4):
            for i in range(4):
                t = 4 * g + i
                for h in range(2):
                    P.op("sp", lambda e, t=t, h=h: e.dma_start(out=X[t][h].ap, in_=x_d[t * 128:(t + 1) * 128, h * 512:(h + 1) * 512]),
                         writes=[X[t][h]], dkey=f"xg{g}")

        def ppc(col):
            return pp.ap[:, col:col + 1]

        def row_rstd(t, junk):
            ss = small()
            P.op("act", lambda e: e.activation(out=junk.ap, in_=xrow(t), func=AF.Square, accum_out=ss.ap[:, 0:1]),
                 reads=[X[t][0], X[t][1]], writes=[ss, junk])
            P.op("act", lambda e: e.activation(out=ss.ap[:, 1:2], in_=ss.ap[:, 0:1], func=AF.Sqrt, bias=EPS, scale=1.0 / D),
                 reads=[ss], writes=[ss])
            P.op("dve", lambda e: e.reciprocal(out=ss.ap[:, 2:3], in_=ss.ap[:, 1:2]), reads=[ss], writes=[ss])
            return ss

        def norm_A(g, xn_tiles):
            for i in range(4):
                t = 4 * g + i
                ss = row_rstd(t, junk_mix if xn_tiles is xnb else junk_moe)
                P.op("dve", lambda e, t=t, i=i, ss=ss: e.tensor_scalar(out=xn_tiles[i].ap, in0=xrow(t), scalar1=ss.ap[:, 2:3],
                                                                       scalar2=None, op0=ALU.mult),
                     reads=[X[t][0], X[t][1], ss], writes=[xn_tiles[i]])

        def norm_T(g, gcol, dst, xn_tiles, do_a=True):
            if do_a:
                norm_A(g, xn_tiles)
            for c in range(8):
                b = bank()
                bv = b.ap.bitcast(BF16)
                for i in range(4):
                    P.op("pe", lambda e, i=i, c=c, bv=bv: e.transpose(out=bv[:, i * 128:(i + 1) * 128],
                                                                      in_=xn_tiles[i].ap[:, c * 128:(c + 1) * 128], identity=identb.ap),
                         reads=[xn_tiles[i], identb], writes=[b])
                P.op("act", lambda e, c=c, bv=bv: e.activation(out=dst[c].ap, in_=bv[:, 0:512], func=AF.Copy, scale=ppc(gcol + c)),
                     reads=[b, pp], writes=[dst[c]])

        def mm_acc(out_ap, out_t, pairs, extra_reads):
            n = len(pairs)
            for k, (lh, rh) in enumerate(pairs):
                P.op("pe", lambda e, lh=lh, rh=rh, k=k: e.matmul(out_ap, lhsT=lh, rhs=rh, start=(k == 0), stop=(k == n - 1)),
                     reads=extra_reads, writes=[out_t])

        ring_i = [0]

        def wload(srcs, key_reads=()):
            ring_i[0] = (ring_i[0] + 1) % 6
            slot = ring[ring_i[0]]
            si = ring_i[0]
            views = []
            off = 0
            for (src, k, c) in srcs:
                v = slot.ap[:, off:off + k * c].rearrange("p (k c) -> p k c", k=k)
                views.append(v)
                P.op("pool", lambda e, v=v, src=src: e.dma_start(out=v, in_=src), writes=[slot], dkey=f"ring{si}")
                off += k * c
            return slot, views

        def mixer_group(l, g):
            pb = l * PPL
            norm_T(g, pb + 0, hT, xnb, do_a=(g == 0))
            gi = l * 4 + g
            cosg, sing = cos2[gi % 2], sin2[gi % 2]

            def cs_load(gi_):
                g_ = gi_ % 4
                P.op("sp", lambda e: e.dma_start(out=cos2[gi_ % 2].ap, in_=cos_d[:, g_ * 512:(g_ + 1) * 512]), writes=[cos2[gi_ % 2]], dkey=f"cs{gi_ % 2}")
                P.op("sp", lambda e: e.dma_start(out=sin2[gi_ % 2].ap, in_=sin_d[:, g_ * 512:(g_ + 1) * 512]), writes=[sin2[gi_ % 2]], dkey=f"cs{gi_ % 2}")
            if g == 0:
                cs_load(gi)
            if g < 3:
                cs_load(gi + 1)
            winl = win_d[l].rearrange("(k p) c -> p k c", p=128)
            waol = wao_d[l].rearrange("(k p) c -> p k c", p=128)
            wcol = wco_d[l].rearrange("(k p) c -> p k c", p=128)
            woutl = wout_d[l].rearrange("(k p) c -> p k c", p=128)

            def win_blk(c0):
                return [(winl[:, :, c0:c0 + 256], 8, 256)]

            tasks = []

            def rope_task(base, dst_fn):
                for b in range(2):
                    def comp(slots, b=b):
                        (s1, v1), (s2, v2) = slots
                        for cc in range(2):
                            c = 2 * b + cc
                            bq = bank()
                            mm_acc(bq.ap, bq, [(v1[0][:, k, cc * 128:(cc + 1) * 128], hT[k].ap) for k in range(8)], [s1] + hT)
                            bs = bank()
                            mm_acc(bs.ap, bs, [(v2[0][:, k, cc * 128:(cc + 1) * 128], hT[k].ap) for k in range(8)], [s2] + hT)
                            t1 = scratch()
                            P.op("dve", lambda e, t1=t1, bq=bq: e.tensor_tensor(out=t1.ap, in0=bq.ap, in1=cosg.ap, op=ALU.mult),
                                 reads=[bq, cosg], writes=[t1])
                            t2 = scratch()
                            P.op("dve", lambda e, t2=t2, bs=bs: e.tensor_tensor(out=t2.ap, in0=bs.ap, in1=sing.ap, op=ALU.mult),
                                 reads=[bs, sing], writes=[t2])
                            dst = dst_fn(c)
                            P.op("pool", lambda e, t1=t1, t2=t2, dst=dst: e.tensor_tensor(out=dst.ap, in0=t1.ap, in1=t2.ap, op=ALU.add),
                                 reads=[t1, t2], writes=[dst])
                    tasks.append(([win_blk(base + 256 * b), win_blk(base + 512 + 256 * b)], comp))
            rope_task(0, lambda c: qT[c])
            rope_task(1024, lambda c: kT[c][g])

            for b in range(2):
                def comp(slots, b=b):
                    (s1, v1), = slots
                    for i in range(4):
                        t = 4 * g + i
                        bv = bank()
                        mm_acc(bv.ap[:, 0:256], bv, [(hT[k].ap[:, i * 128:(i + 1) * 128], v1[0][:, k, :]) for k in range(8)], [s1] + hT)
                        vv = vA[t].ap.rearrange("p (h e) -> p h e", h=8)
                        if b == 0:
                            P.op("pool", lambda e, vv=vv: e.memset(vv[:, :, 64:65], 1.0), writes=[vA[t]])
                        P.op("act", lambda e, vv=vv, bv=bv, b=b: e.activation(
                            out=vv[:, 4 * b:4 * b + 4, 0:64], in_=bv.ap[:, 0:256].rearrange("p (h e) -> p h e", h=4), func=AF.Copy),
                            reads=[bv], writes=[vA[t]])
                tasks.append(([win_blk(2048 + 256 * b)], comp))

            for b in range(2):
                def comp(slots, b=b):
                    (s1, v1), (s2, v2) = slots
                    for cc in range(2):
                        c = 2 * b + cc
                        bu = bank()
                        mm_acc(bu.ap, bu, [(v1[0][:, k, cc * 128:(cc + 1) * 128], hT[k].ap) for k in range(8)], [s1] + hT)
                        bg = bank()
                        mm_acc(bg.ap, bg, [(v2[0][:, k, cc * 128:(cc + 1) * 128], hT[k].ap) for k in range(8)], [s2] + hT)
                        sg = scratch()
                        P.op("act", lambda e, sg=sg, bg=bg: e.activation(out=sg.ap, in_=bg.ap, func=AF.Sigmoid), reads=[bg], writes=[sg])
                        if g == 0:
                            P.op("pool", lambda e, c=c: e.memset(U[c].ap[:, 0:30], 0.0), writes=[U[c]])
                        else:
                            P.op("pool", lambda e, c=c: e.tensor_copy(out=U[c].ap[:, 0:30], in_=U[c].ap[:, 512:542]), reads=[U[c]], writes=[U[c]])
                        P.op("dve", lambda e, c=c, bu=bu, sg=sg: e.tensor_tensor(out=U[c].ap[:, 30:542], in0=bu.ap, in1=sg.ap, op=ALU.mult),
                             reads=[bu, sg], writes=[U[c]])
                        wc0 = pb + 44
                        for j in range(31):
                            if j % 2 == 0:
                                P.op("dve", lambda e, j=j, c=c: e.tensor_scalar(out=dgs[j].ap, in0=identb.ap, scalar1=ppc(wc0 + 4 * j + c), scalar2=None,
                                                                              op0=ALU.mult), reads=[identb, pp], writes=[dgs[j]])
                            else:
                                P.op("act", lambda e, j=j, c=c: e.activation(out=dgs[j].ap, in_=identb.ap, func=AF.Copy, scale=ppc(wc0 + 4 * j + c)),
                                     reads=[identb, pp], writes=[dgs[j]])
                        by = bank()
                        for j in range(31):
                            P.op("pe", lambda e, j=j, c=c, by=by: e.matmul(by.ap, lhsT=dgs[j].ap, rhs=U[c].ap[:, j:j + 512], start=(j == 0), stop=(j == 30)),
                                 reads=[dgs[j], U[c]], writes=[by])
                        P.op("dve", lambda e, c=c, by=by: e.tensor_scalar(out=Y[c].ap, in0=by.ap, scalar1=ppc(pb + 32 + c), scalar2=None, op0=ALU.add),
                             reads=[by, pp], writes=[Y[c]])
                tasks.append(([win_blk(2560 + 256 * b), win_blk(3072 + 256 * b)], comp))

            def comp_ln_attn(slots):
                bm = bank()
                mm_acc(bm.ap, bm, [(ones512.ap, Y[c].ap) for c in range(4)], [ones512] + Y)
                be = bank()
                for c in range(4):
                    sq = scratch()
                    P.op("act", lambda e, sq=sq, c=c: e.activation(out=sq.ap, in_=Y[c].ap, func=AF.Square), reads=[Y[c]], writes=[sq])
                    P.op("pe", lambda e, sq=sq, c=c: e.matmul(be.ap, lhsT=ones512.ap, rhs=sq.ap, start=(c == 0), stop=(c == 3)),
                         reads=[ones512, sq], writes=[be])
                msq = scratch()
                P.op("act", lambda e: e.activation(out=msq.ap, in_=bm.ap, func=AF.Square), reads=[bm], writes=[msq])
                var = scratch()
                P.op("dve", lambda e: e.tensor_tensor(out=var.ap, in0=be.ap, in1=msq.ap, op=ALU.subtract), reads=[be, msq], writes=[var])
                P.op("act", lambda e: e.activation(out=var.ap, in_=var.ap, func=AF.Sqrt, bias=EPS, scale=1.0), reads=[var], writes=[var])
                P.op("dve", lambda e: e.reciprocal(out=var.ap, in_=var.ap), reads=[var], writes=[var])
                for c in range(4):
                    yn = scratch()
                    P.op("dve", lambda e, yn=yn, c=c: e.tensor_tensor(out=yn.ap, in0=Y[c].ap, in1=bm.ap, op=ALU.subtract),
                         reads=[Y[c], bm], writes=[yn])
                    P.op("pool", lambda e, yn=yn: e.tensor_tensor(out=yn.ap, in0=yn.ap, in1=var.ap, op=ALU.mult), reads=[yn, var], writes=[yn])
                    P.op("act", lambda e, yn=yn, c=c: e.activation(out=sT[c].ap, in_=yn.ap, func=AF.Silu, scale=ppc(pb + 36 + c), bias=ppc(pb + 40 + c)),
                         reads=[yn, pp], writes=[sT[c]])
                if g < 3:
                    norm_A(g + 1, xnb)
                nkt = 4 * g + 4
                items = [(h, kt) for h in range(8) for kt in range(nkt)]
                stg = {}

                def S_(n):
                    h, kt = items[n]
                    c, pbase = h // 2, (h % 2) * 64
                    j0 = max(kt - 4 * g, 0)
                    c0 = j0 * 128
                    bs = bank()
                    ktile = kT[c][kt // 4]
                    P.op("pe", lambda e: e.matmul(
                        bs.ap[:, c0:512], lhsT=ktile.ap[pbase:pbase + 64, (kt % 4) * 128:(kt % 4) * 128 + 128],
                        rhs=qT[c].ap[pbase:pbase + 64, c0:512], start=True, stop=True),
                        reads=[ktile, qT[c]], writes=[bs])
                    pe_t = pexp[n % 3]
                    P.op("act", lambda e: e.activation(out=pe_t.ap[:, c0:512], in_=bs.ap[:, c0:512], func=AF.Exp, scale=0.125),
                         reads=[bs], writes=[pe_t])
                    pm_t = pmsk[n % 3]
                    o0 = 4 * g + j0 - kt
                    P.op("dve", lambda e: e.tensor_tensor(
                        out=pm_t.ap[:, c0:512], in0=pe_t.ap[:, c0:512], in1=maskT.ap[:, o0 * 128:o0 * 128 + 512 - c0], op=ALU.mult),
                        reads=[pe_t, maskT], writes=[pm_t])
                    stg[n] = (pm_t, j0)

                def V_(n):
                    h, kt = items[n]
                    pm_t, j0 = stg.pop(n)
                    accb = ps[6 + (h % 2)]
                    acc = accb.ap[:, 0:260].rearrange("p (j e) -> p j e", j=4)
                    for j in range(j0, 4):
                        P.op("pe", lambda e, j=j: e.matmul(
                            acc[:, j, :], lhsT=pm_t.ap[:, j * 128:(j + 1) * 128], rhs=vA[kt].ap[:, h * 65:(h + 1) * 65],
                            start=(kt == 0 and j == 0), stop=(kt == nkt - 1 and j == 3)),
                            reads=[pm_t, vA[kt]], writes=[accb])
                    if kt == nkt - 1:
                        rc = small()
                        P.op("dve", lambda e: e.reciprocal(out=rc.ap[:, 0:4], in_=acc[:, :, 64]), reads=[accb], writes=[rc])
                        for j in range(4):
                            P.op("dve", lambda e, j=j: e.tensor_scalar(
                                out=otok[j].ap[:, h * 64:(h + 1) * 64], in0=acc[:, j, 0:64], scalar1=rc.ap[:, j:j + 1], scalar2=None, op0=ALU.mult),
                                reads=[accb, rc], writes=[otok[j]])
                LA = 2
                for n in range(min(LA, len(items))):
                    S_(n)
                for n in range(len(items)):
                    if n + LA < len(items):
                        S_(n + LA)
                    V_(n)
                for c in range(4):
                    b = bank()
                    bv = b.ap.bitcast(BF16)
                    for j in range(4):
                        P.op("pe", lambda e, bv=bv, j=j, c=c: e.transpose(out=bv[:, j * 128:(j + 1) * 128], in_=otok[j].ap[:, c * 128:(c + 1) * 128],
                                                                          identity=identb.ap), reads=[otok[j], identb], writes=[b])
                    P.op("act", lambda e, bv=bv, c=c: e.activation(out=oT[c].ap, in_=bv[:, 0:512], func=AF.Copy), reads=[b], writes=[oT[c]])
            tasks.append(([], comp_ln_attn))

            for ob in range(4):
                def comp(slots, ob=ob):
                    (s1, v1), (s2, v2), (s3, v3) = slots
                    for cc in range(2):
                        oc = 2 * ob + cc
                        sl = slice(cc * 128, (cc + 1) * 128)
                        bga = bank()
                        mm_acc(bga.ap, bga, [(v1[0][:, k, sl], hT[k].ap) for k in range(8)], [s1] + hT)
                        bgb = bank()
                        mm_acc(bgb.ap, bgb, [(v2[0][:, k, sl], hT[k].ap) for k in range(8)], [s2] + hT)
                        bya = bank()
                        mm_acc(bya.ap, bya, [(v3[0][:, k, sl], oT[k].ap) for k in range(4)], [s3] + oT)
                        byb = bank()
                        mm_acc(byb.ap, byb, [(v3[1][:, k, sl], sT[k].ap) for k in range(4)], [s3] + sT)
                        sga = scratch()
                        P.op("act", lambda e, sga=sga, bga=bga: e.activation(out=sga.ap, in_=bga.ap, func=AF.Sigmoid), reads=[bga], writes=[sga])
                        sgb = scratch()
                        P.op("act", lambda e, sgb=sgb, bgb=bgb: e.activation(out=sgb.ap, in_=bgb.ap, func=AF.Sigmoid), reads=[bgb], writes=[sgb])
                        P.op("dve", lambda e, sga=sga, bya=bya: e.tensor_tensor(out=sga.ap, in0=bya.ap, in1=sga.ap, op=ALU.mult),
                             reads=[bya, sga], writes=[sga])
                        P.op("dve", lambda e, sgb=sgb, byb=byb, oc=oc: e.scalar_tensor_tensor(out=sgb.ap, in0=byb.ap, scalar=ppc(pb + 24 + oc), in1=sgb.ap,
                                                                                            op0=ALU.add, op1=ALU.mult),
                             reads=[byb, sgb, pp], writes=[sgb])
                        P.op("pool", lambda e, sga=sga, sgb=sgb, oc=oc: e.tensor_tensor(out=mg[oc].ap, in0=sga.ap, in1=sgb.ap, op=ALU.add),
                             reads=[sga, sgb], writes=[mg[oc]])
                tasks.append(([win_blk(3584 + 256 * ob), win_blk(4608 + 256 * ob),
                               [(waol[:, :, 256 * ob:256 * ob + 256], 4, 256), (wcol[:, :, 256 * ob:256 * ob + 256], 4, 256)]], comp))

            for nb_ in range(4):
                def comp(slots, nb_=nb_):
                    (s1, v1), = slots
                    for i in range(4):
                        t = 4 * g + i
                        b = bank()
                        mm_acc(b.ap[:, 0:256], b, [(mg[k].ap[:, i * 128:(i + 1) * 128], v1[0][:, k, :]) for k in range(8)], [s1] + mg)
                        xt = X[t][nb_ // 2]
                        xs = xt.ap[:, (nb_ % 2) * 256:(nb_ % 2) * 256 + 256]
                        P.op("dve", lambda e, xs=xs, b=b: e.tensor_tensor(out=xs, in0=b.ap[:, 0:256], in1=xs, op=ALU.add),
                             reads=[b, xt], writes=[xt])
                tasks.append(([[(woutl[:, :, 256 * nb_:256 * nb_ + 256], 8, 256)]], comp))

            return tasks

        def moe_phase(l):
            pb = l * PPL
            I32 = mybir.dt.int32
            P.op("sp", lambda e: e.dma_start(out=cst.ap, in_=cst_d), writes=[cst], dkey="cst")
            P.op("pool", lambda e: e.dma_start(out=ltri.ap, in_=cst_d[:, 192:320]), writes=[ltri], dkey="cstb")
            P.op("pool", lambda e: e.memset(onesb.ap, 1.0), writes=[onesb])
            for g in range(4):
                norm_T(g, pb + 8, [h2T[c][g] for c in range(8)], xnb2)
            P.op("pool", lambda e: e.dma_start(out=wrt.ap.rearrange("p (k c) -> p k c", k=8), in_=wr_d[l].rearrange("(k p) c -> p k c", p=128)),
                 writes=[wrt], dkey="wr")
            wrv = wrt.ap.rearrange("p (k c) -> p k c", k=8)

            def route_tile(t):
                g, i = t // 4, t % 4
                b = bank()
                mm_acc(b.ap[:, 0:36], b, [(h2T[k][g].ap[:, i * 128:(i + 1) * 128], wrv[:, k, :]) for k in range(8)],
                       [wrt] + [h2T[k][g] for k in range(8)])
                lg = small()
                P.op("dve", lambda e, lg=lg, b=b: e.tensor_tensor(out=lg.ap, in0=b.ap[:, 0:36], in1=bcp.ap[:, l * 36:(l + 1) * 36], op=ALU.add),
                     reads=[b, bcp], writes=[lg])
                w1 = small()
                W = w1.ap
                P.op("dve", lambda e, lg=lg, W=W: e.tensor_reduce(out=W[:, 0:1], in_=lg.ap[:, 0:4], axis=AX.X, op=ALU.max), reads=[lg], writes=[w1])
                P.op("dve", lambda e, W=W: e.tensor_scalar(out=W[:, 1:2], in0=W[:, 0:1], scalar1=-1.0, scalar2=None, op0=ALU.mult), reads=[w1], writes=[w1])
                P.op("act", lambda e, lg=lg, W=W: e.activation(out=W[:, 8:12], in_=lg.ap[:, 0:4], func=AF.Exp, bias=W[:, 1:2], scale=1.0, accum_out=W[:, 2:3]),
                     reads=[lg, w1], writes=[w1])
                P.op("dve", lambda e, W=W: e.reciprocal(out=W[:, 3:4], in_=W[:, 2:3]), reads=[w1], writes=[w1])
                P.op("dve", lambda e, lg=lg, W=W: e.tensor_scalar(out=W[:, 4:8], in0=lg.ap[:, 0:4], scalar1=W[:, 0:1], scalar2=None, op0=ALU.is_equal),
                     reads=[lg, w1], writes=[w1])
                P.op("dve", lambda e, W=W: e.tensor_scalar(out=W[:, 4:8], in0=W[:, 4:8], scalar1=BIG, scalar2=-BIG, op0=ALU.mult, op1=ALU.add),
                     reads=[w1], writes=[w1])
                lem = small()
                for gg in range(4):
                    P.op("dve", lambda e, lem=lem, lg=lg, W=W, gg=gg: e.tensor_scalar(out=lem.ap[:, gg * 8:(gg + 1) * 8], in0=lg.ap[:, 4 + gg * 8:12 + gg * 8],
                                                                                  scalar1=W[:, 4 + gg:5 + gg], scalar2=None, op0=ALU.add),
                         reads=[lg, w1], writes=[lem])
                w2 = small()
                V = w2.ap
                oh1, oh2 = OH1[t], OH2[t]
                lem2 = small()
                P.op("dve", lambda e, lem=lem, V=V: e.tensor_reduce(out=V[:, 0:1], in_=lem.ap[:, 0:32], axis=AX.X, op=ALU.max), reads=[lem], writes=[w2])
                P.op("dve", lambda e, lem=lem, V=V, oh1=oh1: e.tensor_scalar(out=oh1.ap, in0=lem.ap[:, 0:32], scalar1=V[:, 0:1], scalar2=None, op0=ALU.is_equal),
                     reads=[lem, w2], writes=[oh1])
                P.op("dve", lambda e, lem=lem, lem2=lem2, oh1=oh1: e.scalar_tensor_tensor(out=lem2.ap[:, 0:32], in0=oh1.ap, scalar=-BIG, in1=lem.ap[:, 0:32],
                                                                                  op0=ALU.mult, op1=ALU.add), reads=[oh1, lem], writes=[lem2])
                P.op("dve", lambda e, lem2=lem2, V=V: e.tensor_reduce(out=V[:, 1:2], in_=lem2.ap[:, 0:32], axis=AX.X, op=ALU.max), reads=[lem2], writes=[w2])
                P.op("dve", lambda e, lem2=lem2, V=V, oh2=oh2: e.tensor_scalar(out=oh2.ap, in0=lem2.ap[:, 0:32], scalar1=V[:, 1:2], scalar2=None, op0=ALU.is_equal),
                     reads=[lem2, w2], writes=[oh2])
                P.op("dve", lambda e, V=V: e.tensor_tensor(out=V[:, 2:3], in0=V[:, 1:2], in1=V[:, 0:1], op=ALU.subtract), reads=[w2], writes=[w2])
                P.op("act", lambda e, V=V: e.activation(out=V[:, 3:4], in_=V[:, 2:3], func=AF.Exp), reads=[w2], writes=[w2])
                P.op("dve", lambda e, V=V: e.tensor_scalar(out=V[:, 4:5], in0=V[:, 3:4], scalar1=1.0, scalar2=None, op0=ALU.add), reads=[w2], writes=[w2])
                P.op("dve", lambda e, V=V: e.reciprocal(out=V[:, 5:6], in_=V[:, 4:5]), reads=[w2], writes=[w2])
                P.op("dve", lambda e, V=V: e.tensor_tensor(out=V[:, 6:7], in0=V[:, 3:4], in1=V[:, 5:6], op=ALU.mult), reads=[w2], writes=[w2])
                P.op("dve", lambda e, V=V, W=W, t=t: e.tensor_scalar(out=G12[t].ap, in0=V[:, 5:7], scalar1=W[:, 3:4], scalar2=None, op0=ALU.mult),
                     reads=[w2, w1], writes=[G12[t]])
                P.op("dve", lambda e, oh1=oh1, oh2=oh2, t=t: e.tensor_tensor(out=ABF[t].ap, in0=oh1.ap, in1=oh2.ap, op=ALU.add),
                     reads=[oh1, oh2], writes=[ABF[t]])

            def capture(fn, *a):
                rec = []
                P.op = lambda *args, **kw: rec.append((args, kw))
                try:
                    fn(*a)
                finally:
                    del P.op
                return rec
            RB = 3
            for t0 in range(0, NT, RB):
                recs = [capture(route_tile, t) for t in range(t0, min(t0 + RB, NT))]
                for k_ in range(max(len(r) for r in recs)):
                    for r in recs:
                        if k_ < len(r):
                            P.op(*r[k_][0], **r[k_][1])

            for t in range(NT):
                b = bank()
                for tp in range(t):
                    P.op("pe", lambda e, b=b, tp=tp: e.matmul(b.ap[:, 0:32], lhsT=onesb.ap, rhs=ABF[tp].ap, start=(tp == 0), stop=False),
                         reads=[onesb, ABF[tp]], writes=[b])
                P.op("pe", lambda e, b=b, t=t: e.matmul(b.ap[:, 0:32], lhsT=ltri.ap, rhs=ABF[t].ap, start=(t == 0), stop=True),
                     reads=[ltri, ABF[t]], writes=[b])
                P.op("act", lambda e, b=b, t=t: e.activation(out=RK[t].ap, in_=b.ap[:, 0:32], func=AF.Copy), reads=[b], writes=[RK[t]])
            bc_ = bank()
            for t in range(NT):
                P.op("pe", lambda e, t=t: e.matmul(bc_.ap[:, 0:32], lhsT=onesb.ap, rhs=ABF[t].ap, start=(t == 0), stop=(t == NT - 1)),
                     reads=[onesb, ABF[t]], writes=[bc_])
            P.op("act", lambda e: e.activation(out=cntn.ap, in_=bc_.ap[:, 0:32], func=AF.Copy), reads=[bc_], writes=[cntn])
            P.op("dve", lambda e: e.tensor_scalar(out=nn_t.ap, in0=cntn.ap, scalar1=0.0, scalar2=None, op0=ALU.is_gt), reads=[cntn], writes=[nn_t])
            for j in range(1, 16):
                P.op("dve", lambda e, j=j: e.scalar_tensor_tensor(out=nn_t.ap, in0=cntn.ap, scalar=128.0 * j, in1=nn_t.ap, op0=ALU.is_gt, op1=ALU.add),
                     reads=[cntn, nn_t], writes=[nn_t])
            P.op("dve", lambda e: e.tensor_scalar(out=np_t.ap, in0=nn_t.ap, scalar1=-2.0, scalar2=0.0, op0=ALU.add, op1=ALU.max), reads=[nn_t], writes=[np_t])
            P.op("dve", lambda e: e.tensor_copy(out=oend_t.ap[:, 0:1], in_=np_t.ap[:, 0:1]), reads=[np_t], writes=[oend_t])
            for ee in range(1, NE):
                P.op("dve", lambda e, ee=ee: e.tensor_tensor(out=oend_t.ap[:, ee:ee + 1], in0=oend_t.ap[:, ee - 1:ee], in1=np_t.ap[:, ee:ee + 1], op=ALU.add),
                     reads=[oend_t, np_t], writes=[oend_t])
            P.op("dve", lambda e: e.tensor_tensor(out=ops_t.ap, in0=oend_t.ap, in1=np_t.ap, op=ALU.subtract), reads=[oend_t, np_t], writes=[ops_t])
            P.op("dve", lambda e: e.scalar_tensor_tensor(out=q_t.ap, in0=ops_t.ap, scalar=128.0, in1=cst.ap[:, 352:384], op0=ALU.mult, op1=ALU.add),
                 reads=[ops_t, cst], writes=[q_t])
            P.op("dve", lambda e: e.tensor_scalar(out=ebacc.ap, in0=cst.ap[:, 0:64], scalar1=oend_t.ap[:, 0:1], scalar2=None, op0=ALU.is_ge),
                 reads=[cst, oend_t], writes=[ebacc])
            for ee in range(1, NE):
                P.op("dve", lambda e, ee=ee: e.scalar_tensor_tensor(out=ebacc.ap, in0=cst.ap[:, 0:64], scalar=oend_t.ap[:, ee:ee + 1], in1=ebacc.ap,
                                                                  op0=ALU.is_ge, op1=ALU.add), reads=[cst, oend_t, ebacc], writes=[ebacc])
            P.op("dve", lambda e: e.scalar_tensor_tensor(out=idxg.ap, in0=ebacc.ap, scalar=256.0, in1=cst.ap[:, 64:128], op0=ALU.mult, op1=ALU.add),
                 reads=[ebacc, cst], writes=[idxg])
            P.op("dve", lambda e: e.scalar_tensor_tensor(out=idxd.ap, in0=ebacc.ap, scalar=256.0, in1=cst.ap[:, 128:192], op0=ALU.mult, op1=ALU.add),
                 reads=[ebacc, cst], writes=[idxd])
            if _DBG == 5:
                P.op("dve", lambda e: e.tensor_copy(out=idxg.ap, in_=cst.ap[:, 64:128]), reads=[cst], writes=[idxg])
                P.op("dve", lambda e: e.tensor_copy(out=idxd.ap, in_=cst.ap[:, 128:192]), reads=[cst], writes=[idxd])
            for t in range(NT):
                tmp = small()
                sel = small()
                P.op("dve", lambda e, sel=sel, t=t: e.scalar_tensor_tensor(out=sel.ap[:, 0:32], in0=RK[t].ap, scalar=256.0, in1=q_t.ap, op0=ALU.is_ge, op1=ALU.mult),
                     reads=[RK[t], q_t], writes=[sel])
                P.op("dve", lambda e, tmp=tmp, t=t: e.tensor_tensor(out=tmp.ap[:, 0:32], in0=RK[t].ap, in1=cst.ap[:, 320:352], op=ALU.add),
                     reads=[RK[t], cst], writes=[tmp])
                P.op("dve", lambda e, tmp=tmp, sel=sel: e.tensor_tensor(out=tmp.ap[:, 0:32], in0=tmp.ap[:, 0:32], in1=sel.ap[:, 0:32], op=ALU.add),
                     reads=[tmp, sel], writes=[tmp])
                for a_, oh in enumerate((OH1[t], OH2[t])):
                    m_ = small()
                    P.op("dve", lambda e, m_=m_, tmp=tmp, oh=oh: e.tensor_tensor(out=m_.ap[:, 0:32], in0=tmp.ap[:, 0:32], in1=oh.ap, op=ALU.mult),
                         reads=[tmp, oh], writes=[m_])
                    P.op("dve", lambda e, m_=m_, t=t, a_=a_: e.tensor_reduce(out=DF[t].ap[:, a_:a_ + 1], in_=m_.ap[:, 0:32], axis=AX.X, op=ALU.add),
                         reads=[m_], writes=[DF[t]])
                P.op("dve", lambda e, t=t: e.tensor_copy(out=DI[t].ap, in_=DF[t].ap), reads=[DF[t]], writes=[DI[t]])
                P.op("dve", lambda e, t=t: e.tensor_scalar(out=DIS[t].ap, in0=DF[t].ap, scalar1=24576.0, scalar2=None, op0=ALU.add), reads=[DF[t]], writes=[DIS[t]])
            for k in range(8):
                P.op("pool", lambda e, k=k: e.tensor_scalar(out=GT.ap[:, k * 128:(k + 1) * 128], in0=onesb.ap, scalar1=ppc(pb + 168 + k), scalar2=None, op0=ALU.mult),
                     reads=[onesb, pp], writes=[GT])
            if _DBG == 6:
                dbt = scr[0]
                P.op("dve", lambda e: e.tensor_copy(out=dbt.ap[:, 0:32], in_=cntn.ap), reads=[cntn], writes=[dbt])
                P.op("dve", lambda e: e.tensor_copy(out=dbt.ap[:, 32:64], in_=nn_t.ap), reads=[nn_t], writes=[dbt])
                P.op("dve", lambda e: e.tensor_copy(out=dbt.ap[:, 64:96], in_=end_t.ap), reads=[end_t], writes=[dbt])
                P.op("dve", lambda e: e.tensor_copy(out=dbt.ap[:, 96:128], in_=psb_t.ap), reads=[psb_t], writes=[dbt])
                P.op("dve", lambda e: e.tensor_copy(out=dbt.ap[:, 128:192], in_=ebacc.ap), reads=[ebacc], writes=[dbt])
                P.op("dve", lambda e: e.tensor_copy(out=dbt.ap[:, 192:256], in_=idxg.ap), reads=[idxg], writes=[dbt])
                P.op("dve", lambda e: e.tensor_copy(out=dbt.ap[:, 256:258], in_=DF[0].ap), reads=[DF[0]], writes=[dbt])
                P.op("dve", lambda e: e.tensor_copy(out=dbt.ap[:, 258:260], in_=DI[0].ap), reads=[DI[0]], writes=[dbt])
                P.op("dve", lambda e: e.tensor_copy(out=dbt.ap[:, 260:292], in_=RK[1].ap), reads=[RK[1]], writes=[dbt])
                P.op("dve", lambda e: e.tensor_copy(out=dbt.ap[:, 292:324], in_=OH1[0].ap), reads=[OH1[0]], writes=[dbt])
                P.op("sp", lambda e: e.dma_start(out=dbg_d, in_=dbt.ap), reads=[dbt], dkey="dbg")
                ple_prep(l)
                return
            if _DBG == 1:
                ple_prep(l)
                return
            sc_tiles = []
            for t in range(NT):
                ss = row_rstd(t, junk_moe)
                xs = xs_t[t % 2]
                P.op("dve", lambda e, t=t, ss=ss, xs=xs: e.tensor_scalar(out=xs.ap, in0=xrow(t), scalar1=ss.ap[:, 2:3], scalar2=None, op0=ALU.mult),
                     reads=[X[t][0], X[t][1], ss], writes=[xs])
                for a_ in range(2):
                    dt_ = dram_tiles[("sc", t, a_)]
                    P.op("pool", lambda e, t=t, a_=a_, xs=xs: e.indirect_dma_start(
                        out=sorted_d, out_offset=bass.IndirectOffsetOnAxis(ap=DIS[t].ap[:, a_:a_ + 1], axis=0),
                        in_=xs.ap, in_offset=None, bounds_check=P.regs['b36351'], oob_is_err=False),
                        reads=[xs, DIS[t]], writes=[dt_], dkey=f"sc{t % 2}")
                    sc_tiles.append(dt_)
            ple_prep(l)
            if _DBG == 2:
                return

            gser = dram_tiles[('gser',)]

            def eload(b_, mats):
                s_ = b_ % 2
                srcs = (weg_d[l], weu_d[l], wed_d[l])
                for m_ in mats:
                    src = srcs[m_]
                    for h_, idt in enumerate((idxg, idxd)):
                        P.op("pool", lambda e, m_=m_, src=src, h_=h_, idt=idt: e.indirect_dma_start(
                            out=ew[s_][m_].ap[:, h_ * 2048:(h_ + 1) * 2048], out_offset=None, in_=src[:, :],
                            in_offset=bass.IndirectOffsetOnAxis(ap=idt.ap[:, b_:b_ + 1], axis=0),
                            bounds_check=P.regs['b8191'], oob_is_err=False),
                            reads=[idt], writes=[ew[s_][m_]], dkey=f"ew{s_}_{m_}")

            ys_tiles = []

            def views(s_):
                return (ew[s_][0].ap.rearrange("p (k c) -> p k c", k=8), ew[s_][1].ap.rearrange("p (k c) -> p k c", k=8),
                        ew[s_][2].ap.rearrange("p (k c) -> p k c", k=4))

            def stA_load(b_):
                xg = xg_t[b_ % 4]
                P.op("sp", lambda e: e.dma_start(out=xg.ap, in_=sorted_d[24576 + b_ * 128:24576 + (b_ + 1) * 128, :]), reads=sc_tiles, writes=[xg], dkey=f"xgl{b_ % 4}")

            def stA(b_):
                x2 = b_ % 2
                xg = xg_t[b_ % 4]
                bt = bank()
                btv = bt.ap.bitcast(BF16)
                xgv = xg.ap.rearrange("p (a k) -> p a k", k=8)
                for k in range(8):
                    P.op("pe", lambda e, k=k: e.transpose(out=btv[:, k * 128:(k + 1) * 128], in_=xgv[:, :, k], identity=identb.ap),
                         reads=[xg, identb], writes=[bt])
                xb = xbT[x2]
                P.op("dve", lambda e: e.tensor_tensor(out=xb.ap, in0=btv[:, 0:1024], in1=GT.ap, op=ALU.mult), reads=[bt, GT], writes=[xb])

            def stB(b_, s_):
                x2 = b_ % 2
                vg, vu, vd = views(s_)
                xb = xbT[x2]
                bg = bank()
                mm_acc(bg.ap, bg, [(xb.ap[:, k * 128:(k + 1) * 128], vg[:, k, :]) for k in range(8)], [xb, ew[s_][0]])
                bu = bank()
                mm_acc(bu.ap, bu, [(xb.ap[:, k * 128:(k + 1) * 128], vu[:, k, :]) for k in range(8)], [xb, ew[s_][1]])
                sg = scratch()
                P.op("act", lambda e: e.activation(out=sg.ap, in_=bg.ap, func=AF.Silu), reads=[bg], writes=[sg])
                hb_ = hbt[x2]
                P.op("dve", lambda e: e.tensor_tensor(out=hb_.ap, in0=bu.ap, in1=sg.ap, op=ALU.mult), reads=[bu, sg], writes=[hb_])

            def stT(b_):
                x2 = b_ % 2
                hb_ = hbt[x2]
                bh = bank()
                bhv = bh.ap.bitcast(BF16)
                for f in range(4):
                    P.op("pe", lambda e, f=f: e.transpose(out=bhv[:, f * 128:(f + 1) * 128], in_=hb_.ap[:, f * 128:(f + 1) * 128], identity=identb.ap),
                         reads=[hb_, identb], writes=[bh])
                hT_ = hbT[x2]
                P.op("act", lambda e: e.activation(out=hT_.ap, in_=bhv[:, 0:512], func=AF.Copy), reads=[bh], writes=[hT_])

            def stC(b_, s_):
                x2 = b_ % 2
                vg, vu, vd = views(s_)
                hT_ = hbT[x2]
                yo = yst[b_ % 4]
                for hh in range(2):
                    bd = bank()
                    mm_acc(bd.ap, bd, [(hT_.ap[:, f * 128:(f + 1) * 128], vd[:, f, hh * 512:(hh + 1) * 512]) for f in range(4)], [hT_, ew[s_][2]])
                    if hh == 0:
                        P.op("act", lambda e, bd=bd: e.activation(out=yo.ap[:, 0:512], in_=bd.ap, func=AF.Copy), reads=[bd], writes=[yo])
                    else:
                        P.op("dve", lambda e, bd=bd: e.tensor_copy(out=yo.ap[:, 512:1024], in_=bd.ap), reads=[bd], writes=[yo])
                yt_ = dram_tiles[("ys", b_)]
                P.op("sp", lambda e: e.dma_start(out=ys_d[b_ * 128:(b_ + 1) * 128, :], in_=yo.ap), reads=[yo], writes=[yt_], dkey=f"yst{b_ % 4}")
                ys_tiles.append(yt_)

            def sload(e_, mats):
                s_ = e_ % 2
                srcs = (weg_d[l], weu_d[l], wed_d[l])
                for m_ in mats:
                    src = srcs[m_]
                    P.op("pool", lambda e, m_=m_, src=src: e.dma_start(
                        out=ew[s_][m_].ap.rearrange("p (h c) -> p h c", h=2), in_=src[256 * e_:256 * (e_ + 1), :].rearrange("(p h) c -> p h c", h=2)),
                        writes=[ew[s_][m_]], dkey=f"ew{s_}_{m_}")

            NOVF = 28
            NB_ALL = 64 + NOVF

            def slot_of(b_):
                return (b_ // 2) % 2 if b_ < 64 else (b_ - 64) % 2

            for i in range(-6, NB_ALL):
                for e_ in range(NE):
                    if i == 2 * e_ - 4:
                        sload(e_, (0, 1))
                    if i == 2 * e_ - 2:
                        sload(e_, (2,))
                for o_ in range(NOVF):
                    b_ = 64 + o_
                    if i == b_ - 3:
                        eload(o_, (0, 1))
                    if i == b_ - 1:
                        eload(o_, (2,))
                if 0 <= i + 6 < NB_ALL:
                    stA_load(i + 6)
                if 0 <= i + 3 < NB_ALL:
                    stA(i + 3)
                if 0 <= i + 2 < NB_ALL:
                    stB(i + 2, slot_of(i + 2))
                if 0 <= i + 1 < NB_ALL:
                    stT(i + 1)
                if 0 <= i < NB_ALL:
                    stC(i, slot_of(i))
            if _DBG in (3, 4, 5):
                return
            cbufs = [ycmb[0], ycmb[1], yst[0], yst[1], yst[2], yst[3]]
            for t in range(NT):
                for a_ in range(2):
                    ci = (2 * t + a_) % 6
                    yc = cbufs[ci]
                    P.op("pool", lambda e, t=t, a_=a_, yc=yc: e.indirect_dma_start(
                        out=yc.ap, out_offset=None, in_=ys_d[:, :], in_offset=bass.IndirectOffsetOnAxis(ap=DI[t].ap[:, a_:a_ + 1], axis=0),
                        bounds_check=P.regs['b11775'], oob_is_err=False),
                        reads=ys_tiles + [DI[t]], writes=[yc], dkey=f"yc{ci}")
                    for hh in range(2):
                        xt = X[t][hh]
                        P.op("dve", lambda e, t=t, a_=a_, yc=yc, hh=hh, xt=xt: e.scalar_tensor_tensor(
                            out=xt.ap, in0=yc.ap[:, hh * 512:(hh + 1) * 512], scalar=G12[t].ap[:, a_:a_ + 1], in1=xt.ap, op0=ALU.mult, op1=ALU.add),
                            reads=[yc, G12[t], xt], writes=[xt])

        def ple_prep(l):
            P.op("pool", lambda e: e.dma_start(out=wpe.ap.rearrange("p (k c) -> p k c", k=2), in_=wpe_d[l].rearrange("(k p) c -> p k c", p=128)),
                 writes=[wpe], dkey="wpe")
            for t in range(NT):
                stt = pst[t % 2]
                P.op("sp", lambda e, t=t, stt=stt: e.dma_start(out=stt.ap, in_=p_d[l, t * 128:(t + 1) * 128, :]), writes=[stt], dkey=f"pst{t % 2}")
                b = bank()
                for kc in range(2):
                    P.op("pe", lambda e, b=b, kc=kc, stt=stt: e.transpose(out=b.ap[:, kc * 128:(kc + 1) * 128], in_=stt.ap[:, kc * 128:(kc + 1) * 128], identity=ident.ap),
                         reads=[stt, ident], writes=[b])
                for kc in range(2):
                    P.op("act", lambda e, b=b, kc=kc, t=t: e.activation(out=pT[kc].ap[:, t * 128:(t + 1) * 128], in_=b.ap[:, kc * 128:(kc + 1) * 128], func=AF.Copy),
                         reads=[b], writes=[pT[kc]])

        def ple_phase(l):
            pb = l * PPL
            wpgl = wpg_d[l].rearrange("(k p) c -> p k c", p=128)
            for i in range(2):
                P.op("pool", lambda e, i=i: e.dma_start(out=wpg[i].ap.rearrange("p (k c) -> p k c", k=8), in_=wpgl[:, :, i * 512:(i + 1) * 512]),
                     writes=[wpg[i]], dkey="wp")
            wpev = wpe.ap.rearrange("p (k c) -> p k c", k=2)

            def ple_main(g_):
                for t in range(4 * g_, 4 * g_ + 4):
                    ple_tile(t)

            def ple_tile(t):
                g, i = t // 4, t % 4
                for hh in range(2):
                    wv = wpg[hh].ap.rearrange("p (k c) -> p k c", k=8)
                    bg = bank()
                    mm_acc(bg.ap, bg, [(h2T[k][g].ap[:, i * 128:(i + 1) * 128], wv[:, k, :]) for k in range(8)], [wpg[hh]] + [h2T[k][g] for k in range(8)])
                    be = bank()
                    mm_acc(be.ap, be, [(pT[kc].ap[:, t * 128:(t + 1) * 128], wpev[:, kc, hh * 512:(hh + 1) * 512]) for kc in range(2)], [wpe] + pT)
                    sg = scratch()
                    P.op("act", lambda e, sg=sg, bg=bg: e.activation(out=sg.ap, in_=bg.ap, func=AF.Sigmoid), reads=[bg], writes=[sg])
                    P.op("dve", lambda e, sg=sg, be=be: e.tensor_tensor(out=sg.ap, in0=be.ap, in1=sg.ap, op=ALU.mult), reads=[be, sg], writes=[sg])
                    xt = X[t][hh]
                    P.op("pool", lambda e, sg=sg, xt=xt: e.tensor_tensor(out=xt.ap, in0=xt.ap, in1=sg.ap, op=ALU.add), reads=[sg, xt], writes=[xt])

            norm_T(0, pb + 16, [h2T[c][0] for c in range(8)], xnb2)
            for g in range(4):
                if g + 1 < 4:
                    norm_T(g + 1, pb + 16, [h2T[c][g + 1] for c in range(8)], xnb2)
                ple_main(g)

        def final_phase():
            P.op("sp", lambda e: e.dma_start(out=nfin.ap, in_=bc_d[:, 72:72 + D]), writes=[nfin], dkey="nf")
            for t in range(NT):
                ss = row_rstd(t, junk_moe)
                o = ost[t % 2]
                P.op("dve", lambda e, t=t, ss=ss, o=o: e.scalar_tensor_tensor(out=o.ap, in0=xrow(t), scalar=ss.ap[:, 2:3], in1=nfin.ap, op0=ALU.mult, op1=ALU.mult),
                     reads=[X[t][0], X[t][1], ss, nfin], writes=[o])
                P.op("sp", lambda e, t=t, o=o: e.dma_start(out=out_d[t * 128:(t + 1) * 128, :], in_=o.ap), reads=[o], dkey=f"ost{t % 2}")
            for i in range(2):
                P.op("sp", lambda e: e.nop(), writes=[ost[i]])

        for l in range(nl):
            for g in range(4):
                tasks = mixer_group(l, g)
                loaded = {}
                n = len(tasks)
                nxt_load = 0
                in_use = 0
                for ti in range(n):
                    while nxt_load < n and (nxt_load <= ti or in_use + len(tasks[nxt_load][0]) <= 6):
                        loaded[nxt_load] = [wload(srcs) for srcs in tasks[nxt_load][0]]
                        in_use += len(tasks[nxt_load][0])
                        nxt_load += 1
                    tasks[ti][1](loaded.pop(ti))
                    in_use -= len(tasks[ti][0])
            nbank[0] = 8
            moe_phase(l)
            ple_phase(l)
            nbank[0] = 6
        final_phase()
        P.emit()
        nops = {k: len(v) for k, v in P.ops.items()}
        print("ops per engine:", nops)
    return nc


def _host_layout(inp):
    f = np.float32
    w_in = np.asarray(inp["w_in"], f)
    swap = np.arange(512).reshape(8, 64)
    swap = np.concatenate([swap[:, 32:], swap[:, :32]], axis=1).reshape(-1)
    q, k, v = w_in[:, :, 0:512], w_in[:, :, 512:1024], w_in[:, :, 1024:1536]
    rest = w_in[:, :, 1536:]
    win = np.ascontiguousarray(np.concatenate([q, q[:, :, swap], k, k[:, :, swap], v, rest], axis=2))
    assert win.shape[2] == WIN_EXT
    wr = np.ascontiguousarray(np.concatenate([np.asarray(inp["w_route_group"], f), np.asarray(inp["w_route_expert"], f)], axis=2))
    pp = np.zeros((128, L * PPL), f)

    def cols(vec, n):
        return np.asarray(vec, f).reshape(n, 128).T
    for l in range(L):
        b = l * PPL
        pp[:, b + 0:b + 8] = cols(inp["norm_mix"][l], 8)
        pp[:, b + 8:b + 16] = cols(inp["norm_ffn"][l], 8)
        pp[:, b + 168:b + 176] = np.asarray(inp["norm_ffn"][l], f).reshape(128, 8)
        pp[:, b + 16:b + 24] = cols(inp["norm_ple"][l], 8)
        pp[:, b + 24:b + 32] = cols(inp["b_conv_out"][l], 8)
        pp[:, b + 32:b + 36] = cols(inp["b_dw"][l], 4)
        pp[:, b + 36:b + 40] = cols(inp["ln_conv_g"][l], 4)
        pp[:, b + 40:b + 44] = cols(inp["ln_conv_b"][l], 4)
        wd = np.asarray(inp["w_dw"][l], f)
        for j in range(31):
            pp[:, b + 44 + 4 * j:b + 48 + 4 * j] = cols(wd[j], 4)
    bc = np.zeros((128, 72 + D), f)
    for l in range(L):
        bc[:, l * 36:l * 36 + 4] = np.asarray(inp["b_route_group"][l], f)[None, :]
        bc[:, l * 36 + 4:l * 36 + 36] = np.asarray(inp["b_route_expert"][l], f)[None, :]
    bc[:, 72:] = np.asarray(inp["norm_final"], f)[None, :]
    inv = np.power(f(10000.0), -np.arange(0, 64, 2, dtype=f) / f(64)).astype(f)
    ang = (np.arange(S, dtype=f)[:, None] * inv[None, :]).astype(f)
    cs, sn = np.cos(ang).astype(f).T, np.sin(ang).astype(f).T
    cosT = np.concatenate([cs, cs, cs, cs], axis=0)
    sinT = np.concatenate([-sn, sn, -sn, sn], axis=0)
    kk = np.arange(128)[:, None]
    col = np.arange(S)[None, :]
    dl = col - kk
    cnt = ((dl >= 0) & (dl <= 128)).astype(f) + ((dl >= 0) & (dl <= 512) & (dl % 4 == 0)).astype(f) \
        + ((dl >= 0) & (dl <= 2048) & (dl % 16 == 0)).astype(f)
    shared = {
        "win": win, "wao": np.ascontiguousarray(inp["w_attn_out"], f), "wco": np.ascontiguousarray(inp["w_conv_out"], f),
        "wout": np.ascontiguousarray(inp["w_out"], f), "wr": wr,
        "wpg": np.ascontiguousarray(inp["w_ple_gate"], f), "wpe": np.ascontiguousarray(inp["w_ple_proj"], f),
        "pp": pp, "bc": bc, "cosT": np.ascontiguousarray(cosT), "sinT": np.ascontiguousarray(sinT),
        "maskT": np.ascontiguousarray(cnt), "ident": np.eye(128, dtype=f),
    }
    for l in range(L):
        shared[f"weg{l}"] = np.ascontiguousarray(inp["w_exp_gate"][l], f).reshape(8192, 2048)
        shared[f"weu{l}"] = np.ascontiguousarray(inp["w_exp_up"][l], f).reshape(8192, 2048)
        shared[f"wed{l}"] = np.ascontiguousarray(
            np.asarray(inp["w_exp_down"][l], f).reshape(NE, 4, 128, D).transpose(0, 2, 1, 3)).reshape(8192, 2048)
    cst = np.zeros((128, 384), f)
    cst[:, 320:352] = (256 * np.arange(32, dtype=f))[None, :]
    cst[:, 352:384] = (7936 - 256 * np.arange(32, dtype=f))[None, :]
    cst[:, 0:64] = np.arange(64, dtype=f)[None, :]
    cst[:, 64:128] = (2 * np.arange(128, dtype=f))[:, None]
    cst[:, 128:192] = (2 * np.arange(128, dtype=f) + 1)[:, None]
    cst[:, 192:320] = (np.arange(128)[:, None] < np.arange(128)[None, :]).astype(f)
    shared["cst"] = cst
    return shared


_NL = L
_DBG = 0


def kernel(**inputs):
    shared = _host_layout(inputs)
    x = np.asarray(inputs["x"], np.float32)
    p = np.asarray(inputs["p"], np.float32)
    nc = build_program(_NL)
    in_maps = []
    for b in range(8):
        m = dict(shared)
        m["x"] = np.ascontiguousarray(x[b])
        m["p"] = np.ascontiguousarray(p[:, b])
        in_maps.append(m)
    res = run_bass_kernel_spmd(nc, in_maps, core_ids=list(range(8)))
    return np.stack([r["out"] for r in res.results], axis=0).astype(np.float32)
```

```python
import contextlib
import numpy as np
import concourse.bass as bass
import concourse.mybir as mybir
from concourse.bass_utils import run_bass_kernel_spmd

F32 = mybir.dt.float32
BF16 = mybir.dt.bfloat16
ALU = mybir.AluOpType
AF = mybir.ActivationFunctionType
AX = mybir.AxisListType

S = 2048
D = 1024
L = 2
NT = 16
NE = 32
WIN_EXT = 5632
PPL = 176
EPS = 1e-6
BIG = 1.0e30

STRICT_SAME = True


class T:
    __slots__ = ("ap", "space", "lo", "hi", "w", "r", "ov", "name")

    def __init__(self, ap, space, lo, hi, name=""):
        self.ap, self.space, self.lo, self.hi, self.name = ap, space, lo, hi, name
        self.w = None
        self.r = {}
        self.ov = [self]


class Op:
    __slots__ = ("eng", "fn", "deps", "ddeps", "sig", "count", "dkey", "dval", "is_write")

    def __init__(self, eng, fn):
        self.eng, self.fn = eng, fn
        self.deps = []
        self.ddeps = {}
        self.sig = False
        self.count = None
        self.dkey = None
        self.dval = None
        self.is_write = False


class Prog:
    def __init__(self, nc):
        self.nc = nc
        self.ops = {e: [] for e in ("pe", "act", "dve", "pool", "sp")}
        self.tiles = {"sb": [], "ps": []}
        self.dma_counts = {}
        self.reg_init = {}
        self.regs = {}

    def tile(self, ap, space, lo, hi, name=""):
        t = T(ap, space, lo, hi, name)
        for o in self.tiles[space]:
            if o.lo < hi and lo < o.hi:
                o.ov.append(t)
                t.ov.append(o)
        self.tiles[space].append(t)
        return t

    def _dep_on(self, op, prod, raw=True):
        if prod is None or prod is op:
            return
        if prod.dkey is not None:
            k = prod.dkey
            if not raw and op.dkey == k and prod.dval is not None and prod.is_write:
                return
            v = self.dma_counts[k]
            if op.ddeps.get(k, 0) < v:
                op.ddeps[k] = v
            return
        if prod.eng == op.eng and op.dkey is None and (op.eng == "pe" or not STRICT_SAME):
            return
        prod.sig = True
        op.deps.append(prod)

    def op(self, eng, fn, reads=(), writes=(), dkey=None):
        o = Op(eng, fn)
        o.dkey = dkey
        o.is_write = len(writes) > 0
        for t in reads:
            for u in t.ov:
                self._dep_on(o, u.w)
        for t in writes:
            for u in t.ov:
                self._dep_on(o, u.w, raw=False)
                for rd in u.r.values():
                    self._dep_on(o, rd)
        if dkey is not None:
            self.dma_counts[dkey] = self.dma_counts.get(dkey, 0) + 16
            o.dval = self.dma_counts[dkey]
        stream = eng if dkey is None else ("dma", id(o))
        for t in reads:
            t.r[stream] = o
        for t in writes:
            t.w = o
            t.r = {}
        self.ops[eng].append(o)
        return o

    def emit(self):
        nc = self.nc
        with contextlib.ExitStack() as st:
            sems = {e: st.enter_context(nc.semaphore("s_" + e)) for e in ("pe", "act", "dve", "pool")}
            dsems = {k: st.enter_context(nc.semaphore("d_" + str(k))) for k in self.dma_counts}
            for e, lst in self.ops.items():
                c = 0
                for o in lst:
                    if o.dkey is None and o.sig:
                        c += 1
                        o.count = c
            block = st.enter_context(nc.Block())

            def run(engname, engobj):
                waited = {}
                if engname == "pool":
                    for nm, val in self.reg_init.items():
                        r = engobj.alloc_register("bnd_" + nm)
                        engobj.reg_mov(r, val)
                        self.regs[nm] = r
                for o in self.ops[engname]:
                    best = {}
                    for p in o.deps:
                        if best.get(p.eng, 0) < p.count:
                            best[p.eng] = p.count
                    for pe_, cnt in best.items():
                        if waited.get(("c", pe_), 0) < cnt:
                            engobj.wait_ge(sems[pe_], cnt)
                            waited[("c", pe_)] = cnt
                    for k, v in o.ddeps.items():
                        if waited.get(("d", k), 0) < v:
                            engobj.wait_ge(dsems[k], v)
                            waited[("d", k)] = v
                    ins = o.fn(engobj)
                    if o.dkey is not None:
                        ins.then_inc(dsems[o.dkey], 16)
                    elif o.sig:
                        ins.then_inc(sems[engname], 1)

            @block.sync
            def _(e):
                run("sp", e)

            @block.tensor
            def _(e):
                run("pe", e)

            @block.scalar
            def _(e):
                run("act", e)

            @block.vector
            def _(e):
                run("dve", e)

            @block.gpsimd
            def _(e):
                run("pool", e)


def build_program(nl=L):
    nc = bass.Bass("TRN2", target_bir_lowering=False)

    def din(name, shape):
        return nc.dram_tensor(name, list(shape), F32, kind="ExternalInput").ap()

    x_d = din("x", [S, D])
    p_d = din("p", [L, S, 256])
    win_d = din("win", [L, D, WIN_EXT])
    wao_d = din("wao", [L, 512, D])
    wco_d = din("wco", [L, 512, D])
    wout_d = din("wout", [L, D, D])
    wr_d = din("wr", [L, D, 36])
    weg_d = [nc.dram_tensor(f"weg{l}", [8192, 2048], F32, kind="ExternalInput") for l in range(L)]
    weu_d = [nc.dram_tensor(f"weu{l}", [8192, 2048], F32, kind="ExternalInput") for l in range(L)]
    wed_d = [nc.dram_tensor(f"wed{l}", [8192, 2048], F32, kind="ExternalInput") for l in range(L)]
    cst_d = din("cst", [128, 384])
    scr_d = nc.dram_tensor("moe_scr", [18432, D], F32)
    ys_d = scr_d
    sorted_d = scr_d[:, :].bitcast(BF16).rearrange("r (two c) -> (r two) c", two=2)
    wpg_d = din("wpg", [L, D, D])
    wpe_d = din("wpe", [L, 256, D])
    pp_d = din("pp", [128, L * PPL])
    bc_d = din("bc", [128, 72 + D])
    cos_d = din("cosT", [128, S])
    sin_d = din("sinT", [128, S])
    mask_d = din("maskT", [128, S])
    id_d = din("ident", [128, 128])
    out_d = nc.dram_tensor("out", [S, D], F32, kind="ExternalOutput").ap()
    dbg_d = nc.dram_tensor("dbg", [128, 512], F32, kind="ExternalOutput").ap() if _DBG == 6 else None

    NB = 212800
    with contextlib.ExitStack() as st:
        big = st.enter_context(nc.sbuf_tensor("arena", [128, NB // 4], F32))
        pbanks = [st.enter_context(nc.psum_tensor(f"pb{i}", [128, 512], F32)) for i in range(8)]
        P = Prog(nc)
        P.reg_init = {'b8191': 8191, 'b11775': 11775, 'b36351': 24576 + 11775}
        top = [0]

        def alloc(nbytes, at=None):
            nbytes = (nbytes + 31) // 32 * 32
            if at is None:
                at = top[0]
                top[0] = at + nbytes
            assert at + nbytes <= NB, ("SBUF overflow", at, nbytes)
            return at, at + nbytes

        def mk(n, dtype, name, at=None):
            es = 2 if dtype == BF16 else 4
            lo, hi = alloc(n * es, at)
            ap = big[:, lo // 4:hi // 4]
            if dtype != F32:
                ap = ap.bitcast(dtype)
            return P.tile(ap[:, 0:n], "sb", lo, hi, name)

        ps = [P.tile(pbanks[i][:, :], "ps", i * 2048, (i + 1) * 2048, f"ps{i}") for i in range(8)]
        rr = {"b": 0, "s": 0, "m": 0}

        nbank = [6]

        def bank():
            rr["b"] = (rr["b"] + 1) % nbank[0]
            return ps[rr["b"]]

        X = [[mk(512, F32, f"x{t}_{h}") for h in range(2)] for t in range(NT)]
        x0 = X[0][0].lo

        def xrow(t):
            lo = x0 + t * 4096
            return big[:, lo // 4: lo // 4 + 1024]
        ident = mk(128, F32, "ident")
        identb = mk(128, BF16, "identb")
        ones512 = mk(128, F32, "ones512")
        maskT = mk(S, BF16, "mask")
        pp = mk(L * PPL, F32, "pp")
        bcp = mk(72, F32, "bc")
        scr = [mk(512, F32, f"scr{i}") for i in range(5)]
        smalls = [mk(36, F32, f"sm{i}") for i in range(18)]
        PH0 = top[0]

        def scratch():
            rr["s"] = (rr["s"] + 1) % 5
            return scr[rr["s"]]

        def small():
            rr["m"] = (rr["m"] + 1) % 18
            return smalls[rr["m"]]

        top[0] = PH0
        kT = [[mk(512, BF16, f"kT{c}_{g}") for g in range(4)] for c in range(4)]
        vA = [mk(520, BF16, f"vA{t}") for t in range(NT)]
        hT = [mk(512, BF16, f"hT{c}") for c in range(8)]
        ring = [mk(2048, BF16, f"ring{i}") for i in range(6)]
        qT = [mk(512, BF16, f"qT{c}") for c in range(4)]
        cos2 = [mk(512, F32, f"cosg{i}") for i in range(2)]
        sin2 = [mk(512, F32, f"sing{i}") for i in range(2)]
        U = [mk(542, BF16, f"u{c}") for c in range(4)]
        dgs = [mk(128, BF16, f"dg{j}") for j in range(31)]
        junk_mix = mk(1024, BF16, "junk_mix", at=dgs[0].lo)
        Y = [mk(512, F32, f"y{c}") for c in range(4)]
        sT = [mk(512, BF16, f"sT{c}") for c in range(4)]
        pexp = [mk(512, BF16, f"pexp{i}") for i in range(3)]
        pmsk = [mk(512, BF16, f"pmsk{i}") for i in range(3)]
        otok = [mk(512, BF16, f"otok{j}") for j in range(4)]
        oT = [mk(512, BF16, f"oT{c}") for c in range(4)]
        mg = [mk(512, BF16, f"mg{c}") for c in range(8)]
        xnb = [mk(1024, BF16, f"xnb{i}", at=otok[0].lo + i * 2048) for i in range(4)]
        MIX_END = top[0]

        top[0] = PH0
        h2T = [[mk(512, BF16, f"h2T{c}_{g}") for g in range(4)] for c in range(8)]
        H2LO = h2T[0][0].lo
        xnb2 = [mk(1024, BF16, f"xnb2_{i}") for i in range(4)]
        XNLO = xnb2[0].lo
        junk_moe = mk(1024, BF16, "junk_moe")
        wrt = mk(8 * 36, BF16, "wrt")
        OH1 = [mk(32, F32, f"oh1_{t}") for t in range(NT)]
        OH2 = [mk(32, F32, f"oh2_{t}") for t in range(NT)]
        ABF = [mk(32, BF16, f"abf{t}") for t in range(NT)]
        RK = [mk(32, F32, f"rk{t}") for t in range(NT)]
        G12 = [mk(2, F32, f"g12_{t}") for t in range(NT)]
        DF = [mk(2, F32, f"df{t}") for t in range(NT)]
        DI = [mk(2, mybir.dt.int32, f"di{t}") for t in range(NT)]
        DIS = [mk(2, mybir.dt.int32, f"dis{t}") for t in range(NT)]
        cst = mk(384, F32, "cst")
        np_t = mk(32, F32, "np_t")
        oend_t = mk(32, F32, "oend")
        ops_t = mk(32, F32, "ops")
        q_t = mk(32, F32, "q_t")
        ltri = mk(128, BF16, "ltri")
        onesb = mk(128, BF16, "onesb")
        cntn = mk(32, F32, "cntn")
        nn_t = mk(32, F32, "nn")
        end_t = mk(32, F32, "end")
        psb_t = mk(32, F32, "psb")
        ebacc = mk(64, F32, "ebacc")
        idxg = mk(64, mybir.dt.int32, "idxg")
        idxd = mk(64, mybir.dt.int32, "idxd")
        GT = mk(1024, BF16, "GT")
        xs_t = [mk(1024, BF16, f"xs{i}") for i in range(2)]
        xg_t = [mk(1024, BF16, f"xg{i}") for i in range(4)]
        xbT = [mk(1024, BF16, f"xbT{i}") for i in range(2)]
        hbt = [mk(512, BF16, f"hbt{i}") for i in range(2)]
        hbT = [mk(512, BF16, f"hbT{i}") for i in range(2)]
        yst = [mk(1024, F32, f"yst{i}") for i in range(2)]
        yst.append(mk(1024, F32, "yst2", at=xs_t[0].lo))
        yst.append(mk(1024, F32, "yst3", at=OH1[0].lo))
        PH1 = top[0]
        ew = [[mk(4096, BF16, f"ew{s}_{m}", at=(H2LO + m * 8192) if s == 0 else None) for m in range(3)] for s in range(2)]
        ycmb = [mk(1024, F32, f"ycmb{i}", at=XNLO + i * 4096) for i in range(2)]
        wpe = mk(2048, BF16, "wpe")
        pT = [mk(S, BF16, f"pT{i}") for i in range(2)]
        pst = [mk(256, F32, f"pst{i}") for i in range(2)]
        MOE_END = top[0]
        top[0] = PH1
        wpg = [mk(4096, BF16, f"wpg{i}") for i in range(2)]
        ost = [mk(1024, F32, f"ost{i}") for i in range(2)]
        nfin = mk(D, F32, "nfin")
        PLE_END = top[0]
        print("SBUF bytes/partition: mixer", MIX_END, "moe", MOE_END, "ple", PLE_END, "limit", NB)

        dram_tiles = {}
        _dn = [0]
        for t_ in range(NT):
            for a_ in range(2):
                _dn[0] += 1
                dram_tiles[("sc", t_, a_)] = P.tile(None, "sb", 10 ** 9 + 10 * _dn[0], 10 ** 9 + 10 * _dn[0] + 1, "scd")
        for b_ in range(92):
            _dn[0] += 1
            dram_tiles[("ys", b_)] = P.tile(None, "sb", 10 ** 9 + 10 * _dn[0], 10 ** 9 + 10 * _dn[0] + 1, "ysd")

        dram_tiles[('gser',)] = P.tile(None, 'sb', 2 * 10 ** 9, 2 * 10 ** 9 + 1, 'gser')
        P.op("sp", lambda e: e.dma_start(out=ident.ap, in_=id_d), writes=[ident], dkey="c0")
        P.op("sp", lambda e: e.dma_start(out=pp.ap, in_=pp_d), writes=[pp], dkey="c0")
        P.op("sp", lambda e: e.dma_start(out=bcp.ap, in_=bc_d[:, 0:72]), writes=[bcp], dkey="c0")
        P.op("pool", lambda e: e.dma_start(out=maskT.ap, in_=mask_d), writes=[maskT], dkey="c1")
        P.op("pool", lambda e: e.dma_start(out=identb.ap, in_=id_d), writes=[identb], dkey="c1")
        P.op("pool", lambda e: e.memset(ones512.ap, 1.0 / 512), writes=[ones512])
        for g in range(4):
            for i in range(4):
                t = 4 * g + i
                for h in range(2):
                    P.op("sp", lambda e, t=t, h=h: e.dma_start(out=X[t][h].ap, in_=x_d[t * 128:(t + 1) * 128, h * 512:(h + 1) * 512]),
                         writes=[X[t][h]], dkey=f"xg{g}")

        def ppc(col):
            return pp.ap[:, col:col + 1]

        def row_rstd(t, junk):
            ss = small()
            P.op("act", lambda e: e.activation(out=junk.ap, in_=xrow(t), func=AF.Square, accum_out=ss.ap[:, 0:1]),
                 reads=[X[t][0], X[t][1]], writes=[ss, junk])
            P.op("act", lambda e: e.activation(out=ss.ap[:, 1:2], in_=ss.ap[:, 0:1], func=AF.Sqrt, bias=EPS, scale=1.0 / D),
                 reads=[ss], writes=[ss])
            P.op("dve", lambda e: e.reciprocal(out=ss.ap[:, 2:3], in_=ss.ap[:, 1:2]), reads=[ss], writes=[ss])
            return ss

        def norm_T(g, gcol, dst, xn_tiles):
            for i in range(4):
                t = 4 * g + i
                ss = row_rstd(t, junk_mix if xn_tiles is xnb else junk_moe)
                P.op("dve", lambda e, t=t, i=i, ss=ss: e.tensor_scalar(out=xn_tiles[i].ap, in0=xrow(t), scalar1=ss.ap[:, 2:3],
                                                                       scalar2=None, op0=ALU.mult),
                     reads=[X[t][0], X[t][1], ss], writes=[xn_tiles[i]])
            for c in range(8):
                b = bank()
                bv = b.ap.bitcast(BF16)
                for i in range(4):
                    P.op("pe", lambda e, i=i, c=c, bv=bv: e.transpose(out=bv[:, i * 128:(i + 1) * 128],
                                                                      in_=xn_tiles[i].ap[:, c * 128:(c + 1) * 128], identity=identb.ap),
                         reads=[xn_tiles[i], identb], writes=[b])
                P.op("act", lambda e, c=c, bv=bv: e.activation(out=dst[c].ap, in_=bv[:, 0:512], func=AF.Copy, scale=ppc(gcol + c)),
                     reads=[b, pp], writes=[dst[c]])

        def mm_acc(out_ap, out_t, pairs, extra_reads):
            n = len(pairs)
            for k, (lh, rh) in enumerate(pairs):
                P.op("pe", lambda e, lh=lh, rh=rh, k=k: e.matmul(out_ap, lhsT=lh, rhs=rh, start=(k == 0), stop=(k == n - 1)),
                     reads=extra_reads, writes=[out_t])

        ring_i = [0]

        def wload(srcs, key_reads=()):
            ring_i[0] = (ring_i[0] + 1) % 6
            slot = ring[ring_i[0]]
            si = ring_i[0]
            views = []
            off = 0
            for (src, k, c) in srcs:
                v = slot.ap[:, off:off + k * c].rearrange("p (k c) -> p k c", k=k)
                views.append(v)
                P.op("pool", lambda e, v=v, src=src: e.dma_start(out=v, in_=src), writes=[slot], dkey=f"ring{si}")
                off += k * c
            return slot, views

        def mixer_group(l, g):
            pb = l * PPL
            norm_T(g, pb + 0, hT, xnb)
            gi = l * 4 + g
            cosg, sing = cos2[gi % 2], sin2[gi % 2]

            def cs_load(gi_):
                g_ = gi_ % 4
                P.op("sp", lambda e: e.dma_start(out=cos2[gi_ % 2].ap, in_=cos_d[:, g_ * 512:(g_ + 1) * 512]), writes=[cos2[gi_ % 2]], dkey=f"cs{gi_ % 2}")
                P.op("sp", lambda e: e.dma_start(out=sin2[gi_ % 2].ap, in_=sin_d[:, g_ * 512:(g_ + 1) * 512]), writes=[sin2[gi_ % 2]], dkey=f"cs{gi_ % 2}")
            if g == 0:
                cs_load(gi)
            if g < 3:
                cs_load(gi + 1)
            winl = win_d[l].rearrange("(k p) c -> p k c", p=128)
            waol = wao_d[l].rearrange("(k p) c -> p k c", p=128)
            wcol = wco_d[l].rearrange("(k p) c -> p k c", p=128)
            woutl = wout_d[l].rearrange("(k p) c -> p k c", p=128)

            def win_blk(c0):
                return [(winl[:, :, c0:c0 + 256], 8, 256)]

            tasks = []

            def rope_task(base, dst_fn):
                for b in range(2):
                    def comp(slots, b=b):
                        (s1, v1), (s2, v2) = slots
                        for cc in range(2):
                            c = 2 * b + cc
                            bq = bank()
                            mm_acc(bq.ap, bq, [(v1[0][:, k, cc * 128:(cc + 1) * 128], hT[k].ap) for k in range(8)], [s1] + hT)
                            bs = bank()
                            mm_acc(bs.ap, bs, [(v2[0][:, k, cc * 128:(cc + 1) * 128], hT[k].ap) for k in range(8)], [s2] + hT)
                            t1 = scratch()
                            P.op("dve", lambda e, t1=t1, bq=bq: e.tensor_tensor(out=t1.ap, in0=bq.ap, in1=cosg.ap, op=ALU.mult),
                                 reads=[bq, cosg], writes=[t1])
                            t2 = scratch()
                            P.op("dve", lambda e, t2=t2, bs=bs: e.tensor_tensor(out=t2.ap, in0=bs.ap, in1=sing.ap, op=ALU.mult),
                                 reads=[bs, sing], writes=[t2])
                            dst = dst_fn(c)
                            P.op("dve", lambda e, t1=t1, t2=t2, dst=dst: e.tensor_tensor(out=dst.ap, in0=t1.ap, in1=t2.ap, op=ALU.add),
                                 reads=[t1, t2], writes=[dst])
                    tasks.append(([win_blk(base + 256 * b), win_blk(base + 512 + 256 * b)], comp))
            rope_task(0, lambda c: qT[c])
            rope_task(1024, lambda c: kT[c][g])

            for b in range(2):
                def comp(slots, b=b):
                    (s1, v1), = slots
                    for i in range(4):
                        t = 4 * g + i
                        bv = bank()
                        mm_acc(bv.ap[:, 0:256], bv, [(hT[k].ap[:, i * 128:(i + 1) * 128], v1[0][:, k, :]) for k in range(8)], [s1] + hT)
                        vv = vA[t].ap.rearrange("p (h e) -> p h e", h=8)
                        if b == 0:
                            P.op("pool", lambda e, vv=vv: e.memset(vv[:, :, 64:65], 1.0), writes=[vA[t]])
                        P.op("act", lambda e, vv=vv, bv=bv, b=b: e.activation(
                            out=vv[:, 4 * b:4 * b + 4, 0:64], in_=bv.ap[:, 0:256].rearrange("p (h e) -> p h e", h=4), func=AF.Copy),
                            reads=[bv], writes=[vA[t]])
                tasks.append(([win_blk(2048 + 256 * b)], comp))

            for b in range(2):
                def comp(slots, b=b):
                    (s1, v1), (s2, v2) = slots
                    for cc in range(2):
                        c = 2 * b + cc
                        bu = bank()
                        mm_acc(bu.ap, bu, [(v1[0][:, k, cc * 128:(cc + 1) * 128], hT[k].ap) for k in range(8)], [s1] + hT)
                        bg = bank()
                        mm_acc(bg.ap, bg, [(v2[0][:, k, cc * 128:(cc + 1) * 128], hT[k].ap) for k in range(8)], [s2] + hT)
                        sg = scratch()
                        P.op("act", lambda e, sg=sg, bg=bg: e.activation(out=sg.ap, in_=bg.ap, func=AF.Sigmoid), reads=[bg], writes=[sg])
                        if g == 0:
                            P.op("pool", lambda e, c=c: e.memset(U[c].ap[:, 0:30], 0.0), writes=[U[c]])
                        else:
                            P.op("pool", lambda e, c=c: e.tensor_copy(out=U[c].ap[:, 0:30], in_=U[c].ap[:, 512:542]), reads=[U[c]], writes=[U[c]])
                        P.op("dve", lambda e, c=c, bu=bu, sg=sg: e.tensor_tensor(out=U[c].ap[:, 30:542], in0=bu.ap, in1=sg.ap, op=ALU.mult),
                             reads=[bu, sg], writes=[U[c]])
                        wc0 = pb + 44
                        for j in range(31):
                            if j % 2 == 0:
                                P.op("dve", lambda e, j=j, c=c: e.tensor_scalar(out=dgs[j].ap, in0=identb.ap, scalar1=ppc(wc0 + 4 * j + c), scalar2=None,
                                                                              op0=ALU.mult), reads=[identb, pp], writes=[dgs[j]])
                            else:
                                P.op("act", lambda e, j=j, c=c: e.activation(out=dgs[j].ap, in_=identb.ap, func=AF.Copy, scale=ppc(wc0 + 4 * j + c)),
                                     reads=[identb, pp], writes=[dgs[j]])
                        by = bank()
                        for j in range(31):
                            P.op("pe", lambda e, j=j, c=c, by=by: e.matmul(by.ap, lhsT=dgs[j].ap, rhs=U[c].ap[:, j:j + 512], start=(j == 0), stop=(j == 30)),
                                 reads=[dgs[j], U[c]], writes=[by])
                        P.op("dve", lambda e, c=c, by=by: e.tensor_scalar(out=Y[c].ap, in0=by.ap, scalar1=ppc(pb + 32 + c), scalar2=None, op0=ALU.add),
                             reads=[by, pp], writes=[Y[c]])
                tasks.append(([win_blk(2560 + 256 * b), win_blk(3072 + 256 * b)], comp))

            def comp_ln_attn(slots):
                bm = bank()
                mm_acc(bm.ap, bm, [(ones512.ap, Y[c].ap) for c in range(4)], [ones512] + Y)
                be = bank()
                for c in range(4):
                    sq = scratch()
                    P.op("act", lambda e, sq=sq, c=c: e.activation(out=sq.ap, in_=Y[c].ap, func=AF.Square), reads=[Y[c]], writes=[sq])
                    P.op("pe", lambda e, sq=sq, c=c: e.matmul(be.ap, lhsT=ones512.ap, rhs=sq.ap, start=(c == 0), stop=(c == 3)),
                         reads=[ones512, sq], writes=[be])
                msq = scratch()
                P.op("act", lambda e: e.activation(out=msq.ap, in_=bm.ap, func=AF.Square), reads=[bm], writes=[msq])
                var = scratch()
                P.op("dve", lambda e: e.tensor_tensor(out=var.ap, in0=be.ap, in1=msq.ap, op=ALU.subtract), reads=[be, msq], writes=[var])
                P.op("act", lambda e: e.activation(out=var.ap, in_=var.ap, func=AF.Sqrt, bias=EPS, scale=1.0), reads=[var], writes=[var])
                P.op("dve", lambda e: e.reciprocal(out=var.ap, in_=var.ap), reads=[var], writes=[var])
                for c in range(4):
                    yn = scratch()
                    P.op("dve", lambda e, yn=yn, c=c: e.tensor_tensor(out=yn.ap, in0=Y[c].ap, in1=bm.ap, op=ALU.subtract),
                         reads=[Y[c], bm], writes=[yn])
                    P.op("dve", lambda e, yn=yn: e.tensor_tensor(out=yn.ap, in0=yn.ap, in1=var.ap, op=ALU.mult), reads=[yn, var], writes=[yn])
                    P.op("act", lambda e, yn=yn, c=c: e.activation(out=sT[c].ap, in_=yn.ap, func=AF.Silu, scale=ppc(pb + 36 + c), bias=ppc(pb + 40 + c)),
                         reads=[yn, pp], writes=[sT[c]])
                nkt = 4 * g + 4
                items = [(h, kt) for h in range(8) for kt in range(nkt)]
                stg = {}

                def S_(n):
                    h, kt = items[n]
                    c, pbase = h // 2, (h % 2) * 64
                    j0 = max(kt - 4 * g, 0)
                    c0 = j0 * 128
                    bs = bank()
                    ktile = kT[c][kt // 4]
                    P.op("pe", lambda e: e.matmul(
                        bs.ap[:, c0:512], lhsT=ktile.ap[pbase:pbase + 64, (kt % 4) * 128:(kt % 4) * 128 + 128],
                        rhs=qT[c].ap[pbase:pbase + 64, c0:512], start=True, stop=True),
                        reads=[ktile, qT[c]], writes=[bs])
                    pe_t = pexp[n % 3]
                    P.op("act", lambda e: e.activation(out=pe_t.ap[:, c0:512], in_=bs.ap[:, c0:512], func=AF.Exp, scale=0.125),
                         reads=[bs], writes=[pe_t])
                    pm_t = pmsk[n % 3]
                    o0 = 4 * g + j0 - kt
                    P.op("dve", lambda e: e.tensor_tensor(
                        out=pm_t.ap[:, c0:512], in0=pe_t.ap[:, c0:512], in1=maskT.ap[:, o0 * 128:o0 * 128 + 512 - c0], op=ALU.mult),
                        reads=[pe_t, maskT], writes=[pm_t])
                    stg[n] = (pm_t, j0)

                def V_(n):
                    h, kt = items[n]
                    pm_t, j0 = stg.pop(n)
                    accb = ps[6 + (h % 2)]
                    acc = accb.ap[:, 0:260].rearrange("p (j e) -> p j e", j=4)
                    for j in range(j0, 4):
                        P.op("pe", lambda e, j=j: e.matmul(
                            acc[:, j, :], lhsT=pm_t.ap[:, j * 128:(j + 1) * 128], rhs=vA[kt].ap[:, h * 65:(h + 1) * 65],
                            start=(kt == 0 and j == 0), stop=(kt == nkt - 1 and j == 3)),
                            reads=[pm_t, vA[kt]], writes=[accb])
                    if kt == nkt - 1:
                        rc = small()
                        P.op("dve", lambda e: e.reciprocal(out=rc.ap[:, 0:4], in_=acc[:, :, 64]), reads=[accb], writes=[rc])
                        for j in range(4):
                            P.op("dve", lambda e, j=j: e.tensor_scalar(
                                out=otok[j].ap[:, h * 64:(h + 1) * 64], in0=acc[:, j, 0:64], scalar1=rc.ap[:, j:j + 1], scalar2=None, op0=ALU.mult),
                                reads=[accb, rc], writes=[otok[j]])
                LA = 2
                for n in range(min(LA, len(items))):
                    S_(n)
                for n in range(len(items)):
                    if n + LA < len(items):
                        S_(n + LA)
                    V_(n)
                for c in range(4):
                    b = bank()
                    bv = b.ap.bitcast(BF16)
                    for j in range(4):
                        P.op("pe", lambda e, bv=bv, j=j, c=c: e.transpose(out=bv[:, j * 128:(j + 1) * 128], in_=otok[j].ap[:, c * 128:(c + 1) * 128],
                                                                          identity=identb.ap), reads=[otok[j], identb], writes=[b])
                    P.op("act", lambda e, bv=bv, c=c: e.activation(out=oT[c].ap, in_=bv[:, 0:512], func=AF.Copy), reads=[b], writes=[oT[c]])
            tasks.append(([], comp_ln_attn))

            for ob in range(4):
                def comp(slots, ob=ob):
                    (s1, v1), (s2, v2), (s3, v3) = slots
                    for cc in range(2):
                        oc = 2 * ob + cc
                        sl = slice(cc * 128, (cc + 1) * 128)
                        bga = bank()
                        mm_acc(bga.ap, bga, [(v1[0][:, k, sl], hT[k].ap) for k in range(8)], [s1] + hT)
                        bgb = bank()
                        mm_acc(bgb.ap, bgb, [(v2[0][:, k, sl], hT[k].ap) for k in range(8)], [s2] + hT)
                        bya = bank()
                        mm_acc(bya.ap, bya, [(v3[0][:, k, sl], oT[k].ap) for k in range(4)], [s3] + oT)
                        byb = bank()
                        mm_acc(byb.ap, byb, [(v3[1][:, k, sl], sT[k].ap) for k in range(4)], [s3] + sT)
                        sga = scratch()
                        P.op("act", lambda e, sga=sga, bga=bga: e.activation(out=sga.ap, in_=bga.ap, func=AF.Sigmoid), reads=[bga], writes=[sga])
                        sgb = scratch()
                        P.op("act", lambda e, sgb=sgb, bgb=bgb: e.activation(out=sgb.ap, in_=bgb.ap, func=AF.Sigmoid), reads=[bgb], writes=[sgb])
                        P.op("dve", lambda e, sga=sga, bya=bya: e.tensor_tensor(out=sga.ap, in0=bya.ap, in1=sga.ap, op=ALU.mult),
                             reads=[bya, sga], writes=[sga])
                        P.op("dve", lambda e, sgb=sgb, byb=byb, oc=oc: e.scalar_tensor_tensor(out=sgb.ap, in0=byb.ap, scalar=ppc(pb + 24 + oc), in1=sgb.ap,
                                                                                            op0=ALU.add, op1=ALU.mult),
                             reads=[byb, sgb, pp], writes=[sgb])
                        P.op("dve", lambda e, sga=sga, sgb=sgb, oc=oc: e.tensor_tensor(out=mg[oc].ap, in0=sga.ap, in1=sgb.ap, op=ALU.add),
                             reads=[sga, sgb], writes=[mg[oc]])
                tasks.append(([win_blk(3584 + 256 * ob), win_blk(4608 + 256 * ob),
                               [(waol[:, :, 256 * ob:256 * ob + 256], 4, 256), (wcol[:, :, 256 * ob:256 * ob + 256], 4, 256)]], comp))

            for nb_ in range(4):
                def comp(slots, nb_=nb_):
                    (s1, v1), = slots
                    for i in range(4):
                        t = 4 * g + i
                        b = bank()
                        mm_acc(b.ap[:, 0:256], b, [(mg[k].ap[:, i * 128:(i + 1) * 128], v1[0][:, k, :]) for k in range(8)], [s1] + mg)
                        xt = X[t][nb_ // 2]
                        xs = xt.ap[:, (nb_ % 2) * 256:(nb_ % 2) * 256 + 256]
                        P.op("dve", lambda e, xs=xs, b=b: e.tensor_tensor(out=xs, in0=b.ap[:, 0:256], in1=xs, op=ALU.add),
                             reads=[b, xt], writes=[xt])
                tasks.append(([[(woutl[:, :, 256 * nb_:256 * nb_ + 256], 8, 256)]], comp))

            return tasks

        def moe_phase(l):
            pb = l * PPL
            I32 = mybir.dt.int32
            P.op("sp", lambda e: e.dma_start(out=cst.ap, in_=cst_d), writes=[cst], dkey="cst")
            P.op("pool", lambda e: e.dma_start(out=ltri.ap, in_=cst_d[:, 192:320]), writes=[ltri], dkey="cstb")
            P.op("pool", lambda e: e.memset(onesb.ap, 1.0), writes=[onesb])
            for g in range(4):
                norm_T(g, pb + 8, [h2T[c][g] for c in range(8)], xnb2)
            P.op("pool", lambda e: e.dma_start(out=wrt.ap.rearrange("p (k c) -> p k c", k=8), in_=wr_d[l].rearrange("(k p) c -> p k c", p=128)),
                 writes=[wrt], dkey="wr")
            wrv = wrt.ap.rearrange("p (k c) -> p k c", k=8)

            def route_tile(t):
                g, i = t // 4, t % 4
                b = bank()
                mm_acc(b.ap[:, 0:36], b, [(h2T[k][g].ap[:, i * 128:(i + 1) * 128], wrv[:, k, :]) for k in range(8)],
                       [wrt] + [h2T[k][g] for k in range(8)])
                lg = small()
                P.op("dve", lambda e, lg=lg, b=b: e.tensor_tensor(out=lg.ap, in0=b.ap[:, 0:36], in1=bcp.ap[:, l * 36:(l + 1) * 36], op=ALU.add),
                     reads=[b, bcp], writes=[lg])
                w1 = small()
                W = w1.ap
                P.op("dve", lambda e, lg=lg, W=W: e.tensor_reduce(out=W[:, 0:1], in_=lg.ap[:, 0:4], axis=AX.X, op=ALU.max), reads=[lg], writes=[w1])
                P.op("dve", lambda e, W=W: e.tensor_scalar(out=W[:, 1:2], in0=W[:, 0:1], scalar1=-1.0, scalar2=None, op0=ALU.mult), reads=[w1], writes=[w1])
                P.op("act", lambda e, lg=lg, W=W: e.activation(out=W[:, 8:12], in_=lg.ap[:, 0:4], func=AF.Exp, bias=W[:, 1:2], scale=1.0, accum_out=W[:, 2:3]),
                     reads=[lg, w1], writes=[w1])
                P.op("dve", lambda e, W=W: e.reciprocal(out=W[:, 3:4], in_=W[:, 2:3]), reads=[w1], writes=[w1])
                P.op("dve", lambda e, lg=lg, W=W: e.tensor_scalar(out=W[:, 4:8], in0=lg.ap[:, 0:4], scalar1=W[:, 0:1], scalar2=None, op0=ALU.is_equal),
                     reads=[lg, w1], writes=[w1])
                P.op("dve", lambda e, W=W: e.tensor_scalar(out=W[:, 4:8], in0=W[:, 4:8], scalar1=BIG, scalar2=-BIG, op0=ALU.mult, op1=ALU.add),
                     reads=[w1], writes=[w1])
                lem = small()
                for gg in range(4):
                    P.op("dve", lambda e, lem=lem, lg=lg, W=W, gg=gg: e.tensor_scalar(out=lem.ap[:, gg * 8:(gg + 1) * 8], in0=lg.ap[:, 4 + gg * 8:12 + gg * 8],
                                                                                  scalar1=W[:, 4 + gg:5 + gg], scalar2=None, op0=ALU.add),
                         reads=[lg, w1], writes=[lem])
                w2 = small()
                V = w2.ap
                oh1, oh2 = OH1[t], OH2[t]
                lem2 = small()
                P.op("dve", lambda e, lem=lem, V=V: e.tensor_reduce(out=V[:, 0:1], in_=lem.ap[:, 0:32], axis=AX.X, op=ALU.max), reads=[lem], writes=[w2])
                P.op("dve", lambda e, lem=lem, V=V, oh1=oh1: e.tensor_scalar(out=oh1.ap, in0=lem.ap[:, 0:32], scalar1=V[:, 0:1], scalar2=None, op0=ALU.is_equal),
                     reads=[lem, w2], writes=[oh1])
                P.op("dve", lambda e, lem=lem, lem2=lem2, oh1=oh1: e.scalar_tensor_tensor(out=lem2.ap[:, 0:32], in0=oh1.ap, scalar=-BIG, in1=lem.ap[:, 0:32],
                                                                                  op0=ALU.mult, op1=ALU.add), reads=[oh1, lem], writes=[lem2])
                P.op("dve", lambda e, lem2=lem2, V=V: e.tensor_reduce(out=V[:, 1:2], in_=lem2.ap[:, 0:32], axis=AX.X, op=ALU.max), reads=[lem2], writes=[w2])
                P.op("dve", lambda e, lem2=lem2, V=V, oh2=oh2: e.tensor_scalar(out=oh2.ap, in0=lem2.ap[:, 0:32], scalar1=V[:, 1:2], scalar2=None, op0=ALU.is_equal),
                     reads=[lem2, w2], writes=[oh2])
                P.op("dve", lambda e, V=V: e.tensor_tensor(out=V[:, 2:3], in0=V[:, 1:2], in1=V[:, 0:1], op=ALU.subtract), reads=[w2], writes=[w2])
                P.op("act", lambda e, V=V: e.activation(out=V[:, 3:4], in_=V[:, 2:3], func=AF.Exp), reads=[w2], writes=[w2])
                P.op("dve", lambda e, V=V: e.tensor_scalar(out=V[:, 4:5], in0=V[:, 3:4], scalar1=1.0, scalar2=None, op0=ALU.add), reads=[w2], writes=[w2])
                P.op("dve", lambda e, V=V: e.reciprocal(out=V[:, 5:6], in_=V[:, 4:5]), reads=[w2], writes=[w2])
                P.op("dve", lambda e, V=V: e.tensor_tensor(out=V[:, 6:7], in0=V[:, 3:4], in1=V[:, 5:6], op=ALU.mult), reads=[w2], writes=[w2])
                P.op("dve", lambda e, V=V, W=W, t=t: e.tensor_scalar(out=G12[t].ap, in0=V[:, 5:7], scalar1=W[:, 3:4], scalar2=None, op0=ALU.mult),
                     reads=[w2, w1], writes=[G12[t]])
                P.op("dve", lambda e, oh1=oh1, oh2=oh2, t=t: e.tensor_tensor(out=ABF[t].ap, in0=oh1.ap, in1=oh2.ap, op=ALU.add),
                     reads=[oh1, oh2], writes=[ABF[t]])

            def capture(fn, *a):
                rec = []
                P.op = lambda *args, **kw: rec.append((args, kw))
                try:
                    fn(*a)
                finally:
                    del P.op
                return rec
            RB = 3
            for t0 in range(0, NT, RB):
                recs = [capture(route_tile, t) for t in range(t0, min(t0 + RB, NT))]
                for k_ in range(max(len(r) for r in recs)):
                    for r in recs:
                        if k_ < len(r):
                            P.op(*r[k_][0], **r[k_][1])

            for t in range(NT):
                b = bank()
                for tp in range(t):
                    P.op("pe", lambda e, b=b, tp=tp: e.matmul(b.ap[:, 0:32], lhsT=onesb.ap, rhs=ABF[tp].ap, start=(tp == 0), stop=False),
                         reads=[onesb, ABF[tp]], writes=[b])
                P.op("pe", lambda e, b=b, t=t: e.matmul(b.ap[:, 0:32], lhsT=ltri.ap, rhs=ABF[t].ap, start=(t == 0), stop=True),
                     reads=[ltri, ABF[t]], writes=[b])
                P.op("act", lambda e, b=b, t=t: e.activation(out=RK[t].ap, in_=b.ap[:, 0:32], func=AF.Copy), reads=[b], writes=[RK[t]])
            bc_ = bank()
            for t in range(NT):
                P.op("pe", lambda e, t=t: e.matmul(bc_.ap[:, 0:32], lhsT=onesb.ap, rhs=ABF[t].ap, start=(t == 0), stop=(t == NT - 1)),
                     reads=[onesb, ABF[t]], writes=[bc_])
            P.op("act", lambda e: e.activation(out=cntn.ap, in_=bc_.ap[:, 0:32], func=AF.Copy), reads=[bc_], writes=[cntn])
            P.op("dve", lambda e: e.tensor_scalar(out=nn_t.ap, in0=cntn.ap, scalar1=0.0, scalar2=None, op0=ALU.is_gt), reads=[cntn], writes=[nn_t])
            for j in range(1, 16):
                P.op("dve", lambda e, j=j: e.scalar_tensor_tensor(out=nn_t.ap, in0=cntn.ap, scalar=128.0 * j, in1=nn_t.ap, op0=ALU.is_gt, op1=ALU.add),
                     reads=[cntn, nn_t], writes=[nn_t])
            P.op("dve", lambda e: e.tensor_scalar(out=np_t.ap, in0=nn_t.ap, scalar1=-2.0, scalar2=0.0, op0=ALU.add, op1=ALU.max), reads=[nn_t], writes=[np_t])
            P.op("dve", lambda e: e.tensor_copy(out=oend_t.ap[:, 0:1], in_=np_t.ap[:, 0:1]), reads=[np_t], writes=[oend_t])
            for ee in range(1, NE):
                P.op("dve", lambda e, ee=ee: e.tensor_tensor(out=oend_t.ap[:, ee:ee + 1], in0=oend_t.ap[:, ee - 1:ee], in1=np_t.ap[:, ee:ee + 1], op=ALU.add),
                     reads=[oend_t, np_t], writes=[oend_t])
            P.op("dve", lambda e: e.tensor_tensor(out=ops_t.ap, in0=oend_t.ap, in1=np_t.ap, op=ALU.subtract), reads=[oend_t, np_t], writes=[ops_t])
            P.op("dve", lambda e: e.scalar_tensor_tensor(out=q_t.ap, in0=ops_t.ap, scalar=128.0, in1=cst.ap[:, 352:384], op0=ALU.mult, op1=ALU.add),
                 reads=[ops_t, cst], writes=[q_t])
            P.op("dve", lambda e: e.tensor_scalar(out=ebacc.ap, in0=cst.ap[:, 0:64], scalar1=oend_t.ap[:, 0:1], scalar2=None, op0=ALU.is_ge),
                 reads=[cst, oend_t], writes=[ebacc])
            for ee in range(1, NE):
                P.op("dve", lambda e, ee=ee: e.scalar_tensor_tensor(out=ebacc.ap, in0=cst.ap[:, 0:64], scalar=oend_t.ap[:, ee:ee + 1], in1=ebacc.ap,
                                                                  op0=ALU.is_ge, op1=ALU.add), reads=[cst, oend_t, ebacc], writes=[ebacc])
            P.op("dve", lambda e: e.scalar_tensor_tensor(out=idxg.ap, in0=ebacc.ap, scalar=256.0, in1=cst.ap[:, 64:128], op0=ALU.mult, op1=ALU.add),
                 reads=[ebacc, cst], writes=[idxg])
            P.op("dve", lambda e: e.scalar_tensor_tensor(out=idxd.ap, in0=ebacc.ap, scalar=256.0, in1=cst.ap[:, 128:192], op0=ALU.mult, op1=ALU.add),
                 reads=[ebacc, cst], writes=[idxd])
            if _DBG == 5:
                P.op("dve", lambda e: e.tensor_copy(out=idxg.ap, in_=cst.ap[:, 64:128]), reads=[cst], writes=[idxg])
                P.op("dve", lambda e: e.tensor_copy(out=idxd.ap, in_=cst.ap[:, 128:192]), reads=[cst], writes=[idxd])
            for t in range(NT):
                tmp = small()
                sel = small()
                P.op("dve", lambda e, sel=sel, t=t: e.scalar_tensor_tensor(out=sel.ap[:, 0:32], in0=RK[t].ap, scalar=256.0, in1=q_t.ap, op0=ALU.is_ge, op1=ALU.mult),
                     reads=[RK[t], q_t], writes=[sel])
                P.op("dve", lambda e, tmp=tmp, t=t: e.tensor_tensor(out=tmp.ap[:, 0:32], in0=RK[t].ap, in1=cst.ap[:, 320:352], op=ALU.add),
                     reads=[RK[t], cst], writes=[tmp])
                P.op("dve", lambda e, tmp=tmp, sel=sel: e.tensor_tensor(out=tmp.ap[:, 0:32], in0=tmp.ap[:, 0:32], in1=sel.ap[:, 0:32], op=ALU.add),
                     reads=[tmp, sel], writes=[tmp])
                for a_, oh in enumerate((OH1[t], OH2[t])):
                    m_ = small()
                    P.op("dve", lambda e, m_=m_, tmp=tmp, oh=oh: e.tensor_tensor(out=m_.ap[:, 0:32], in0=tmp.ap[:, 0:32], in1=oh.ap, op=ALU.mult),
                         reads=[tmp, oh], writes=[m_])
                    P.op("dve", lambda e, m_=m_, t=t, a_=a_: e.tensor_reduce(out=DF[t].ap[:, a_:a_ + 1], in_=m_.ap[:, 0:32], axis=AX.X, op=ALU.add),
                         reads=[m_], writes=[DF[t]])
                P.op("dve", lambda e, t=t: e.tensor_copy(out=DI[t].ap, in_=DF[t].ap), reads=[DF[t]], writes=[DI[t]])
                P.op("dve", lambda e, t=t: e.tensor_scalar(out=DIS[t].ap, in0=DF[t].ap, scalar1=24576.0, scalar2=None, op0=ALU.add), reads=[DF[t]], writes=[DIS[t]])
            for k in range(8):
                P.op("pool", lambda e, k=k: e.tensor_scalar(out=GT.ap[:, k * 128:(k + 1) * 128], in0=onesb.ap, scalar1=ppc(pb + 168 + k), scalar2=None, op0=ALU.mult),
                     reads=[onesb, pp], writes=[GT])
            if _DBG == 6:
                dbt = scr[0]
                P.op("dve", lambda e: e.tensor_copy(out=dbt.ap[:, 0:32], in_=cntn.ap), reads=[cntn], writes=[dbt])
                P.op("dve", lambda e: e.tensor_copy(out=dbt.ap[:, 32:64], in_=nn_t.ap), reads=[nn_t], writes=[dbt])
                P.op("dve", lambda e: e.tensor_copy(out=dbt.ap[:, 64:96], in_=end_t.ap), reads=[end_t], writes=[dbt])
                P.op("dve", lambda e: e.tensor_copy(out=dbt.ap[:, 96:128], in_=psb_t.ap), reads=[psb_t], writes=[dbt])
                P.op("dve", lambda e: e.tensor_copy(out=dbt.ap[:, 128:192], in_=ebacc.ap), reads=[ebacc], writes=[dbt])
                P.op("dve", lambda e: e.tensor_copy(out=dbt.ap[:, 192:256], in_=idxg.ap), reads=[idxg], writes=[dbt])
                P.op("dve", lambda e: e.tensor_copy(out=dbt.ap[:, 256:258], in_=DF[0].ap), reads=[DF[0]], writes=[dbt])
                P.op("dve", lambda e: e.tensor_copy(out=dbt.ap[:, 258:260], in_=DI[0].ap), reads=[DI[0]], writes=[dbt])
                P.op("dve", lambda e: e.tensor_copy(out=dbt.ap[:, 260:292], in_=RK[1].ap), reads=[RK[1]], writes=[dbt])
                P.op("dve", lambda e: e.tensor_copy(out=dbt.ap[:, 292:324], in_=OH1[0].ap), reads=[OH1[0]], writes=[dbt])
                P.op("sp", lambda e: e.dma_start(out=dbg_d, in_=dbt.ap), reads=[dbt], dkey="dbg")
                ple_prep(l)
                return
            if _DBG == 1:
                ple_prep(l)
                return
            sc_tiles = []
            for t in range(NT):
                ss = row_rstd(t, junk_moe)
                xs = xs_t[t % 2]
                P.op("dve", lambda e, t=t, ss=ss, xs=xs: e.tensor_scalar(out=xs.ap, in0=xrow(t), scalar1=ss.ap[:, 2:3], scalar2=None, op0=ALU.mult),
                     reads=[X[t][0], X[t][1], ss], writes=[xs])
                for a_ in range(2):
                    dt_ = dram_tiles[("sc", t, a_)]
                    P.op("pool", lambda e, t=t, a_=a_, xs=xs: e.indirect_dma_start(
                        out=sorted_d, out_offset=bass.IndirectOffsetOnAxis(ap=DIS[t].ap[:, a_:a_ + 1], axis=0),
                        in_=xs.ap, in_offset=None, bounds_check=P.regs['b36351'], oob_is_err=False),
                        reads=[xs, DIS[t]], writes=[dt_], dkey=f"sc{t % 2}")
                    sc_tiles.append(dt_)
            ple_prep(l)
            if _DBG == 2:
                return

            gser = dram_tiles[('gser',)]

            def eload(b_, mats):
                s_ = b_ % 2
                srcs = (weg_d[l], weu_d[l], wed_d[l])
                for m_ in mats:
                    src = srcs[m_]
                    for h_, idt in enumerate((idxg, idxd)):
                        P.op("pool", lambda e, m_=m_, src=src, h_=h_, idt=idt: e.indirect_dma_start(
                            out=ew[s_][m_].ap[:, h_ * 2048:(h_ + 1) * 2048], out_offset=None, in_=src[:, :],
                            in_offset=bass.IndirectOffsetOnAxis(ap=idt.ap[:, b_:b_ + 1], axis=0),
                            bounds_check=P.regs['b8191'], oob_is_err=False),
                            reads=[idt], writes=[ew[s_][m_]], dkey=f"ew{s_}_{m_}")

            ys_tiles = []

            def views(s_):
                return (ew[s_][0].ap.rearrange("p (k c) -> p k c", k=8), ew[s_][1].ap.rearrange("p (k c) -> p k c", k=8),
                        ew[s_][2].ap.rearrange("p (k c) -> p k c", k=4))

            def stA_load(b_):
                xg = xg_t[b_ % 4]
                P.op("sp", lambda e: e.dma_start(out=xg.ap, in_=sorted_d[24576 + b_ * 128:24576 + (b_ + 1) * 128, :]), reads=sc_tiles, writes=[xg], dkey=f"xgl{b_ % 4}")

            def stA(b_):
                x2 = b_ % 2
                xg = xg_t[b_ % 4]
                bt = bank()
                btv = bt.ap.bitcast(BF16)
                xgv = xg.ap.rearrange("p (a k) -> p a k", k=8)
                for k in range(8):
                    P.op("pe", lambda e, k=k: e.transpose(out=btv[:, k * 128:(k + 1) * 128], in_=xgv[:, :, k], identity=identb.ap),
                         reads=[xg, identb], writes=[bt])
                xb = xbT[x2]
                P.op("dve", lambda e: e.tensor_tensor(out=xb.ap, in0=btv[:, 0:1024], in1=GT.ap, op=ALU.mult), reads=[bt, GT], writes=[xb])

            def stB(b_, s_):
                x2 = b_ % 2
                vg, vu, vd = views(s_)
                xb = xbT[x2]
                bg = bank()
                mm_acc(bg.ap, bg, [(xb.ap[:, k * 128:(k + 1) * 128], vg[:, k, :]) for k in range(8)], [xb, ew[s_][0]])
                bu = bank()
                mm_acc(bu.ap, bu, [(xb.ap[:, k * 128:(k + 1) * 128], vu[:, k, :]) for k in range(8)], [xb, ew[s_][1]])
                sg = scratch()
                P.op("act", lambda e: e.activation(out=sg.ap, in_=bg.ap, func=AF.Silu), reads=[bg], writes=[sg])
                hb_ = hbt[x2]
                P.op("dve", lambda e: e.tensor_tensor(out=hb_.ap, in0=bu.ap, in1=sg.ap, op=ALU.mult), reads=[bu, sg], writes=[hb_])

            def stT(b_):
                x2 = b_ % 2
                hb_ = hbt[x2]
                bh = bank()
                bhv = bh.ap.bitcast(BF16)
                for f in range(4):
                    P.op("pe", lambda e, f=f: e.transpose(out=bhv[:, f * 128:(f + 1) * 128], in_=hb_.ap[:, f * 128:(f + 1) * 128], identity=identb.ap),
                         reads=[hb_, identb], writes=[bh])
                hT_ = hbT[x2]
                P.op("act", lambda e: e.activation(out=hT_.ap, in_=bhv[:, 0:512], func=AF.Copy), reads=[bh], writes=[hT_])

            def stC(b_, s_):
                x2 = b_ % 2
                vg, vu, vd = views(s_)
                hT_ = hbT[x2]
                yo = yst[b_ % 4]
                for hh in range(2):
                    bd = bank()
                    mm_acc(bd.ap, bd, [(hT_.ap[:, f * 128:(f + 1) * 128], vd[:, f, hh * 512:(hh + 1) * 512]) for f in range(4)], [hT_, ew[s_][2]])
                    if hh == 0:
                        P.op("act", lambda e, bd=bd: e.activation(out=yo.ap[:, 0:512], in_=bd.ap, func=AF.Copy), reads=[bd], writes=[yo])
                    else:
                        P.op("dve", lambda e, bd=bd: e.tensor_copy(out=yo.ap[:, 512:1024], in_=bd.ap), reads=[bd], writes=[yo])
                yt_ = dram_tiles[("ys", b_)]
                P.op("sp", lambda e: e.dma_start(out=ys_d[b_ * 128:(b_ + 1) * 128, :], in_=yo.ap), reads=[yo], writes=[yt_], dkey=f"yst{b_ % 4}")
                ys_tiles.append(yt_)

            def sload(e_, mats):
                s_ = e_ % 2
                srcs = (weg_d[l], weu_d[l], wed_d[l])
                for m_ in mats:
                    src = srcs[m_]
                    P.op("pool", lambda e, m_=m_, src=src: e.dma_start(
                        out=ew[s_][m_].ap.rearrange("p (h c) -> p h c", h=2), in_=src[256 * e_:256 * (e_ + 1), :].rearrange("(p h) c -> p h c", h=2)),
                        writes=[ew[s_][m_]], dkey=f"ew{s_}_{m_}")

            NOVF = 28
            NB_ALL = 64 + NOVF

            def slot_of(b_):
                return (b_ // 2) % 2 if b_ < 64 else (b_ - 64) % 2

            for i in range(-6, NB_ALL):
                for e_ in range(NE):
                    if i == 2 * e_ - 4:
                        sload(e_, (0, 1))
                    if i == 2 * e_ - 2:
                        sload(e_, (2,))
                for o_ in range(NOVF):
                    b_ = 64 + o_
                    if i == b_ - 3:
                        eload(o_, (0, 1))
                    if i == b_ - 1:
                        eload(o_, (2,))
                if 0 <= i + 6 < NB_ALL:
                    stA_load(i + 6)
                if 0 <= i + 3 < NB_ALL:
                    stA(i + 3)
                if 0 <= i + 2 < NB_ALL:
                    stB(i + 2, slot_of(i + 2))
                if 0 <= i + 1 < NB_ALL:
                    stT(i + 1)
                if 0 <= i < NB_ALL:
                    stC(i, slot_of(i))
            if _DBG in (3, 4, 5):
                return
            cbufs = [ycmb[0], ycmb[1], yst[0], yst[1], yst[2], yst[3]]
            for t in range(NT):
                for a_ in range(2):
                    ci = (2 * t + a_) % 6
                    yc = cbufs[ci]
                    P.op("pool", lambda e, t=t, a_=a_, yc=yc: e.indirect_dma_start(
                        out=yc.ap, out_offset=None, in_=ys_d[:, :], in_offset=bass.IndirectOffsetOnAxis(ap=DI[t].ap[:, a_:a_ + 1], axis=0),
                        bounds_check=P.regs['b11775'], oob_is_err=False),
                        reads=ys_tiles + [DI[t]], writes=[yc], dkey=f"yc{ci}")
                    for hh in range(2):
                        xt = X[t][hh]
                        P.op("dve", lambda e, t=t, a_=a_, yc=yc, hh=hh, xt=xt: e.scalar_tensor_tensor(
                            out=xt.ap, in0=yc.ap[:, hh * 512:(hh + 1) * 512], scalar=G12[t].ap[:, a_:a_ + 1], in1=xt.ap, op0=ALU.mult, op1=ALU.add),
                            reads=[yc, G12[t], xt], writes=[xt])

        def ple_prep(l):
            P.op("pool", lambda e: e.dma_start(out=wpe.ap.rearrange("p (k c) -> p k c", k=2), in_=wpe_d[l].rearrange("(k p) c -> p k c", p=128)),
                 writes=[wpe], dkey="wpe")
            for t in range(NT):
                stt = pst[t % 2]
                P.op("sp", lambda e, t=t, stt=stt: e.dma_start(out=stt.ap, in_=p_d[l, t * 128:(t + 1) * 128, :]), writes=[stt], dkey=f"pst{t % 2}")
                b = bank()
                for kc in range(2):
                    P.op("pe", lambda e, b=b, kc=kc, stt=stt: e.transpose(out=b.ap[:, kc * 128:(kc + 1) * 128], in_=stt.ap[:, kc * 128:(kc + 1) * 128], identity=ident.ap),
                         reads=[stt, ident], writes=[b])
                for kc in range(2):
                    P.op("act", lambda e, b=b, kc=kc, t=t: e.activation(out=pT[kc].ap[:, t * 128:(t + 1) * 128], in_=b.ap[:, kc * 128:(kc + 1) * 128], func=AF.Copy),
                         reads=[b], writes=[pT[kc]])

        def ple_phase(l):
            pb = l * PPL
            wpgl = wpg_d[l].rearrange("(k p) c -> p k c", p=128)
            for i in range(2):
                P.op("pool", lambda e, i=i: e.dma_start(out=wpg[i].ap.rearrange("p (k c) -> p k c", k=8), in_=wpgl[:, :, i * 512:(i + 1) * 512]),
                     writes=[wpg[i]], dkey="wp")
            wpev = wpe.ap.rearrange("p (k c) -> p k c", k=2)

            def ple_main(g_):
                for t in range(4 * g_, 4 * g_ + 4):
                    ple_tile(t)

            def ple_tile(t):
                g, i = t // 4, t % 4
                for hh in range(2):
                    wv = wpg[hh].ap.rearrange("p (k c) -> p k c", k=8)
                    bg = bank()
                    mm_acc(bg.ap, bg, [(h2T[k][g].ap[:, i * 128:(i + 1) * 128], wv[:, k, :]) for k in range(8)], [wpg[hh]] + [h2T[k][g] for k in range(8)])
                    be = bank()
                    mm_acc(be.ap, be, [(pT[kc].ap[:, t * 128:(t + 1) * 128], wpev[:, kc, hh * 512:(hh + 1) * 512]) for kc in range(2)], [wpe] + pT)
                    sg = scratch()
                    P.op("act", lambda e, sg=sg, bg=bg: e.activation(out=sg.ap, in_=bg.ap, func=AF.Sigmoid), reads=[bg], writes=[sg])
                    P.op("dve", lambda e, sg=sg, be=be: e.tensor_tensor(out=sg.ap, in0=be.ap, in1=sg.ap, op=ALU.mult), reads=[be, sg], writes=[sg])
                    xt = X[t][hh]
                    P.op("pool", lambda e, sg=sg, xt=xt: e.tensor_tensor(out=xt.ap, in0=xt.ap, in1=sg.ap, op=ALU.add), reads=[sg, xt], writes=[xt])

            norm_T(0, pb + 16, [h2T[c][0] for c in range(8)], xnb2)
            for g in range(4):
                if g + 1 < 4:
                    norm_T(g + 1, pb + 16, [h2T[c][g + 1] for c in range(8)], xnb2)
                ple_main(g)

        def final_phase():
            P.op("sp", lambda e: e.dma_start(out=nfin.ap, in_=bc_d[:, 72:72 + D]), writes=[nfin], dkey="nf")
            for t in range(NT):
                ss = row_rstd(t, junk_moe)
                o = ost[t % 2]
                P.op("dve", lambda e, t=t, ss=ss, o=o: e.scalar_tensor_tensor(out=o.ap, in0=xrow(t), scalar=ss.ap[:, 2:3], in1=nfin.ap, op0=ALU.mult, op1=ALU.mult),
                     reads=[X[t][0], X[t][1], ss, nfin], writes=[o])
                P.op("sp", lambda e, t=t, o=o: e.dma_start(out=out_d[t * 128:(t + 1) * 128, :], in_=o.ap), reads=[o], dkey=f"ost{t % 2}")
            for i in range(2):
                P.op("sp", lambda e: e.nop(), writes=[ost[i]])

        for l in range(nl):
            for g in range(4):
                tasks = mixer_group(l, g)
                loaded = {}
                n = len(tasks)
                nxt_load = 0
                in_use = 0
                for ti in range(n):
                    while nxt_load < n and (nxt_load <= ti or in_use + len(tasks[nxt_load][0]) <= 6):
                        loaded[nxt_load] = [wload(srcs) for srcs in tasks[nxt_load][0]]
                        in_use += len(tasks[nxt_load][0])
                        nxt_load += 1
                    tasks[ti][1](loaded.pop(ti))
                    in_use -= len(tasks[ti][0])
            nbank[0] = 8
            moe_phase(l)
            ple_phase(l)
            nbank[0] = 6
        final_phase()
        P.emit()
        nops = {k: len(v) for k, v in P.ops.items()}
        print("ops per engine:", nops)
    return nc


def _host_layout(inp):
    f = np.float32
    w_in = np.asarray(inp["w_in"], f)
    swap = np.arange(512).reshape(8, 64)
    swap = np.concatenate([swap[:, 32:], swap[:, :32]], axis=1).reshape(-1)
    q, k, v = w_in[:, :, 0:512], w_in[:, :, 512:1024], w_in[:, :, 1024:1536]
    rest = w_in[:, :, 1536:]
    win = np.ascontiguousarray(np.concatenate([q, q[:, :, swap], k, k[:, :, swap], v, rest], axis=2))
    assert win.shape[2] == WIN_EXT
    wr = np.ascontiguousarray(np.concatenate([np.asarray(inp["w_route_group"], f), np.asarray(inp["w_route_expert"], f)], axis=2))
    pp = np.zeros((128, L * PPL), f)

    def cols(vec, n):
        return np.asarray(vec, f).reshape(n, 128).T
    for l in range(L):
        b = l * PPL
        pp[:, b + 0:b + 8] = cols(inp["norm_mix"][l], 8)
        pp[:, b + 8:b + 16] = cols(inp["norm_ffn"][l], 8)
        pp[:, b + 168:b + 176] = np.asarray(inp["norm_ffn"][l], f).reshape(128, 8)
        pp[:, b + 16:b + 24] = cols(inp["norm_ple"][l], 8)
        pp[:, b + 24:b + 32] = cols(inp["b_conv_out"][l], 8)
        pp[:, b + 32:b + 36] = cols(inp["b_dw"][l], 4)
        pp[:, b + 36:b + 40] = cols(inp["ln_conv_g"][l], 4)
        pp[:, b + 40:b + 44] = cols(inp["ln_conv_b"][l], 4)
        wd = np.asarray(inp["w_dw"][l], f)
        for j in range(31):
            pp[:, b + 44 + 4 * j:b + 48 + 4 * j] = cols(wd[j], 4)
    bc = np.zeros((128, 72 + D), f)
    for l in range(L):
        bc[:, l * 36:l * 36 + 4] = np.asarray(inp["b_route_group"][l], f)[None, :]
        bc[:, l * 36 + 4:l * 36 + 36] = np.asarray(inp["b_route_expert"][l], f)[None, :]
    bc[:, 72:] = np.asarray(inp["norm_final"], f)[None, :]
    inv = np.power(f(10000.0), -np.arange(0, 64, 2, dtype=f) / f(64)).astype(f)
    ang = (np.arange(S, dtype=f)[:, None] * inv[None, :]).astype(f)
    cs, sn = np.cos(ang).astype(f).T, np.sin(ang).astype(f).T
    cosT = np.concatenate([cs, cs, cs, cs], axis=0)
    sinT = np.concatenate([-sn, sn, -sn, sn], axis=0)
    kk = np.arange(128)[:, None]
    col = np.arange(S)[None, :]
    dl = col - kk
    cnt = ((dl >= 0) & (dl <= 128)).astype(f) + ((dl >= 0) & (dl <= 512) & (dl % 4 == 0)).astype(f) \
        + ((dl >= 0) & (dl <= 2048) & (dl % 16 == 0)).astype(f)
    shared = {
        "win": win, "wao": np.ascontiguousarray(inp["w_attn_out"], f), "wco": np.ascontiguousarray(inp["w_conv_out"], f),
        "wout": np.ascontiguousarray(inp["w_out"], f), "wr": wr,
        "wpg": np.ascontiguousarray(inp["w_ple_gate"], f), "wpe": np.ascontiguousarray(inp["w_ple_proj"], f),
        "pp": pp, "bc": bc, "cosT": np.ascontiguousarray(cosT), "sinT": np.ascontiguousarray(sinT),
        "maskT": np.ascontiguousarray(cnt), "ident": np.eye(128, dtype=f),
    }
    for l in range(L):
        shared[f"weg{l}"] = np.ascontiguousarray(inp["w_exp_gate"][l], f).reshape(8192, 2048)
        shared[f"weu{l}"] = np.ascontiguousarray(inp["w_exp_up"][l], f).reshape(8192, 2048)
        shared[f"wed{l}"] = np.ascontiguousarray(
            np.asarray(inp["w_exp_down"][l], f).reshape(NE, 4, 128, D).transpose(0, 2, 1, 3)).reshape(8192, 2048)
    cst = np.zeros((128, 384), f)
    cst[:, 320:352] = (256 * np.arange(32, dtype=f))[None, :]
    cst[:, 352:384] = (7936 - 256 * np.arange(32, dtype=f))[None, :]
    cst[:, 0:64] = np.arange(64, dtype=f)[None, :]
    cst[:, 64:128] = (2 * np.arange(128, dtype=f))[:, None]
    cst[:, 128:192] = (2 * np.arange(128, dtype=f) + 1)[:, None]
    cst[:, 192:320] = (np.arange(128)[:, None] < np.arange(128)[None, :]).astype(f)
    shared["cst"] = cst
    return shared


_NL = L
_DBG = 0


def kernel(**inputs):
    shared = _host_layout(inputs)
    x = np.asarray(inputs["x"], np.float32)
    p = np.asarray(inputs["p"], np.float32)
    nc = build_program(_NL)
    in_maps = []
    for b in range(8):
        m = dict(shared)
        m["x"] = np.ascontiguousarray(x[b])
        m["p"] = np.ascontiguousarray(p[:, b])
        in_maps.append(m)
    res = run_bass_kernel_spmd(nc, in_maps, core_ids=list(range(8)))
    return np.stack([r["out"] for r in res.results], axis=0).astype(np.float32)
```

```python
import contextlib
import numpy as np
import concourse.bass as bass
import concourse.mybir as mybir
from concourse.bass_utils import run_bass_kernel_spmd

F32 = mybir.dt.float32
BF16 = mybir.dt.bfloat16
ALU = mybir.AluOpType
AF = mybir.ActivationFunctionType
AX = mybir.AxisListType

S = 2048
D = 1024
L = 2
NT = 16
NE = 32
WIN_EXT = 5632
PPL = 176
EPS = 1e-6
BIG = 1.0e30

STRICT_SAME = True


class T:
    __slots__ = ("ap", "space", "lo", "hi", "w", "r", "ov", "name")

    def __init__(self, ap, space, lo, hi, name=""):
        self.ap, self.space, self.lo, self.hi, self.name = ap, space, lo, hi, name
        self.w = None
        self.r = {}
        self.ov = [self]


class Op:
    __slots__ = ("eng", "fn", "deps", "ddeps", "sig", "count", "dkey", "dval", "is_write")

    def __init__(self, eng, fn):
        self.eng, self.fn = eng, fn
        self.deps = []
        self.ddeps = {}
        self.sig = False
        self.count = None
        self.dkey = None
        self.dval = None
        self.is_write = False


class Prog:
    def __init__(self, nc):
        self.nc = nc
        self.ops = {e: [] for e in ("pe", "act", "dve", "pool", "sp")}
        self.tiles = {"sb": [], "ps": []}
        self.dma_counts = {}
        self.reg_init = {}
        self.regs = {}

    def tile(self, ap, space, lo, hi, name=""):
        t = T(ap, space, lo, hi, name)
        for o in self.tiles[space]:
            if o.lo < hi and lo < o.hi:
                o.ov.append(t)
                t.ov.append(o)
        self.tiles[space].append(t)
        return t

    def _dep_on(self, op, prod, raw=True):
        if prod is None or prod is op:
            return
        if prod.dkey is not None:
            k = prod.dkey
            if not raw and op.dkey == k and prod.dval is not None and prod.is_write:
                return
            v = self.dma_counts[k]
            if op.ddeps.get(k, 0) < v:
                op.ddeps[k] = v
            return
        if prod.eng == op.eng and op.dkey is None and (op.eng == "pe" or not STRICT_SAME):
            return
        prod.sig = True
        op.deps.append(prod)

    def op(self, eng, fn, reads=(), writes=(), dkey=None):
        o = Op(eng, fn)
        o.dkey = dkey
        o.is_write = len(writes) > 0
        for t in reads:
            for u in t.ov:
                self._dep_on(o, u.w)
        for t in writes:
            for u in t.ov:
                self._dep_on(o, u.w, raw=False)
                for rd in u.r.values():
                    self._dep_on(o, rd)
        if dkey is not None:
            self.dma_counts[dkey] = self.dma_counts.get(dkey, 0) + 16
            o.dval = self.dma_counts[dkey]
        stream = eng if dkey is None else ("dma", id(o))
        for t in reads:
            t.r[stream] = o
        for t in writes:
            t.w = o
            t.r = {}
        self.ops[eng].append(o)
        return o

    def emit(self):
        nc = self.nc
        with contextlib.ExitStack() as st:
            sems = {e: st.enter_context(nc.semaphore("s_" + e)) for e in ("pe", "act", "dve", "pool")}
            dsems = {k: st.enter_context(nc.semaphore("d_" + str(k))) for k in self.dma_counts}
            for e, lst in self.ops.items():
                c = 0
                for o in lst:
                    if o.dkey is None and o.sig:
                        c += 1
                        o.count = c
            block = st.enter_context(nc.Block())

            def run(engname, engobj):
                waited = {}
                if engname == "pool":
                    for nm, val in self.reg_init.items():
                        r = engobj.alloc_register("bnd_" + nm)
                        engobj.reg_mov(r, val)
                        self.regs[nm] = r
                for o in self.ops[engname]:
                    best = {}
                    for p in o.deps:
                        if best.get(p.eng, 0) < p.count:
                            best[p.eng] = p.count
                    for pe_, cnt in best.items():
                        if waited.get(("c", pe_), 0) < cnt:
                            engobj.wait_ge(sems[pe_], cnt)
                            waited[("c", pe_)] = cnt
                    for k, v in o.ddeps.items():
                        if waited.get(("d", k), 0) < v:
                            engobj.wait_ge(dsems[k], v)
                            waited[("d", k)] = v
                    ins = o.fn(engobj)
                    if o.dkey is not None:
                        ins.then_inc(dsems[o.dkey], 16)
                    elif o.sig:
                        ins.then_inc(sems[engname], 1)

            @block.sync
            def _(e):
                run("sp", e)

            @block.tensor
            def _(e):
                run("pe", e)

            @block.scalar
            def _(e):
                run("act", e)

            @block.vector
            def _(e):
                run("dve", e)

            @block.gpsimd
            def _(e):
                run("pool", e)


def build_program(nl=L):
    nc = bass.Bass("TRN2", target_bir_lowering=False)

    def din(name, shape):
        return nc.dram_tensor(name, list(shape), F32, kind="ExternalInput").ap()

    x_d = din("x", [S, D])
    p_d = din("p", [L, S, 256])
    win_d = din("win", [L, D, WIN_EXT])
    wao_d = din("wao", [L, 512, D])
    wco_d = din("wco", [L, 512, D])
    wout_d = din("wout", [L, D, D])
    wr_d = din("wr", [L, D, 36])
    weg_d = [nc.dram_tensor(f"weg{l}", [8192, 2048], F32, kind="ExternalInput") for l in range(L)]
    weu_d = [nc.dram_tensor(f"weu{l}", [8192, 2048], F32, kind="ExternalInput") for l in range(L)]
    wed_d = [nc.dram_tensor(f"wed{l}", [8192, 2048], F32, kind="ExternalInput") for l in range(L)]
    cst_d = din("cst", [128, 384])
    scr_d = nc.dram_tensor("moe_scr", [18432, D], F32)
    ys_d = scr_d
    sorted_d = scr_d[:, :].bitcast(BF16).rearrange("r (two c) -> (r two) c", two=2)
    wpg_d = din("wpg", [L, D, D])
    wpe_d = din("wpe", [L, 256, D])
    pp_d = din("pp", [128, L * PPL])
    bc_d = din("bc", [128, 72 + D])
    cos_d = din("cosT", [128, S])
    sin_d = din("sinT", [128, S])
    mask_d = din("maskT", [128, S])
    id_d = din("ident", [128, 128])
    out_d = nc.dram_tensor("out", [S, D], F32, kind="ExternalOutput").ap()
    dbg_d = nc.dram_tensor("dbg", [128, 512], F32, kind="ExternalOutput").ap() if _DBG == 6 else None

    NB = 212800
    with contextlib.ExitStack() as st:
        big = st.enter_context(nc.sbuf_tensor("arena", [128, NB // 4], F32))
        pbanks = [st.enter_context(nc.psum_tensor(f"pb{i}", [128, 512], F32)) for i in range(8)]
        P = Prog(nc)
        P.reg_init = {'b8191': 8191, 'b11775': 11775, 'b36351': 24576 + 11775}
        top = [0]

        def alloc(nbytes, at=None):
            nbytes = (nbytes + 31) // 32 * 32
            if at is None:
                at = top[0]
                top[0] = at + nbytes
            assert at + nbytes <= NB, ("SBUF overflow", at, nbytes)
            return at, at + nbytes

        def mk(n, dtype, name, at=None):
            es = 2 if dtype == BF16 else 4
            lo, hi = alloc(n * es, at)
            ap = big[:, lo // 4:hi // 4]
            if dtype != F32:
                ap = ap.bitcast(dtype)
            return P.tile(ap[:, 0:n], "sb", lo, hi, name)

        ps = [P.tile(pbanks[i][:, :], "ps", i * 2048, (i + 1) * 2048, f"ps{i}") for i in range(8)]
        rr = {"b": 0, "s": 0, "m": 0}

        nbank = [6]

        def bank():
            rr["b"] = (rr["b"] + 1) % nbank[0]
            return ps[rr["b"]]

        X = [[mk(512, F32, f"x{t}_{h}") for h in range(2)] for t in range(NT)]
        x0 = X[0][0].lo

        def xrow(t):
            lo = x0 + t * 4096
            return big[:, lo // 4: lo // 4 + 1024]
        ident = mk(128, F32, "ident")
        identb = mk(128, BF16, "identb")
        ones512 = mk(128, F32, "ones512")
        maskT = mk(S, BF16, "mask")
        pp = mk(L * PPL, F32, "pp")
        bcp = mk(72, F32, "bc")
        scr = [mk(512, F32, f"scr{i}") for i in range(5)]
        smalls = [mk(36, F32, f"sm{i}") for i in range(18)]
        PH0 = top[0]

        def scratch():
            rr["s"] = (rr["s"] + 1) % 5
            return scr[rr["s"]]

        def small():
            rr["m"] = (rr["m"] + 1) % 18
            return smalls[rr["m"]]

        top[0] = PH0
        kT = [[mk(512, BF16, f"kT{c}_{g}") for g in range(4)] for c in range(4)]
        vA = [mk(520, BF16, f"vA{t}") for t in range(NT)]
        hT = [mk(512, BF16, f"hT{c}") for c in range(8)]
        ring = [mk(2048, BF16, f"ring{i}") for i in range(6)]
        qT = [mk(512, BF16, f"qT{c}") for c in range(4)]
        cos2 = [mk(512, F32, f"cosg{i}") for i in range(2)]
        sin2 = [mk(512, F32, f"sing{i}") for i in range(2)]
        U = [mk(542, BF16, f"u{c}") for c in range(4)]
        dgs = [mk(128, BF16, f"dg{j}") for j in range(31)]
        junk_mix = mk(1024, BF16, "junk_mix", at=dgs[0].lo)
        Y = [mk(512, F32, f"y{c}") for c in range(4)]
        sT = [mk(512, BF16, f"sT{c}") for c in range(4)]
        pexp = [mk(512, BF16, f"pexp{i}") for i in range(3)]
        pmsk = [mk(512, BF16, f"pmsk{i}") for i in range(3)]
        otok = [mk(512, BF16, f"otok{j}") for j in range(4)]
        oT = [mk(512, BF16, f"oT{c}") for c in range(4)]
        mg = [mk(512, BF16, f"mg{c}") for c in range(8)]
        xnb = [mk(1024, BF16, f"xnb{i}", at=otok[0].lo + i * 2048) for i in range(4)]
        MIX_END = top[0]

        top[0] = PH0
        h2T = [[mk(512, BF16, f"h2T{c}_{g}") for g in range(4)] for c in range(8)]
        H2LO = h2T[0][0].lo
        xnb2 = [mk(1024, BF16, f"xnb2_{i}") for i in range(4)]
        XNLO = xnb2[0].lo
        junk_moe = mk(1024, BF16, "junk_moe")
        wrt = mk(8 * 36, BF16, "wrt")
        OH1 = [mk(32, F32, f"oh1_{t}") for t in range(NT)]
        OH2 = [mk(32, F32, f"oh2_{t}") for t in range(NT)]
        ABF = [mk(32, BF16, f"abf{t}") for t in range(NT)]
        RK = [mk(32, F32, f"rk{t}") for t in range(NT)]
        G12 = [mk(2, F32, f"g12_{t}") for t in range(NT)]
        DF = [mk(2, F32, f"df{t}") for t in range(NT)]
        DI = [mk(2, mybir.dt.int32, f"di{t}") for t in range(NT)]
        DIS = [mk(2, mybir.dt.int32, f"dis{t}") for t in range(NT)]
        cst = mk(384, F32, "cst")
        np_t = mk(32, F32, "np_t")
        oend_t = mk(32, F32, "oend")
        ops_t = mk(32, F32, "ops")
        q_t = mk(32, F32, "q_t")
        ltri = mk(128, BF16, "ltri")
        onesb = mk(128, BF16, "onesb")
        cntn = mk(32, F32, "cntn")
        nn_t = mk(32, F32, "nn")
        end_t = mk(32, F32, "end")
        psb_t = mk(32, F32, "psb")
        ebacc = mk(64, F32, "ebacc")
        idxg = mk(64, mybir.dt.int32, "idxg")
        idxd = mk(64, mybir.dt.int32, "idxd")
        GT = mk(1024, BF16, "GT")
        xs_t = [mk(1024, BF16, f"xs{i}") for i in range(2)]
        xg_t = [mk(1024, BF16, f"xg{i}") for i in range(4)]
        xbT = [mk(1024, BF16, f"xbT{i}") for i in range(2)]
        hbt = [mk(512, BF16, f"hbt{i}") for i in range(2)]
        hbT = [mk(512, BF16, f"hbT{i}") for i in range(2)]
        yst = [mk(1024, F32, f"yst{i}") for i in range(2)]
        yst.append(mk(1024, F32, "yst2", at=xs_t[0].lo))
        yst.append(mk(1024, F32, "yst3", at=OH1[0].lo))
        PH1 = top[0]
        ew = [[mk(4096, BF16, f"ew{s}_{m}", at=(H2LO + m * 8192) if s == 0 else None) for m in range(3)] for s in range(2)]
        ycmb = [mk(1024, F32, f"ycmb{i}", at=XNLO + i * 4096) for i in range(2)]
        wpe = mk(2048, BF16, "wpe")
        pT = [mk(S, BF16, f"pT{i}") for i in range(2)]
        pst = [mk(256, F32, f"pst{i}") for i in range(2)]
        MOE_END = top[0]
        top[0] = PH1
        wpg = [mk(4096, BF16, f"wpg{i}") for i in range(2)]
        ost = [mk(1024, F32, f"ost{i}") for i in range(2)]
        nfin = mk(D, F32, "nfin")
        PLE_END = top[0]
        print("SBUF bytes/partition: mixer", MIX_END, "moe", MOE_END, "ple", PLE_END, "limit", NB)

        dram_tiles = {}
        _dn = [0]
        for t_ in range(NT):
            for a_ in range(2):
                _dn[0] += 1
                dram_tiles[("sc", t_, a_)] = P.tile(None, "sb", 10 ** 9 + 10 * _dn[0], 10 ** 9 + 10 * _dn[0] + 1, "scd")
        for b_ in range(92):
            _dn[0] += 1
            dram_tiles[("ys", b_)] = P.tile(None, "sb", 10 ** 9 + 10 * _dn[0], 10 ** 9 + 10 * _dn[0] + 1, "ysd")

        dram_tiles[('gser',)] = P.tile(None, 'sb', 2 * 10 ** 9, 2 * 10 ** 9 + 1, 'gser')
        P.op("sp", lambda e: e.dma_start(out=ident.ap, in_=id_d), writes=[ident], dkey="c0")
        P.op("sp", lambda e: e.dma_start(out=pp.ap, in_=pp_d), writes=[pp], dkey="c0")
        P.op("sp", lambda e: e.dma_start(out=bcp.ap, in_=bc_d[:, 0:72]), writes=[bcp], dkey="c0")
        P.op("pool", lambda e: e.dma_start(out=maskT.ap, in_=mask_d), writes=[maskT], dkey="c1")
        P.op("pool", lambda e: e.dma_start(out=identb.ap, in_=id_d), writes=[identb], dkey="c1")
        P.op("pool", lambda e: e.memset(ones512.ap, 1.0 / 512), writes=[ones512])
        for g in range(4):
            for i in range(4):
                t = 4 * g + i
                for h in range(2):
                    P.op("sp", lambda e, t=t, h=h: e.dma_start(out=X[t][h].ap, in_=x_d[t * 128:(t + 1) * 128, h * 512:(h + 1) * 512]),
                         writes=[X[t][h]], dkey=f"xg{g}")

        def ppc(col):
            return pp.ap[:, col:col + 1]

        def row_rstd(t, junk):
            ss = small()
            P.op("act", lambda e: e.activation(out=junk.ap, in_=xrow(t), func=AF.Square, accum_out=ss.ap[:, 0:1]),
                 reads=[X[t][0], X[t][1]], writes=[ss, junk])
            P.op("act", lambda e: e.activation(out=ss.ap[:, 1:2], in_=ss.ap[:, 0:1], func=AF.Sqrt, bias=EPS, scale=1.0 / D),
                 reads=[ss], writes=[ss])
            P.op("dve", lambda e: e.reciprocal(out=ss.ap[:, 2:3], in_=ss.ap[:, 1:2]), reads=[ss], writes=[ss])
            return ss

        def norm_T(g, gcol, dst, xn_tiles):
            for i in range(4):
                t = 4 * g + i
                ss = row_rstd(t, junk_mix if xn_tiles is xnb else junk_moe)
                P.op("dve", lambda e, t=t, i=i, ss=ss: e.tensor_scalar(out=xn_tiles[i].ap, in0=xrow(t), scalar1=ss.ap[:, 2:3],
                                                                       scalar2=None, op0=ALU.mult),
                     reads=[X[t][0], X[t][1], ss], writes=[xn_tiles[i]])
            for c in range(8):
                b = bank()
                bv = b.ap.bitcast(BF16)
                for i in range(4):
                    P.op("pe", lambda e, i=i, c=c, bv=bv: e.transpose(out=bv[:, i * 128:(i + 1) * 128],
                                                                      in_=xn_tiles[i].ap[:, c * 128:(c + 1) * 128], identity=identb.ap),
                         reads=[xn_tiles[i], identb], writes=[b])
                P.op("act", lambda e, c=c, bv=bv: e.activation(out=dst[c].ap, in_=bv[:, 0:512], func=AF.Copy, scale=ppc(gcol + c)),
                     reads=[b, pp], writes=[dst[c]])

        def mm_acc(out_ap, out_t, pairs, extra_reads):
            n = len(pairs)
            for k, (lh, rh) in enumerate(pairs):
                P.op("pe", lambda e, lh=lh, rh=rh, k=k: e.matmul(out_ap, lhsT=lh, rhs=rh, start=(k == 0), stop=(k == n - 1)),
                     reads=extra_reads, writes=[out_t])

        ring_i = [0]

        def wload(srcs, key_reads=()):
            ring_i[0] = (ring_i[0] + 1) % 6
            slot = ring[ring_i[0]]
            si = ring_i[0]
            views = []
            off = 0
            for (src, k, c) in srcs:
                v = slot.ap[:, off:off + k * c].rearrange("p (k c) -> p k c", k=k)
                views.append(v)
                P.op("pool", lambda e, v=v, src=src: e.dma_start(out=v, in_=src), writes=[slot], dkey=f"ring{si}")
                off += k * c
            return slot, views

        def mixer_group(l, g):
            pb = l * PPL
            norm_T(g, pb + 0, hT, xnb)
            gi = l * 4 + g
            cosg, sing = cos2[gi % 2], sin2[gi % 2]

            def cs_load(gi_):
                g_ = gi_ % 4
                P.op("sp", lambda e: e.dma_start(out=cos2[gi_ % 2].ap, in_=cos_d[:, g_ * 512:(g_ + 1) * 512]), writes=[cos2[gi_ % 2]], dkey=f"cs{gi_ % 2}")
                P.op("sp", lambda e: e.dma_start(out=sin2[gi_ % 2].ap, in_=sin_d[:, g_ * 512:(g_ + 1) * 512]), writes=[sin2[gi_ % 2]], dkey=f"cs{gi_ % 2}")
            if g == 0:
                cs_load(gi)
            if g < 3:
                cs_load(gi + 1)
            winl = win_d[l].rearrange("(k p) c -> p k c", p=128)
            waol = wao_d[l].rearrange("(k p) c -> p k c", p=128)
            wcol = wco_d[l].rearrange("(k p) c -> p k c", p=128)
            woutl = wout_d[l].rearrange("(k p) c -> p k c", p=128)

            def win_blk(c0):
                return [(winl[:, :, c0:c0 + 256], 8, 256)]

            tasks = []

            def rope_task(base, dst_fn):
                for b in range(2):
                    def comp(slots, b=b):
                        (s1, v1), (s2, v2) = slots
                        for cc in range(2):
                            c = 2 * b + cc
                            bq = bank()
                            mm_acc(bq.ap, bq, [(v1[0][:, k, cc * 128:(cc + 1) * 128], hT[k].ap) for k in range(8)], [s1] + hT)
                            bs = bank()
                            mm_acc(bs.ap, bs, [(v2[0][:, k, cc * 128:(cc + 1) * 128], hT[k].ap) for k in range(8)], [s2] + hT)
                            t1 = scratch()
                            P.op("dve", lambda e, t1=t1, bq=bq: e.tensor_tensor(out=t1.ap, in0=bq.ap, in1=cosg.ap, op=ALU.mult),
                                 reads=[bq, cosg], writes=[t1])
                            t2 = scratch()
                            P.op("dve", lambda e, t2=t2, bs=bs: e.tensor_tensor(out=t2.ap, in0=bs.ap, in1=sing.ap, op=ALU.mult),
                                 reads=[bs, sing], writes=[t2])
                            dst = dst_fn(c)
                            P.op("dve", lambda e, t1=t1, t2=t2, dst=dst: e.tensor_tensor(out=dst.ap, in0=t1.ap, in1=t2.ap, op=ALU.add),
                                 reads=[t1, t2], writes=[dst])
                    tasks.append(([win_blk(base + 256 * b), win_blk(base + 512 + 256 * b)], comp))
            rope_task(0, lambda c: qT[c])
            rope_task(1024, lambda c: kT[c][g])

            for b in range(2):
                def comp(slots, b=b):
                    (s1, v1), = slots
                    for i in range(4):
                        t = 4 * g + i
                        bv = bank()
                        mm_acc(bv.ap[:, 0:256], bv, [(hT[k].ap[:, i * 128:(i + 1) * 128], v1[0][:, k, :]) for k in range(8)], [s1] + hT)
                        vv = vA[t].ap.rearrange("p (h e) -> p h e", h=8)
                        if b == 0:
                            P.op("pool", lambda e, vv=vv: e.memset(vv[:, :, 64:65], 1.0), writes=[vA[t]])
                        P.op("act", lambda e, vv=vv, bv=bv, b=b: e.activation(
                            out=vv[:, 4 * b:4 * b + 4, 0:64], in_=bv.ap[:, 0:256].rearrange("p (h e) -> p h e", h=4), func=AF.Copy),
                            reads=[bv], writes=[vA[t]])
                tasks.append(([win_blk(2048 + 256 * b)], comp))

            for b in range(2):
                def comp(slots, b=b):
                    (s1, v1), (s2, v2) = slots
                    for cc in range(2):
                        c = 2 * b + cc
                        bu = bank()
                        mm_acc(bu.ap, bu, [(v1[0][:, k, cc * 128:(cc + 1) * 128], hT[k].ap) for k in range(8)], [s1] + hT)
                        bg = bank()
                        mm_acc(bg.ap, bg, [(v2[0][:, k, cc * 128:(cc + 1) * 128], hT[k].ap) for k in range(8)], [s2] + hT)
                        sg = scratch()
                        P.op("act", lambda e, sg=sg, bg=bg: e.activation(out=sg.ap, in_=bg.ap, func=AF.Sigmoid), reads=[bg], writes=[sg])
                        if g == 0:
                            P.op("pool", lambda e, c=c: e.memset(U[c].ap[:, 0:30], 0.0), writes=[U[c]])
                        else:
                            P.op("pool", lambda e, c=c: e.tensor_copy(out=U[c].ap[:, 0:30], in_=U[c].ap[:, 512:542]), reads=[U[c]], writes=[U[c]])
                        P.op("dve", lambda e, c=c, bu=bu, sg=sg: e.tensor_tensor(out=U[c].ap[:, 30:542], in0=bu.ap, in1=sg.ap, op=ALU.mult),
                             reads=[bu, sg], writes=[U[c]])
                        wc0 = pb + 44
                        for j in range(31):
                            if j % 2 == 0:
                                P.op("dve", lambda e, j=j, c=c: e.tensor_scalar(out=dgs[j].ap, in0=identb.ap, scalar1=ppc(wc0 + 4 * j + c), scalar2=None,
                                                                              op0=ALU.mult), reads=[identb, pp], writes=[dgs[j]])
                            else:
                                P.op("act", lambda e, j=j, c=c: e.activation(out=dgs[j].ap, in_=identb.ap, func=AF.Copy, scale=ppc(wc0 + 4 * j + c)),
                                     reads=[identb, pp], writes=[dgs[j]])
                        by = bank()
                        for j in range(31):
                            P.op("pe", lambda e, j=j, c=c, by=by: e.matmul(by.ap, lhsT=dgs[j].ap, rhs=U[c].ap[:, j:j + 512], start=(j == 0), stop=(j == 30)),
                                 reads=[dgs[j], U[c]], writes=[by])
                        P.op("dve", lambda e, c=c, by=by: e.tensor_scalar(out=Y[c].ap, in0=by.ap, scalar1=ppc(pb + 32 + c), scalar2=None, op0=ALU.add),
                             reads=[by, pp], writes=[Y[c]])
                tasks.append(([win_blk(2560 + 256 * b), win_blk(3072 + 256 * b)], comp))

            def comp_ln_attn(slots):
                bm = bank()
                mm_acc(bm.ap, bm, [(ones512.ap, Y[c].ap) for c in range(4)], [ones512] + Y)
                be = bank()
                for c in range(4):
                    sq = scratch()
                    P.op("act", lambda e, sq=sq, c=c: e.activation(out=sq.ap, in_=Y[c].ap, func=AF.Square), reads=[Y[c]], writes=[sq])
                    P.op("pe", lambda e, sq=sq, c=c: e.matmul(be.ap, lhsT=ones512.ap, rhs=sq.ap, start=(c == 0), stop=(c == 3)),
                         reads=[ones512, sq], writes=[be])
                msq = scratch()
                P.op("act", lambda e: e.activation(out=msq.ap, in_=bm.ap, func=AF.Square), reads=[bm], writes=[msq])
                var = scratch()
                P.op("dve", lambda e: e.tensor_tensor(out=var.ap, in0=be.ap, in1=msq.ap, op=ALU.subtract), reads=[be, msq], writes=[var])
                P.op("act", lambda e: e.activation(out=var.ap, in_=var.ap, func=AF.Sqrt, bias=EPS, scale=1.0), reads=[var], writes=[var])
                P.op("dve", lambda e: e.reciprocal(out=var.ap, in_=var.ap), reads=[var], writes=[var])
                for c in range(4):
                    yn = scratch()
                    P.op("dve", lambda e, yn=yn, c=c: e.tensor_tensor(out=yn.ap, in0=Y[c].ap, in1=bm.ap, op=ALU.subtract),
                         reads=[Y[c], bm], writes=[yn])
                    P.op("dve", lambda e, yn=yn: e.tensor_tensor(out=yn.ap, in0=yn.ap, in1=var.ap, op=ALU.mult), reads=[yn, var], writes=[yn])
                    P.op("act", lambda e, yn=yn, c=c: e.activation(out=sT[c].ap, in_=yn.ap, func=AF.Silu, scale=ppc(pb + 36 + c), bias=ppc(pb + 40 + c)),
                         reads=[yn, pp], writes=[sT[c]])
                nkt = 4 * g + 4
                items = [(h, kt) for h in range(8) for kt in range(nkt)]
                stg = {}

                def S_(n):
                    h, kt = items[n]
                    c, pbase = h // 2, (h % 2) * 64
                    j0 = max(kt - 4 * g, 0)
                    c0 = j0 * 128
                    bs = bank()
                    ktile = kT[c][kt // 4]
                    P.op("pe", lambda e: e.matmul(
                        bs.ap[:, c0:512], lhsT=ktile.ap[pbase:pbase + 64, (kt % 4) * 128:(kt % 4) * 128 + 128],
                        rhs=qT[c].ap[pbase:pbase + 64, c0:512], start=True, stop=True),
                        reads=[ktile, qT[c]], writes=[bs])
                    pe_t = pexp[n % 3]
                    P.op("act", lambda e: e.activation(out=pe_t.ap[:, c0:512], in_=bs.ap[:, c0:512], func=AF.Exp, scale=0.125),
                         reads=[bs], writes=[pe_t])
                    pm_t = pmsk[n % 3]
                    o0 = 4 * g + j0 - kt
                    P.op("dve", lambda e: e.tensor_tensor(
                        out=pm_t.ap[:, c0:512], in0=pe_t.ap[:, c0:512], in1=maskT.ap[:, o0 * 128:o0 * 128 + 512 - c0], op=ALU.mult),
                        reads=[pe_t, maskT], writes=[pm_t])
                    stg[n] = (pm_t, j0)

                def V_(n):
                    h, kt = items[n]
                    pm_t, j0 = stg.pop(n)
                    accb = ps[6 + (h % 2)]
                    acc = accb.ap[:, 0:260].rearrange("p (j e) -> p j e", j=4)
                    for j in range(j0, 4):
                        P.op("pe", lambda e, j=j: e.matmul(
                            acc[:, j, :], lhsT=pm_t.ap[:, j * 128:(j + 1) * 128], rhs=vA[kt].ap[:, h * 65:(h + 1) * 65],
                            start=(kt == 0 and j == 0), stop=(kt == nkt - 1 and j == 3)),
                            reads=[pm_t, vA[kt]], writes=[accb])
                    if kt == nkt - 1:
                        rc = small()
                        P.op("dve", lambda e: e.reciprocal(out=rc.ap[:, 0:4], in_=acc[:, :, 64]), reads=[accb], writes=[rc])
                        for j in range(4):
                            P.op("dve", lambda e, j=j: e.tensor_scalar(
                                out=otok[j].ap[:, h * 64:(h + 1) * 64], in0=acc[:, j, 0:64], scalar1=rc.ap[:, j:j + 1], scalar2=None, op0=ALU.mult),
                                reads=[accb, rc], writes=[otok[j]])
                LA = 2
                for n in range(min(LA, len(items))):
                    S_(n)
                for n in range(len(items)):
                    if n + LA < len(items):
                        S_(n + LA)
                    V_(n)
                for c in range(4):
                    b = bank()
                    bv = b.ap.bitcast(BF16)
                    for j in range(4):
                        P.op("pe", lambda e, bv=bv, j=j, c=c: e.transpose(out=bv[:, j * 128:(j + 1) * 128], in_=otok[j].ap[:, c * 128:(c + 1) * 128],
                                                                          identity=identb.ap), reads=[otok[j], identb], writes=[b])
                    P.op("act", lambda e, bv=bv, c=c: e.activation(out=oT[c].ap, in_=bv[:, 0:512], func=AF.Copy), reads=[b], writes=[oT[c]])
            tasks.append(([], comp_ln_attn))

            for ob in range(4):
                def comp(slots, ob=ob):
                    (s1, v1), (s2, v2), (s3, v3) = slots
                    for cc in range(2):
                        oc = 2 * ob + cc
                        sl = slice(cc * 128, (cc + 1) * 128)
                        bga = bank()
                        mm_acc(bga.ap, bga, [(v1[0][:, k, sl], hT[k].ap) for k in range(8)], [s1] + hT)
                        bgb = bank()
                        mm_acc(bgb.ap, bgb, [(v2[0][:, k, sl], hT[k].ap) for k in range(8)], [s2] + hT)
                        bya = bank()
                        mm_acc(bya.ap, bya, [(v3[0][:, k, sl], oT[k].ap) for k in range(4)], [s3] + oT)
                        byb = bank()
                        mm_acc(byb.ap, byb, [(v3[1][:, k, sl], sT[k].ap) for k in range(4)], [s3] + sT)
                        sga = scratch()
                        P.op("act", lambda e, sga=sga, bga=bga: e.activation(out=sga.ap, in_=bga.ap, func=AF.Sigmoid), reads=[bga], writes=[sga])
                        sgb = scratch()
                        P.op("act", lambda e, sgb=sgb, bgb=bgb: e.activation(out=sgb.ap, in_=bgb.ap, func=AF.Sigmoid), reads=[bgb], writes=[sgb])
                        P.op("dve", lambda e, sga=sga, bya=bya: e.tensor_tensor(out=sga.ap, in0=bya.ap, in1=sga.ap, op=ALU.mult),
                             reads=[bya, sga], writes=[sga])
                        P.op("dve", lambda e, sgb=sgb, byb=byb, oc=oc: e.scalar_tensor_tensor(out=sgb.ap, in0=byb.ap, scalar=ppc(pb + 24 + oc), in1=sgb.ap,
                                                                                            op0=ALU.add, op1=ALU.mult),
                             reads=[byb, sgb, pp], writes=[sgb])
                        P.op("dve", lambda e, sga=sga, sgb=sgb, oc=oc: e.tensor_tensor(out=mg[oc].ap, in0=sga.ap, in1=sgb.ap, op=ALU.add),
                             reads=[sga, sgb], writes=[mg[oc]])
                tasks.append(([win_blk(3584 + 256 * ob), win_blk(4608 + 256 * ob),
                               [(waol[:, :, 256 * ob:256 * ob + 256], 4, 256), (wcol[:, :, 256 * ob:256 * ob + 256], 4, 256)]], comp))

            for nb_ in range(4):
                def comp(slots, nb_=nb_):
                    (s1, v1), = slots
                    for i in range(4):
                        t = 4 * g + i
                        b = bank()
                        mm_acc(b.ap[:, 0:256], b, [(mg[k].ap[:, i * 128:(i + 1) * 128], v1[0][:, k, :]) for k in range(8)], [s1] + mg)
                        xt = X[t][nb_ // 2]
                        xs = xt.ap[:, (nb_ % 2) * 256:(nb_ % 2) * 256 + 256]
                        P.op("dve", lambda e, xs=xs, b=b: e.tensor_tensor(out=xs, in0=b.ap[:, 0:256], in1=xs, op=ALU.add),
                             reads=[b, xt], writes=[xt])
                tasks.append(([[(woutl[:, :, 256 * nb_:256 * nb_ + 256], 8, 256)]], comp))

            return tasks

        def moe_phase(l):
            pb = l * PPL
            I32 = mybir.dt.int32
            P.op("sp", lambda e: e.dma_start(out=cst.ap, in_=cst_d), writes=[cst], dkey="cst")
            P.op("pool", lambda e: e.dma_start(out=ltri.ap, in_=cst_d[:, 192:320]), writes=[ltri], dkey="cstb")
            P.op("pool", lambda e: e.memset(onesb.ap, 1.0), writes=[onesb])
            for g in range(4):
                norm_T(g, pb + 8, [h2T[c][g] for c in range(8)], xnb2)
            P.op("pool", lambda e: e.dma_start(out=wrt.ap.rearrange("p (k c) -> p k c", k=8), in_=wr_d[l].rearrange("(k p) c -> p k c", p=128)),
                 writes=[wrt], dkey="wr")
            wrv = wrt.ap.rearrange("p (k c) -> p k c", k=8)

            def route_tile(t):
                g, i = t // 4, t % 4
                b = bank()
                mm_acc(b.ap[:, 0:36], b, [(h2T[k][g].ap[:, i * 128:(i + 1) * 128], wrv[:, k, :]) for k in range(8)],
                       [wrt] + [h2T[k][g] for k in range(8)])
                lg = small()
                P.op("dve", lambda e, lg=lg, b=b: e.tensor_tensor(out=lg.ap, in0=b.ap[:, 0:36], in1=bcp.ap[:, l * 36:(l + 1) * 36], op=ALU.add),
                     reads=[b, bcp], writes=[lg])
                w1 = small()
                W = w1.ap
                P.op("dve", lambda e, lg=lg, W=W: e.tensor_reduce(out=W[:, 0:1], in_=lg.ap[:, 0:4], axis=AX.X, op=ALU.max), reads=[lg], writes=[w1])
                P.op("dve", lambda e, W=W: e.tensor_scalar(out=W[:, 1:2], in0=W[:, 0:1], scalar1=-1.0, scalar2=None, op0=ALU.mult), reads=[w1], writes=[w1])
                P.op("act", lambda e, lg=lg, W=W: e.activation(out=W[:, 8:12], in_=lg.ap[:, 0:4], func=AF.Exp, bias=W[:, 1:2], scale=1.0, accum_out=W[:, 2:3]),
                     reads=[lg, w1], writes=[w1])
                P.op("dve", lambda e, W=W: e.reciprocal(out=W[:, 3:4], in_=W[:, 2:3]), reads=[w1], writes=[w1])
                P.op("dve", lambda e, lg=lg, W=W: e.tensor_scalar(out=W[:, 4:8], in0=lg.ap[:, 0:4], scalar1=W[:, 0:1], scalar2=None, op0=ALU.is_equal),
                     reads=[lg, w1], writes=[w1])
                P.op("dve", lambda e, W=W: e.tensor_scalar(out=W[:, 4:8], in0=W[:, 4:8], scalar1=BIG, scalar2=-BIG, op0=ALU.mult, op1=ALU.add),
                     reads=[w1], writes=[w1])
                lem = small()
                for gg in range(4):
                    P.op("dve", lambda e, lem=lem, lg=lg, W=W, gg=gg: e.tensor_scalar(out=lem.ap[:, gg * 8:(gg + 1) * 8], in0=lg.ap[:, 4 + gg * 8:12 + gg * 8],
                                                                                  scalar1=W[:, 4 + gg:5 + gg], scalar2=None, op0=ALU.add),
                         reads=[lg, w1], writes=[lem])
                w2 = small()
                V = w2.ap
                oh1, oh2 = OH1[t], OH2[t]
                lem2 = small()
                P.op("dve", lambda e, lem=lem, V=V: e.tensor_reduce(out=V[:, 0:1], in_=lem.ap[:, 0:32], axis=AX.X, op=ALU.max), reads=[lem], writes=[w2])
                P.op("dve", lambda e, lem=lem, V=V, oh1=oh1: e.tensor_scalar(out=oh1.ap, in0=lem.ap[:, 0:32], scalar1=V[:, 0:1], scalar2=None, op0=ALU.is_equal),
                     reads=[lem, w2], writes=[oh1])
                P.op("dve", lambda e, lem=lem, lem2=lem2, oh1=oh1: e.scalar_tensor_tensor(out=lem2.ap[:, 0:32], in0=oh1.ap, scalar=-BIG, in1=lem.ap[:, 0:32],
                                                                                  op0=ALU.mult, op1=ALU.add), reads=[oh1, lem], writes=[lem2])
                P.op("dve", lambda e, lem2=lem2, V=V: e.tensor_reduce(out=V[:, 1:2], in_=lem2.ap[:, 0:32], axis=AX.X, op=ALU.max), reads=[lem2], writes=[w2])
                P.op("dve", lambda e, lem2=lem2, V=V, oh2=oh2: e.tensor_scalar(out=oh2.ap, in0=lem2.ap[:, 0:32], scalar1=V[:, 1:2], scalar2=None, op0=ALU.is_equal),
                     reads=[lem2, w2], writes=[oh2])
                P.op("dve", lambda e, V=V: e.tensor_tensor(out=V[:, 2:3], in0=V[:, 1:2], in1=V[:, 0:1], op=ALU.subtract), reads=[w2], writes=[w2])
                P.op("act", lambda e, V=V: e.activation(out=V[:, 3:4], in_=V[:, 2:3], func=AF.Exp), reads=[w2], writes=[w2])
                P.op("dve", lambda e, V=V: e.tensor_scalar(out=V[:, 4:5], in0=V[:, 3:4], scalar1=1.0, scalar2=None, op0=ALU.add), reads=[w2], writes=[w2])
                P.op("dve", lambda e, V=V: e.reciprocal(out=V[:, 5:6], in_=V[:, 4:5]), reads=[w2], writes=[w2])
                P.op("dve", lambda e, V=V: e.tensor_tensor(out=V[:, 6:7], in0=V[:, 3:4], in1=V[:, 5:6], op=ALU.mult), reads=[w2], writes=[w2])
                P.op("dve", lambda e, V=V, W=W, t=t: e.tensor_scalar(out=G12[t].ap, in0=V[:, 5:7], scalar1=W[:, 3:4], scalar2=None, op0=ALU.mult),
                     reads=[w2, w1], writes=[G12[t]])
                P.op("dve", lambda e, oh1=oh1, oh2=oh2, t=t: e.tensor_tensor(out=ABF[t].ap, in0=oh1.ap, in1=oh2.ap, op=ALU.add),
                     reads=[oh1, oh2], writes=[ABF[t]])

            def capture(fn, *a):
                rec = []
                P.op = lambda *args, **kw: rec.append((args, kw))
                try:
                    fn(*a)
                finally:
                    del P.op
                return rec
            RB = 3
            for t0 in range(0, NT, RB):
                recs = [capture(route_tile, t) for t in range(t0, min(t0 + RB, NT))]
                for k_ in range(max(len(r) for r in recs)):
                    for r in recs:
                        if k_ < len(r):
                            P.op(*r[k_][0], **r[k_][1])

            for t in range(NT):
                b = bank()
                for tp in range(t):
                    P.op("pe", lambda e, b=b, tp=tp: e.matmul(b.ap[:, 0:32], lhsT=onesb.ap, rhs=ABF[tp].ap, start=(tp == 0), stop=False),
                         reads=[onesb, ABF[tp]], writes=[b])
                P.op("pe", lambda e, b=b, t=t: e.matmul(b.ap[:, 0:32], lhsT=ltri.ap, rhs=ABF[t].ap, start=(t == 0), stop=True),
                     reads=[ltri, ABF[t]], writes=[b])
                P.op("act", lambda e, b=b, t=t: e.activation(out=RK[t].ap, in_=b.ap[:, 0:32], func=AF.Copy), reads=[b], writes=[RK[t]])
            bc_ = bank()
            for t in range(NT):
                P.op("pe", lambda e, t=t: e.matmul(bc_.ap[:, 0:32], lhsT=onesb.ap, rhs=ABF[t].ap, start=(t == 0), stop=(t == NT - 1)),
                     reads=[onesb, ABF[t]], writes=[bc_])
            P.op("act", lambda e: e.activation(out=cntn.ap, in_=bc_.ap[:, 0:32], func=AF.Copy), reads=[bc_], writes=[cntn])
            P.op("dve", lambda e: e.tensor_scalar(out=nn_t.ap, in0=cntn.ap, scalar1=0.0, scalar2=None, op0=ALU.is_gt), reads=[cntn], writes=[nn_t])
            for j in range(1, 16):
                P.op("dve", lambda e, j=j: e.scalar_tensor_tensor(out=nn_t.ap, in0=cntn.ap, scalar=128.0 * j, in1=nn_t.ap, op0=ALU.is_gt, op1=ALU.add),
                     reads=[cntn, nn_t], writes=[nn_t])
            P.op("dve", lambda e: e.tensor_scalar(out=np_t.ap, in0=nn_t.ap, scalar1=-2.0, scalar2=0.0, op0=ALU.add, op1=ALU.max), reads=[nn_t], writes=[np_t])
            P.op("dve", lambda e: e.tensor_copy(out=oend_t.ap[:, 0:1], in_=np_t.ap[:, 0:1]), reads=[np_t], writes=[oend_t])
            for ee in range(1, NE):
                P.op("dve", lambda e, ee=ee: e.tensor_tensor(out=oend_t.ap[:, ee:ee + 1], in0=oend_t.ap[:, ee - 1:ee], in1=np_t.ap[:, ee:ee + 1], op=ALU.add),
                     reads=[oend_t, np_t], writes=[oend_t])
            P.op("dve", lambda e: e.tensor_tensor(out=ops_t.ap, in0=oend_t.ap, in1=np_t.ap, op=ALU.subtract), reads=[oend_t, np_t], writes=[ops_t])
            P.op("dve", lambda e: e.scalar_tensor_tensor(out=q_t.ap, in0=ops_t.ap, scalar=128.0, in1=cst.ap[:, 352:384], op0=ALU.mult, op1=ALU.add),
                 reads=[ops_t, cst], writes=[q_t])
            P.op("dve", lambda e: e.tensor_scalar(out=ebacc.ap, in0=cst.ap[:, 0:64], scalar1=oend_t.ap[:, 0:1], scalar2=None, op0=ALU.is_ge),
                 reads=[cst, oend_t], writes=[ebacc])
            for ee in range(1, NE):
                P.op("dve", lambda e, ee=ee: e.scalar_tensor_tensor(out=ebacc.ap, in0=cst.ap[:, 0:64], scalar=oend_t.ap[:, ee:ee + 1], in1=ebacc.ap,
                                                                  op0=ALU.is_ge, op1=ALU.add), reads=[cst, oend_t, ebacc], writes=[ebacc])
            P.op("dve", lambda e: e.scalar_tensor_tensor(out=idxg.ap, in0=ebacc.ap, scalar=256.0, in1=cst.ap[:, 64:128], op0=ALU.mult, op1=ALU.add),
                 reads=[ebacc, cst], writes=[idxg])
            P.op("dve", lambda e: e.scalar_tensor_tensor(out=idxd.ap, in0=ebacc.ap, scalar=256.0, in1=cst.ap[:, 128:192], op0=ALU.mult, op1=ALU.add),
                 reads=[ebacc, cst], writes=[idxd])
            if _DBG == 5:
                P.op("dve", lambda e: e.tensor_copy(out=idxg.ap, in_=cst.ap[:, 64:128]), reads=[cst], writes=[idxg])
                P.op("dve", lambda e: e.tensor_copy(out=idxd.ap, in_=cst.ap[:, 128:192]), reads=[cst], writes=[idxd])
            for t in range(NT):
                tmp = small()
                sel = small()
                P.op("dve", lambda e, sel=sel, t=t: e.scalar_tensor_tensor(out=sel.ap[:, 0:32], in0=RK[t].ap, scalar=256.0, in1=q_t.ap, op0=ALU.is_ge, op1=ALU.mult),
                     reads=[RK[t], q_t], writes=[sel])
                P.op("dve", lambda e, tmp=tmp, t=t: e.tensor_tensor(out=tmp.ap[:, 0:32], in0=RK[t].ap, in1=cst.ap[:, 320:352], op=ALU.add),
                     reads=[RK[t], cst], writes=[tmp])
                P.op("dve", lambda e, tmp=tmp, sel=sel: e.tensor_tensor(out=tmp.ap[:, 0:32], in0=tmp.ap[:, 0:32], in1=sel.ap[:, 0:32], op=ALU.add),
                     reads=[tmp, sel], writes=[tmp])
                for a_, oh in enumerate((OH1[t], OH2[t])):
                    m_ = small()
                    P.op("dve", lambda e, m_=m_, tmp=tmp, oh=oh: e.tensor_tensor(out=m_.ap[:, 0:32], in0=tmp.ap[:, 0:32], in1=oh.ap, op=ALU.mult),
                         reads=[tmp, oh], writes=[m_])
                    P.op("dve", lambda e, m_=m_, t=t, a_=a_: e.tensor_reduce(out=DF[t].ap[:, a_:a_ + 1], in_=m_.ap[:, 0:32], axis=AX.X, op=ALU.add),
                         reads=[m_], writes=[DF[t]])
                P.op("dve", lambda e, t=t: e.tensor_copy(out=DI[t].ap, in_=DF[t].ap), reads=[DF[t]], writes=[DI[t]])
                P.op("dve", lambda e, t=t: e.tensor_scalar(out=DIS[t].ap, in0=DF[t].ap, scalar1=24576.0, scalar2=None, op0=ALU.add), reads=[DF[t]], writes=[DIS[t]])
            for k in range(8):
                P.op("pool", lambda e, k=k: e.tensor_scalar(out=GT.ap[:, k * 128:(k + 1) * 128], in0=onesb.ap, scalar1=ppc(pb + 168 + k), scalar2=None, op0=ALU.mult),
                     reads=[onesb, pp], writes=[GT])
            if _DBG == 6:
                dbt = scr[0]
                P.op("dve", lambda e: e.tensor_copy(out=dbt.ap[:, 0:32], in_=cntn.ap), reads=[cntn], writes=[dbt])
                P.op("dve", lambda e: e.tensor_copy(out=dbt.ap[:, 32:64], in_=nn_t.ap), reads=[nn_t], writes=[dbt])
                P.op("dve", lambda e: e.tensor_copy(out=dbt.ap[:, 64:96], in_=end_t.ap), reads=[end_t], writes=[dbt])
                P.op("dve", lambda e: e.tensor_copy(out=dbt.ap[:, 96:128], in_=psb_t.ap), reads=[psb_t], writes=[dbt])
                P.op("dve", lambda e: e.tensor_copy(out=dbt.ap[:, 128:192], in_=ebacc.ap), reads=[ebacc], writes=[dbt])
                P.op("dve", lambda e: e.tensor_copy(out=dbt.ap[:, 192:256], in_=idxg.ap), reads=[idxg], writes=[dbt])
                P.op("dve", lambda e: e.tensor_copy(out=dbt.ap[:, 256:258], in_=DF[0].ap), reads=[DF[0]], writes=[dbt])
                P.op("dve", lambda e: e.tensor_copy(out=dbt.ap[:, 258:260], in_=DI[0].ap), reads=[DI[0]], writes=[dbt])
                P.op("dve", lambda e: e.tensor_copy(out=dbt.ap[:, 260:292], in_=RK[1].ap), reads=[RK[1]], writes=[dbt])
                P.op("dve", lambda e: e.tensor_copy(out=dbt.ap[:, 292:324], in_=OH1[0].ap), reads=[OH1[0]], writes=[dbt])
                P.op("sp", lambda e: e.dma_start(out=dbg_d, in_=dbt.ap), reads=[dbt], dkey="dbg")
                ple_prep(l)
                return
            if _DBG == 1:
                ple_prep(l)
                return
            sc_tiles = []
            for t in range(NT):
                ss = row_rstd(t, junk_moe)
                xs = xs_t[t % 2]
                P.op("dve", lambda e, t=t, ss=ss, xs=xs: e.tensor_scalar(out=xs.ap, in0=xrow(t), scalar1=ss.ap[:, 2:3], scalar2=None, op0=ALU.mult),
                     reads=[X[t][0], X[t][1], ss], writes=[xs])
                for a_ in range(2):
                    dt_ = dram_tiles[("sc", t, a_)]
                    P.op("pool", lambda e, t=t, a_=a_, xs=xs: e.indirect_dma_start(
                        out=sorted_d, out_offset=bass.IndirectOffsetOnAxis(ap=DIS[t].ap[:, a_:a_ + 1], axis=0),
                        in_=xs.ap, in_offset=None, bounds_check=P.regs['b36351'], oob_is_err=False),
                        reads=[xs, DIS[t]], writes=[dt_], dkey=f"sc{t % 2}")
                    sc_tiles.append(dt_)
            ple_prep(l)
            if _DBG == 2:
                return

            gser = dram_tiles[('gser',)]

            def eload(b_, mats):
                s_ = b_ % 2
                srcs = (weg_d[l], weu_d[l], wed_d[l])
                for m_ in mats:
                    src = srcs[m_]
                    for h_, idt in enumerate((idxg, idxd)):
                        P.op("pool", lambda e, m_=m_, src=src, h_=h_, idt=idt: e.indirect_dma_start(
                            out=ew[s_][m_].ap[:, h_ * 2048:(h_ + 1) * 2048], out_offset=None, in_=src[:, :],
                            in_offset=bass.IndirectOffsetOnAxis(ap=idt.ap[:, b_:b_ + 1], axis=0),
                            bounds_check=P.regs['b8191'], oob_is_err=False),
                            reads=[idt], writes=[ew[s_][m_]], dkey=f"ew{s_}_{m_}")

            ys_tiles = []

            def views(s_):
                return (ew[s_][0].ap.rearrange("p (k c) -> p k c", k=8), ew[s_][1].ap.rearrange("p (k c) -> p k c", k=8),
                        ew[s_][2].ap.rearrange("p (k c) -> p k c", k=4))

            def stA_load(b_):
                xg = xg_t[b_ % 4]
                P.op("sp", lambda e: e.dma_start(out=xg.ap, in_=sorted_d[24576 + b_ * 128:24576 + (b_ + 1) * 128, :]), reads=sc_tiles, writes=[xg], dkey=f"xgl{b_ % 4}")

            def stA(b_):
                x2 = b_ % 2
                xg = xg_t[b_ % 4]
                bt = bank()
                btv = bt.ap.bitcast(BF16)
                xgv = xg.ap.rearrange("p (a k) -> p a k", k=8)
                for k in range(8):
                    P.op("pe", lambda e, k=k: e.transpose(out=btv[:, k * 128:(k + 1) * 128], in_=xgv[:, :, k], identity=identb.ap),
                         reads=[xg, identb], writes=[bt])
                xb = xbT[x2]
                P.op("dve", lambda e: e.tensor_tensor(out=xb.ap, in0=btv[:, 0:1024], in1=GT.ap, op=ALU.mult), reads=[bt, GT], writes=[xb])

            def stB(b_, s_):
                x2 = b_ % 2
                vg, vu, vd = views(s_)
                xb = xbT[x2]
                bg = bank()
                mm_acc(bg.ap, bg, [(xb.ap[:, k * 128:(k + 1) * 128], vg[:, k, :]) for k in range(8)], [xb, ew[s_][0]])
                bu = bank()
                mm_acc(bu.ap, bu, [(xb.ap[:, k * 128:(k + 1) * 128], vu[:, k, :]) for k in range(8)], [xb, ew[s_][1]])
                sg = scratch()
                P.op("act", lambda e: e.activation(out=sg.ap, in_=bg.ap, func=AF.Silu), reads=[bg], writes=[sg])
                hb_ = hbt[x2]
                P.op("dve", lambda e: e.tensor_tensor(out=hb_.ap, in0=bu.ap, in1=sg.ap, op=ALU.mult), reads=[bu, sg], writes=[hb_])

            def stT(b_):
                x2 = b_ % 2
                hb_ = hbt[x2]
                bh = bank()
                bhv = bh.ap.bitcast(BF16)
                for f in range(4):
                    P.op("pe", lambda e, f=f: e.transpose(out=bhv[:, f * 128:(f + 1) * 128], in_=hb_.ap[:, f * 128:(f + 1) * 128], identity=identb.ap),
                         reads=[hb_, identb], writes=[bh])
                hT_ = hbT[x2]
                P.op("act", lambda e: e.activation(out=hT_.ap, in_=bhv[:, 0:512], func=AF.Copy), reads=[bh], writes=[hT_])

            def stC(b_, s_):
                x2 = b_ % 2
                vg, vu, vd = views(s_)
                hT_ = hbT[x2]
                yo = yst[b_ % 4]
                for hh in range(2):
                    bd = bank()
                    mm_acc(bd.ap, bd, [(hT_.ap[:, f * 128:(f + 1) * 128], vd[:, f, hh * 512:(hh + 1) * 512]) for f in range(4)], [hT_, ew[s_][2]])
                    if hh == 0:
                        P.op("act", lambda e, bd=bd: e.activation(out=yo.ap[:, 0:512], in_=bd.ap, func=AF.Copy), reads=[bd], writes=[yo])
                    else:
                        P.op("dve", lambda e, bd=bd: e.tensor_copy(out=yo.ap[:, 512:1024], in_=bd.ap), reads=[bd], writes=[yo])
                yt_ = dram_tiles[("ys", b_)]
                P.op("sp", lambda e: e.dma_start(out=ys_d[b_ * 128:(b_ + 1) * 128, :], in_=yo.ap), reads=[yo], writes=[yt_], dkey=f"yst{b_ % 4}")
                ys_tiles.append(yt_)

            def sload(e_, mats):
                s_ = e_ % 2
                srcs = (weg_d[l], weu_d[l], wed_d[l])
                for m_ in mats:
                    src = srcs[m_]
                    P.op("pool", lambda e, m_=m_, src=src: e.dma_start(
                        out=ew[s_][m_].ap.rearrange("p (h c) -> p h c", h=2), in_=src[256 * e_:256 * (e_ + 1), :].rearrange("(p h) c -> p h c", h=2)),
                        writes=[ew[s_][m_]], dkey=f"ew{s_}_{m_}")

            NOVF = 28
            NB_ALL = 64 + NOVF

            def slot_of(b_):
                return (b_ // 2) % 2 if b_ < 64 else (b_ - 64) % 2

            for i in range(-6, NB_ALL):
                for e_ in range(NE):
                    if i == 2 * e_ - 4:
                        sload(e_, (0, 1))
                    if i == 2 * e_ - 2:
                        sload(e_, (2,))
                for o_ in range(NOVF):
                    b_ = 64 + o_
                    if i == b_ - 3:
                        eload(o_, (0, 1))
                    if i == b_ - 1:
                        eload(o_, (2,))
                if 0 <= i + 6 < NB_ALL:
                    stA_load(i + 6)
                if 0 <= i + 3 < NB_ALL:
                    stA(i + 3)
                if 0 <= i + 2 < NB_ALL:
                    stB(i + 2, slot_of(i + 2))
                if 0 <= i + 1 < NB_ALL:
                    stT(i + 1)
                if 0 <= i < NB_ALL:
                    stC(i, slot_of(i))
            if _DBG in (3, 4, 5):
                return
            cbufs = [ycmb[0], ycmb[1], yst[0], yst[1], yst[2], yst[3]]
            for t in range(NT):
                for a_ in range(2):
                    ci = (2 * t + a_) % 6
                    yc = cbufs[ci]
                    P.op("pool", lambda e, t=t, a_=a_, yc=yc: e.indirect_dma_start(
                        out=yc.ap, out_offset=None, in_=ys_d[:, :], in_offset=bass.IndirectOffsetOnAxis(ap=DI[t].ap[:, a_:a_ + 1], axis=0),
                        bounds_check=P.regs['b11775'], oob_is_err=False),
                        reads=ys_tiles + [DI[t]], writes=[yc], dkey=f"yc{ci}")
                    for hh in range(2):
                        xt = X[t][hh]
                        P.op("dve", lambda e, t=t, a_=a_, yc=yc, hh=hh, xt=xt: e.scalar_tensor_tensor(
                            out=xt.ap, in0=yc.ap[:, hh * 512:(hh + 1) * 512], scalar=G12[t].ap[:, a_:a_ + 1], in1=xt.ap, op0=ALU.mult, op1=ALU.add),
                            reads=[yc, G12[t], xt], writes=[xt])

        def ple_prep(l):
            P.op("pool", lambda e: e.dma_start(out=wpe.ap.rearrange("p (k c) -> p k c", k=2), in_=wpe_d[l].rearrange("(k p) c -> p k c", p=128)),
                 writes=[wpe], dkey="wpe")
            for t in range(NT):
                stt = pst[t % 2]
                P.op("sp", lambda e, t=t, stt=stt: e.dma_start(out=stt.ap, in_=p_d[l, t * 128:(t + 1) * 128, :]), writes=[stt], dkey=f"pst{t % 2}")
                b = bank()
                for kc in range(2):
                    P.op("pe", lambda e, b=b, kc=kc, stt=stt: e.transpose(out=b.ap[:, kc * 128:(kc + 1) * 128], in_=stt.ap[:, kc * 128:(kc + 1) * 128], identity=ident.ap),
                         reads=[stt, ident], writes=[b])
                for kc in range(2):
                    P.op("act", lambda e, b=b, kc=kc, t=t: e.activation(out=pT[kc].ap[:, t * 128:(t + 1) * 128], in_=b.ap[:, kc * 128:(kc + 1) * 128], func=AF.Copy),
                         reads=[b], writes=[pT[kc]])

        def ple_phase(l):
            pb = l * PPL
            wpgl = wpg_d[l].rearrange("(k p) c -> p k c", p=128)
            for i in range(2):
                P.op("pool", lambda e, i=i: e.dma_start(out=wpg[i].ap.rearrange("p (k c) -> p k c", k=8), in_=wpgl[:, :, i * 512:(i + 1) * 512]),
                     writes=[wpg[i]], dkey="wp")
            wpev = wpe.ap.rearrange("p (k c) -> p k c", k=2)

            def ple_main(g_):
                for t in range(4 * g_, 4 * g_ + 4):
                    ple_tile(t)

            def ple_tile(t):
                g, i = t // 4, t % 4
                for hh in range(2):
                    wv = wpg[hh].ap.rearrange("p (k c) -> p k c", k=8)
                    bg = bank()
                    mm_acc(bg.ap, bg, [(h2T[k][g].ap[:, i * 128:(i + 1) * 128], wv[:, k, :]) for k in range(8)], [wpg[hh]] + [h2T[k][g] for k in range(8)])
                    be = bank()
                    mm_acc(be.ap, be, [(pT[kc].ap[:, t * 128:(t + 1) * 128], wpev[:, kc, hh * 512:(hh + 1) * 512]) for kc in range(2)], [wpe] + pT)
                    sg = scratch()
                    P.op("act", lambda e, sg=sg, bg=bg: e.activation(out=sg.ap, in_=bg.ap, func=AF.Sigmoid), reads=[bg], writes=[sg])
                    P.op("dve", lambda e, sg=sg, be=be: e.tensor_tensor(out=sg.ap, in0=be.ap, in1=sg.ap, op=ALU.mult), reads=[be, sg], writes=[sg])
                    xt = X[t][hh]
                    P.op("dve", lambda e, sg=sg, xt=xt: e.tensor_tensor(out=xt.ap, in0=xt.ap, in1=sg.ap, op=ALU.add), reads=[sg, xt], writes=[xt])

            norm_T(0, pb + 16, [h2T[c][0] for c in range(8)], xnb2)
            for g in range(4):
                if g + 1 < 4:
                    norm_T(g + 1, pb + 16, [h2T[c][g + 1] for c in range(8)], xnb2)
                ple_main(g)

        def final_phase():
            P.op("sp", lambda e: e.dma_start(out=nfin.ap, in_=bc_d[:, 72:72 + D]), writes=[nfin], dkey="nf")
            for t in range(NT):
                ss = row_rstd(t, junk_moe)
                o = ost[t % 2]
                P.op("dve", lambda e, t=t, ss=ss, o=o: e.scalar_tensor_tensor(out=o.ap, in0=xrow(t), scalar=ss.ap[:, 2:3], in1=nfin.ap, op0=ALU.mult, op1=ALU.mult),
                     reads=[X[t][0], X[t][1], ss, nfin], writes=[o])
                P.op("sp", lambda e, t=t, o=o: e.dma_start(out=out_d[t * 128:(t + 1) * 128, :], in_=o.ap), reads=[o], dkey=f"ost{t % 2}")
            for i in range(2):
                P.op("sp", lambda e: e.nop(), writes=[ost[i]])

        for l in range(nl):
            for g in range(4):
                tasks = mixer_group(l, g)
                loaded = {}
                n = len(tasks)
                nxt_load = 0
                in_use = 0
                for ti in range(n):
                    while nxt_load < n and (nxt_load <= ti or in_use + len(tasks[nxt_load][0]) <= 6):
                        loaded[nxt_load] = [wload(srcs) for srcs in tasks[nxt_load][0]]
                        in_use += len(tasks[nxt_load][0])
                        nxt_load += 1
                    tasks[ti][1](loaded.pop(ti))
                    in_use -= len(tasks[ti][0])
            nbank[0] = 8
            moe_phase(l)
            ple_phase(l)
            nbank[0] = 6
        final_phase()
        P.emit()
        nops = {k: len(v) for k, v in P.ops.items()}
        print("ops per engine:", nops)
    return nc


def _host_layout(inp):
    f = np.float32
    w_in = np.asarray(inp["w_in"], f)
    swap = np.arange(512).reshape(8, 64)
    swap = np.concatenate([swap[:, 32:], swap[:, :32]], axis=1).reshape(-1)
    q, k, v = w_in[:, :, 0:512], w_in[:, :, 512:1024], w_in[:, :, 1024:1536]
    rest = w_in[:, :, 1536:]
    win = np.ascontiguousarray(np.concatenate([q, q[:, :, swap], k, k[:, :, swap], v, rest], axis=2))
    assert win.shape[2] == WIN_EXT
    wr = np.ascontiguousarray(np.concatenate([np.asarray(inp["w_route_group"], f), np.asarray(inp["w_route_expert"], f)], axis=2))
    pp = np.zeros((128, L * PPL), f)

    def cols(vec, n):
        return np.asarray(vec, f).reshape(n, 128).T
    for l in range(L):
        b = l * PPL
        pp[:, b + 0:b + 8] = cols(inp["norm_mix"][l], 8)
        pp[:, b + 8:b + 16] = cols(inp["norm_ffn"][l], 8)
        pp[:, b + 168:b + 176] = np.asarray(inp["norm_ffn"][l], f).reshape(128, 8)
        pp[:, b + 16:b + 24] = cols(inp["norm_ple"][l], 8)
        pp[:, b + 24:b + 32] = cols(inp["b_conv_out"][l], 8)
        pp[:, b + 32:b + 36] = cols(inp["b_dw"][l], 4)
        pp[:, b + 36:b + 40] = cols(inp["ln_conv_g"][l], 4)
        pp[:, b + 40:b + 44] = cols(inp["ln_conv_b"][l], 4)
        wd = np.asarray(inp["w_dw"][l], f)
        for j in range(31):
            pp[:, b + 44 + 4 * j:b + 48 + 4 * j] = cols(wd[j], 4)
    bc = np.zeros((128, 72 + D), f)
    for l in range(L):
        bc[:, l * 36:l * 36 + 4] = np.asarray(inp["b_route_group"][l], f)[None, :]
        bc[:, l * 36 + 4:l * 36 + 36] = np.asarray(inp["b_route_expert"][l], f)[None, :]
    bc[:, 72:] = np.asarray(inp["norm_final"], f)[None, :]
    inv = np.power(f(10000.0), -np.arange(0, 64, 2, dtype=f) / f(64)).astype(f)
    ang = (np.arange(S, dtype=f)[:, None] * inv[None, :]).astype(f)
    cs, sn = np.cos(ang).astype(f).T, np.sin(ang).astype(f).T
    cosT = np.concatenate([cs, cs, cs, cs], axis=0)
    sinT = np.concatenate([-sn, sn, -sn, sn], axis=0)
    kk = np.arange(128)[:, None]
    col = np.arange(S)[None, :]
    dl = col - kk
    cnt = ((dl >= 0) & (dl <= 128)).astype(f) + ((dl >= 0) & (dl <= 512) & (dl % 4 == 0)).astype(f) \
        + ((dl >= 0) & (dl <= 2048) & (dl % 16 == 0)).astype(f)
    shared = {
        "win": win, "wao": np.ascontiguousarray(inp["w_attn_out"], f), "wco": np.ascontiguousarray(inp["w_conv_out"], f),
        "wout": np.ascontiguousarray(inp["w_out"], f), "wr": wr,
        "wpg": np.ascontiguousarray(inp["w_ple_gate"], f), "wpe": np.ascontiguousarray(inp["w_ple_proj"], f),
        "pp": pp, "bc": bc, "cosT": np.ascontiguousarray(cosT), "sinT": np.ascontiguousarray(sinT),
        "maskT": np.ascontiguousarray(cnt), "ident": np.eye(128, dtype=f),
    }
    for l in range(L):
        shared[f"weg{l}"] = np.ascontiguousarray(inp["w_exp_gate"][l], f).reshape(8192, 2048)
        shared[f"weu{l}"] = np.ascontiguousarray(inp["w_exp_up"][l], f).reshape(8192, 2048)
        shared[f"wed{l}"] = np.ascontiguousarray(
            np.asarray(inp["w_exp_down"][l], f).reshape(NE, 4, 128, D).transpose(0, 2, 1, 3)).reshape(8192, 2048)
    cst = np.zeros((128, 384), f)
    cst[:, 320:352] = (256 * np.arange(32, dtype=f))[None, :]
    cst[:, 352:384] = (7936 - 256 * np.arange(32, dtype=f))[None, :]
    cst[:, 0:64] = np.arange(64, dtype=f)[None, :]
    cst[:, 64:128] = (2 * np.arange(128, dtype=f))[:, None]
    cst[:, 128:192] = (2 * np.arange(128, dtype=f) + 1)[:, None]
    cst[:, 192:320] = (np.arange(128)[:, None] < np.arange(128)[None, :]).astype(f)
    shared["cst"] = cst
    return shared


_NL = L
_DBG = 0


def kernel(**inputs):
    shared = _host_layout(inputs)
    x = np.asarray(inputs["x"], np.float32)
    p = np.asarray(inputs["p"], np.float32)
    nc = build_program(_NL)
    in_maps = []
    for b in range(8):
        m = dict(shared)
        m["x"] = np.ascontiguousarray(x[b])
        m["p"] = np.ascontiguousarray(p[:, b])
        in_maps.append(m)
    res = run_bass_kernel_spmd(nc, in_maps, core_ids=list(range(8)))
    return np.stack([r["out"] for r in res.results], axis=0).astype(np.float32)
```

```python
import contextlib
import numpy as np
import concourse.bass as bass
import concourse.mybir as mybir
from concourse.bass_utils import run_bass_kernel_spmd

F32 = mybir.dt.float32
BF16 = mybir.dt.bfloat16
ALU = mybir.AluOpType
AF = mybir.ActivationFunctionType
AX = mybir.AxisListType

S = 2048
D = 1024
L = 2
NT = 16
NE = 32
WIN_EXT = 5632
PPL = 176
EPS = 1e-6
BIG = 1.0e30

STRICT_SAME = True


class T:
    __slots__ = ("ap", "space", "lo", "hi", "w", "r", "ov", "name")

    def __init__(self, ap, space, lo, hi, name=""):
        self.ap, self.space, self.lo, self.hi, self.name = ap, space, lo, hi, name
        self.w = None
        self.r = {}
        self.ov = [self]


class Op:
    __slots__ = ("eng", "fn", "deps", "ddeps", "sig", "count", "dkey", "dval", "is_write")

    def __init__(self, eng, fn):
        self.eng, self.fn = eng, fn
        self.deps = []
        self.ddeps = {}
        self.sig = False
        self.count = None
        self.dkey = None
        self.dval = None
        self.is_write = False


class Prog:
    def __init__(self, nc):
        self.nc = nc
        self.ops = {e: [] for e in ("pe", "act", "dve", "pool", "sp")}
        self.tiles = {"sb": [], "ps": []}
        self.dma_counts = {}
        self.reg_init = {}
        self.regs = {}

    def tile(self, ap, space, lo, hi, name=""):
        t = T(ap, space, lo, hi, name)
        for o in self.tiles[space]:
            if o.lo < hi and lo < o.hi:
                o.ov.append(t)
                t.ov.append(o)
        self.tiles[space].append(t)
        return t

    def _dep_on(self, op, prod, raw=True):
        if prod is None or prod is op:
            return
        if prod.dkey is not None:
            k = prod.dkey
            if not raw and op.dkey == k and prod.dval is not None and prod.is_write:
                return
            v = self.dma_counts[k]
            if op.ddeps.get(k, 0) < v:
                op.ddeps[k] = v
            return
        if prod.eng == op.eng and op.dkey is None and (op.eng == "pe" or not STRICT_SAME):
            return
        prod.sig = True
        op.deps.append(prod)

    def op(self, eng, fn, reads=(), writes=(), dkey=None):
        o = Op(eng, fn)
        o.dkey = dkey
        o.is_write = len(writes) > 0
        for t in reads:
            for u in t.ov:
                self._dep_on(o, u.w)
        for t in writes:
            for u in t.ov:
                self._dep_on(o, u.w, raw=False)
                for rd in u.r.values():
                    self._dep_on(o, rd)
        if dkey is not None:
            self.dma_counts[dkey] = self.dma_counts.get(dkey, 0) + 16
            o.dval = self.dma_counts[dkey]
        stream = eng if dkey is None else ("dma", id(o))
        for t in reads:
            t.r[stream] = o
        for t in writes:
            t.w = o
            t.r = {}
        self.ops[eng].append(o)
        return o

    def emit(self):
        nc = self.nc
        with contextlib.ExitStack() as st:
            sems = {e: st.enter_context(nc.semaphore("s_" + e)) for e in ("pe", "act", "dve", "pool")}
            dsems = {k: st.enter_context(nc.semaphore("d_" + str(k))) for k in self.dma_counts}
            for e, lst in self.ops.items():
                c = 0
                for o in lst:
                    if o.dkey is None and o.sig:
                        c += 1
                        o.count = c
            block = st.enter_context(nc.Block())

            def run(engname, engobj):
                waited = {}
                if engname == "pool":
                    for nm, val in self.reg_init.items():
                        r = engobj.alloc_register("bnd_" + nm)
                        engobj.reg_mov(r, val)
                        self.regs[nm] = r
                for o in self.ops[engname]:
                    best = {}
                    for p in o.deps:
                        if best.get(p.eng, 0) < p.count:
                            best[p.eng] = p.count
                    for pe_, cnt in best.items():
                        if waited.get(("c", pe_), 0) < cnt:
                            engobj.wait_ge(sems[pe_], cnt)
                            waited[("c", pe_)] = cnt
                    for k, v in o.ddeps.items():
                        if waited.get(("d", k), 0) < v:
                            engobj.wait_ge(dsems[k], v)
                            waited[("d", k)] = v
                    ins = o.fn(engobj)
                    if o.dkey is not None:
                        ins.then_inc(dsems[o.dkey], 16)
                    elif o.sig:
                        ins.then_inc(sems[engname], 1)

            @block.sync
            def _(e):
                run("sp", e)

            @block.tensor
            def _(e):
                run("pe", e)

            @block.scalar
            def _(e):
                run("act", e)

            @block.vector
            def _(e):
                run("dve", e)

            @block.gpsimd
            def _(e):
                run("pool", e)


def build_program(nl=L):
    nc = bass.Bass("TRN2", target_bir_lowering=False)

    def din(name, shape):
        return nc.dram_tensor(name, list(shape), F32, kind="ExternalInput").ap()

    x_d = din("x", [S, D])
    p_d = din("p", [L, S, 256])
    win_d = din("win", [L, D, WIN_EXT])
    wao_d = din("wao", [L, 512, D])
    wco_d = din("wco", [L, 512, D])
    wout_d = din("wout", [L, D, D])
    wr_d = din("wr", [L, D, 36])
    weg_d = [nc.dram_tensor(f"weg{l}", [8192, 2048], F32, kind="ExternalInput") for l in range(L)]
    weu_d = [nc.dram_tensor(f"weu{l}", [8192, 2048], F32, kind="ExternalInput") for l in range(L)]
    wed_d = [nc.dram_tensor(f"wed{l}", [8192, 2048], F32, kind="ExternalInput") for l in range(L)]
    cst_d = din("cst", [128, 384])
    scr_d = nc.dram_tensor("moe_scr", [18432, D], F32)
    ys_d = scr_d
    sorted_d = scr_d[:, :].bitcast(BF16).rearrange("r (two c) -> (r two) c", two=2)
    wpg_d = din("wpg", [L, D, D])
    wpe_d = din("wpe", [L, 256, D])
    pp_d = din("pp", [128, L * PPL])
    bc_d = din("bc", [128, 72 + D])
    cos_d = din("cosT", [128, S])
    sin_d = din("sinT", [128, S])
    mask_d = din("maskT", [128, S])
    id_d = din("ident", [128, 128])
    out_d = nc.dram_tensor("out", [S, D], F32, kind="ExternalOutput").ap()
    dbg_d = nc.dram_tensor("dbg", [128, 512], F32, kind="ExternalOutput").ap() if _DBG == 6 else None

    NB = 212800
    with contextlib.ExitStack() as st:
        big = st.enter_context(nc.sbuf_tensor("arena", [128, NB // 4], F32))
        pbanks = [st.enter_context(nc.psum_tensor(f"pb{i}", [128, 512], F32)) for i in range(8)]
        P = Prog(nc)
        P.reg_init = {'b8191': 8191, 'b11775': 11775, 'b36351': 24576 + 11775}
        top = [0]

        def alloc(nbytes, at=None):
            nbytes = (nbytes + 31) // 32 * 32
            if at is None:
                at = top[0]
                top[0] = at + nbytes
            assert at + nbytes <= NB, ("SBUF overflow", at, nbytes)
            return at, at + nbytes

        def mk(n, dtype, name, at=None):
            es = 2 if dtype == BF16 else 4
            lo, hi = alloc(n * es, at)
            ap = big[:, lo // 4:hi // 4]
            if dtype != F32:
                ap = ap.bitcast(dtype)
            return P.tile(ap[:, 0:n], "sb", lo, hi, name)

        ps = [P.tile(pbanks[i][:, :], "ps", i * 2048, (i + 1) * 2048, f"ps{i}") for i in range(8)]
        rr = {"b": 0, "s": 0, "m": 0}

        nbank = [6]

        def bank():
            rr["b"] = (rr["b"] + 1) % nbank[0]
            return ps[rr["b"]]

        X = [[mk(512, F32, f"x{t}_{h}") for h in range(2)] for t in range(NT)]
        x0 = X[0][0].lo

        def xrow(t):
            lo = x0 + t * 4096
            return big[:, lo // 4: lo // 4 + 1024]
        ident = mk(128, F32, "ident")
        identb = mk(128, BF16, "identb")
        ones512 = mk(128, F32, "ones512")
        maskT = mk(S, BF16, "mask")
        pp = mk(L * PPL, F32, "pp")
        bcp = mk(72, F32, "bc")
        scr = [mk(512, F32, f"scr{i}") for i in range(5)]
        smalls = [mk(36, F32, f"sm{i}") for i in range(18)]
        PH0 = top[0]

        def scratch():
            rr["s"] = (rr["s"] + 1) % 5
            return scr[rr["s"]]

        def small():
            rr["m"] = (rr["m"] + 1) % 18
            return smalls[rr["m"]]

        top[0] = PH0
        kT = [[mk(512, BF16, f"kT{c}_{g}") for g in range(4)] for c in range(4)]
        vA = [mk(520, BF16, f"vA{t}") for t in range(NT)]
        hT = [mk(512, BF16, f"hT{c}") for c in range(8)]
        ring = [mk(2048, BF16, f"ring{i}") for i in range(6)]
        qT = [mk(512, BF16, f"qT{c}") for c in range(4)]
        cos2 = [mk(512, F32, f"cosg{i}") for i in range(2)]
        sin2 = [mk(512, F32, f"sing{i}") for i in range(2)]
        U = [mk(542, BF16, f"u{c}") for c in range(4)]
        dgs = [mk(128, BF16, f"dg{j}") for j in range(31)]
        junk_mix = mk(1024, BF16, "junk_mix", at=dgs[0].lo)
        Y = [mk(512, F32, f"y{c}") for c in range(4)]
        sT = [mk(512, BF16, f"sT{c}") for c in range(4)]
        pexp = [mk(512, BF16, f"pexp{i}") for i in range(3)]
        pmsk = [mk(512, BF16, f"pmsk{i}") for i in range(3)]
        otok = [mk(512, BF16, f"otok{j}") for j in range(4)]
        oT = [mk(512, BF16, f"oT{c}") for c in range(4)]
        mg = [mk(512, BF16, f"mg{c}") for c in range(8)]
        xnb = [mk(1024, BF16, f"xnb{i}", at=otok[0].lo + i * 2048) for i in range(4)]
        MIX_END = top[0]

        top[0] = PH0
        h2T = [[mk(512, BF16, f"h2T{c}_{g}") for g in range(4)] for c in range(8)]
        H2LO = h2T[0][0].lo
        xnb2 = [mk(1024, BF16, f"xnb2_{i}") for i in range(4)]
        XNLO = xnb2[0].lo
        junk_moe = mk(1024, BF16, "junk_moe")
        wrt = mk(8 * 36, BF16, "wrt")
        OH1 = [mk(32, F32, f"oh1_{t}") for t in range(NT)]
        OH2 = [mk(32, F32, f"oh2_{t}") for t in range(NT)]
        ABF = [mk(32, BF16, f"abf{t}") for t in range(NT)]
        RK = [mk(32, F32, f"rk{t}") for t in range(NT)]
        G12 = [mk(2, F32, f"g12_{t}") for t in range(NT)]
        DF = [mk(2, F32, f"df{t}") for t in range(NT)]
        DI = [mk(2, mybir.dt.int32, f"di{t}") for t in range(NT)]
        DIS = [mk(2, mybir.dt.int32, f"dis{t}") for t in range(NT)]
        cst = mk(384, F32, "cst")
        np_t = mk(32, F32, "np_t")
        oend_t = mk(32, F32, "oend")
        ops_t = mk(32, F32, "ops")
        q_t = mk(32, F32, "q_t")
        ltri = mk(128, BF16, "ltri")
        onesb = mk(128, BF16, "onesb")
        cntn = mk(32, F32, "cntn")
        nn_t = mk(32, F32, "nn")
        end_t = mk(32, F32, "end")
        psb_t = mk(32, F32, "psb")
        ebacc = mk(64, F32, "ebacc")
        idxg = mk(64, mybir.dt.int32, "idxg")
        idxd = mk(64, mybir.dt.int32, "idxd")
        GT = mk(1024, BF16, "GT")
        xs_t = [mk(1024, BF16, f"xs{i}") for i in range(2)]
        xg_t = [mk(1024, BF16, f"xg{i}") for i in range(4)]
        xbT = [mk(1024, BF16, f"xbT{i}") for i in range(2)]
        hbt = [mk(512, BF16, f"hbt{i}") for i in range(2)]
        hbT = [mk(512, BF16, f"hbT{i}") for i in range(2)]
        yst = [mk(1024, F32, f"yst{i}") for i in range(2)]
        yst.append(mk(1024, F32, "yst2", at=xs_t[0].lo))
        yst.append(mk(1024, F32, "yst3", at=OH1[0].lo))
        PH1 = top[0]
        ew = [[mk(4096, BF16, f"ew{s}_{m}", at=(H2LO + m * 8192) if s == 0 else None) for m in range(3)] for s in range(2)]
        ycmb = [mk(1024, F32, f"ycmb{i}", at=XNLO + i * 4096) for i in range(2)]
        wpe = mk(2048, BF16, "wpe")
        pT = [mk(S, BF16, f"pT{i}") for i in range(2)]
        pst = [mk(256, F32, f"pst{i}") for i in range(2)]
        MOE_END = top[0]
        top[0] = PH1
        wpg = [mk(4096, BF16, f"wpg{i}") for i in range(2)]
        ost = [mk(1024, F32, f"ost{i}") for i in range(2)]
        nfin = mk(D, F32, "nfin")
        PLE_END = top[0]
        print("SBUF bytes/partition: mixer", MIX_END, "moe", MOE_END, "ple", PLE_END, "limit", NB)

        dram_tiles = {}
        _dn = [0]
        for t_ in range(NT):
            for a_ in range(2):
                _dn[0] += 1
                dram_tiles[("sc", t_, a_)] = P.tile(None, "sb", 10 ** 9 + 10 * _dn[0], 10 ** 9 + 10 * _dn[0] + 1, "scd")
        for b_ in range(92):
            _dn[0] += 1
            dram_tiles[("ys", b_)] = P.tile(None, "sb", 10 ** 9 + 10 * _dn[0], 10 ** 9 + 10 * _dn[0] + 1, "ysd")

        dram_tiles[('gser',)] = P.tile(None, 'sb', 2 * 10 ** 9, 2 * 10 ** 9 + 1, 'gser')
        P.op("sp", lambda e: e.dma_start(out=ident.ap, in_=id_d), writes=[ident], dkey="c0")
        P.op("sp", lambda e: e.dma_start(out=pp.ap, in_=pp_d), writes=[pp], dkey="c0")
        P.op("sp", lambda e: e.dma_start(out=bcp.ap, in_=bc_d[:, 0:72]), writes=[bcp], dkey="c0")
        P.op("pool", lambda e: e.dma_start(out=maskT.ap, in_=mask_d), writes=[maskT], dkey="c1")
        P.op("pool", lambda e: e.dma_start(out=identb.ap, in_=id_d), writes=[identb], dkey="c1")
        P.op("pool", lambda e: e.memset(ones512.ap, 1.0 / 512), writes=[ones512])
        for g in range(4):
            for i in range(4):
                t = 4 * g + i
                for h in range(2):
                    P.op("sp", lambda e, t=t, h=h: e.dma_start(out=X[t][h].ap, in_=x_d[t * 128:(t + 1) * 128, h * 512:(h + 1) * 512]),
                         writes=[X[t][h]], dkey=f"xg{g}")

        def ppc(col):
            return pp.ap[:, col:col + 1]

        def row_rstd(t, junk):
            ss = small()
            P.op("act", lambda e: e.activation(out=junk.ap, in_=xrow(t), func=AF.Square, accum_out=ss.ap[:, 0:1]),
                 reads=[X[t][0], X[t][1]], writes=[ss, junk])
            P.op("act", lambda e: e.activation(out=ss.ap[:, 1:2], in_=ss.ap[:, 0:1], func=AF.Sqrt, bias=EPS, scale=1.0 / D),
                 reads=[ss], writes=[ss])
            P.op("dve", lambda e: e.reciprocal(out=ss.ap[:, 2:3], in_=ss.ap[:, 1:2]), reads=[ss], writes=[ss])
            return ss

        def norm_T(g, gcol, dst, xn_tiles):
            for i in range(4):
                t = 4 * g + i
                ss = row_rstd(t, junk_mix if xn_tiles is xnb else junk_moe)
                P.op("dve", lambda e, t=t, i=i, ss=ss: e.tensor_scalar(out=xn_tiles[i].ap, in0=xrow(t), scalar1=ss.ap[:, 2:3],
                                                                       scalar2=None, op0=ALU.mult),
                     reads=[X[t][0], X[t][1], ss], writes=[xn_tiles[i]])
            for c in range(8):
                b = bank()
                bv = b.ap.bitcast(BF16)
                for i in range(4):
                    P.op("pe", lambda e, i=i, c=c, bv=bv: e.transpose(out=bv[:, i * 128:(i + 1) * 128],
                                                                      in_=xn_tiles[i].ap[:, c * 128:(c + 1) * 128], identity=identb.ap),
                         reads=[xn_tiles[i], identb], writes=[b])
                P.op("act", lambda e, c=c, bv=bv: e.activation(out=dst[c].ap, in_=bv[:, 0:512], func=AF.Copy, scale=ppc(gcol + c)),
                     reads=[b, pp], writes=[dst[c]])

        def mm_acc(out_ap, out_t, pairs, extra_reads):
            n = len(pairs)
            for k, (lh, rh) in enumerate(pairs):
                P.op("pe", lambda e, lh=lh, rh=rh, k=k: e.matmul(out_ap, lhsT=lh, rhs=rh, start=(k == 0), stop=(k == n - 1)),
                     reads=extra_reads, writes=[out_t])

        ring_i = [0]

        def wload(srcs, key_reads=()):
            ring_i[0] = (ring_i[0] + 1) % 6
            slot = ring[ring_i[0]]
            si = ring_i[0]
            views = []
            off = 0
            for (src, k, c) in srcs:
                v = slot.ap[:, off:off + k * c].rearrange("p (k c) -> p k c", k=k)
                views.append(v)
                P.op("pool", lambda e, v=v, src=src: e.dma_start(out=v, in_=src), writes=[slot], dkey=f"ring{si}")
                off += k * c
            return slot, views

        def mixer_group(l, g):
            pb = l * PPL
            norm_T(g, pb + 0, hT, xnb)
            gi = l * 4 + g
            cosg, sing = cos2[gi % 2], sin2[gi % 2]

            def cs_load(gi_):
                g_ = gi_ % 4
                P.op("sp", lambda e: e.dma_start(out=cos2[gi_ % 2].ap, in_=cos_d[:, g_ * 512:(g_ + 1) * 512]), writes=[cos2[gi_ % 2]], dkey=f"cs{gi_ % 2}")
                P.op("sp", lambda e: e.dma_start(out=sin2[gi_ % 2].ap, in_=sin_d[:, g_ * 512:(g_ + 1) * 512]), writes=[sin2[gi_ % 2]], dkey=f"cs{gi_ % 2}")
            if g == 0:
                cs_load(gi)
            if g < 3:
                cs_load(gi + 1)
            winl = win_d[l].rearrange("(k p) c -> p k c", p=128)
            waol = wao_d[l].rearrange("(k p) c -> p k c", p=128)
            wcol = wco_d[l].rearrange("(k p) c -> p k c", p=128)
            woutl = wout_d[l].rearrange("(k p) c -> p k c", p=128)

            def win_blk(c0):
                return [(winl[:, :, c0:c0 + 256], 8, 256)]

            tasks = []

            def rope_task(base, dst_fn):
                for b in range(2):
                    def comp(slots, b=b):
                        (s1, v1), (s2, v2) = slots
                        for cc in range(2):
                            c = 2 * b + cc
                            bq = bank()
                            mm_acc(bq.ap, bq, [(v1[0][:, k, cc * 128:(cc + 1) * 128], hT[k].ap) for k in range(8)], [s1] + hT)
                            bs = bank()
                            mm_acc(bs.ap, bs, [(v2[0][:, k, cc * 128:(cc + 1) * 128], hT[k].ap) for k in range(8)], [s2] + hT)
                            t1 = scratch()
                            P.op("dve", lambda e, t1=t1, bq=bq: e.tensor_tensor(out=t1.ap, in0=bq.ap, in1=cosg.ap, op=ALU.mult),
                                 reads=[bq, cosg], writes=[t1])
                            t2 = scratch()
                            P.op("dve", lambda e, t2=t2, bs=bs: e.tensor_tensor(out=t2.ap, in0=bs.ap, in1=sing.ap, op=ALU.mult),
                                 reads=[bs, sing], writes=[t2])
                            dst = dst_fn(c)
                            P.op("dve", lambda e, t1=t1, t2=t2, dst=dst: e.tensor_tensor(out=dst.ap, in0=t1.ap, in1=t2.ap, op=ALU.add),
                                 reads=[t1, t2], writes=[dst])
                    tasks.append(([win_blk(base + 256 * b), win_blk(base + 512 + 256 * b)], comp))
            rope_task(0, lambda c: qT[c])
            rope_task(1024, lambda c: kT[c][g])

            for b in range(2):
                def comp(slots, b=b):
                    (s1, v1), = slots
                    for i in range(4):
                        t = 4 * g + i
                        bv = bank()
                        mm_acc(bv.ap[:, 0:256], bv, [(hT[k].ap[:, i * 128:(i + 1) * 128], v1[0][:, k, :]) for k in range(8)], [s1] + hT)
                        vv = vA[t].ap.rearrange("p (h e) -> p h e", h=8)
                        if b == 0:
                            P.op("pool", lambda e, vv=vv: e.memset(vv[:, :, 64:65], 1.0), writes=[vA[t]])
                        P.op("act", lambda e, vv=vv, bv=bv, b=b: e.activation(
                            out=vv[:, 4 * b:4 * b + 4, 0:64], in_=bv.ap[:, 0:256].rearrange("p (h e) -> p h e", h=4), func=AF.Copy),
                            reads=[bv], writes=[vA[t]])
                tasks.append(([win_blk(2048 + 256 * b)], comp))

            for b in range(2):
                def comp(slots, b=b):
                    (s1, v1), (s2, v2) = slots
                    for cc in range(2):
                        c = 2 * b + cc
                        bu = bank()
                        mm_acc(bu.ap, bu, [(v1[0][:, k, cc * 128:(cc + 1) * 128], hT[k].ap) for k in range(8)], [s1] + hT)
                        bg = bank()
                        mm_acc(bg.ap, bg, [(v2[0][:, k, cc * 128:(cc + 1) * 128], hT[k].ap) for k in range(8)], [s2] + hT)
                        sg = scratch()
                        P.op("act", lambda e, sg=sg, bg=bg: e.activation(out=sg.ap, in_=bg.ap, func=AF.Sigmoid), reads=[bg], writes=[sg])
                        if g == 0:
                            P.op("pool", lambda e, c=c: e.memset(U[c].ap[:, 0:30], 0.0), writes=[U[c]])
                        else:
                            P.op("pool", lambda e, c=c: e.tensor_copy(out=U[c].ap[:, 0:30], in_=U[c].ap[:, 512:542]), reads=[U[c]], writes=[U[c]])
                        P.op("dve", lambda e, c=c, bu=bu, sg=sg: e.tensor_tensor(out=U[c].ap[:, 30:542], in0=bu.ap, in1=sg.ap, op=ALU.mult),
                             reads=[bu, sg], writes=[U[c]])
                        wc0 = pb + 44
                        for j in range(31):
                            if j % 2 == 0:
                                P.op("dve", lambda e, j=j, c=c: e.tensor_scalar(out=dgs[j].ap, in0=identb.ap, scalar1=ppc(wc0 + 4 * j + c), scalar2=None,
                                                                              op0=ALU.mult), reads=[identb, pp], writes=[dgs[j]])
                            else:
                                P.op("act", lambda e, j=j, c=c: e.activation(out=dgs[j].ap, in_=identb.ap, func=AF.Copy, scale=ppc(wc0 + 4 * j + c)),
                                     reads=[identb, pp], writes=[dgs[j]])
                        by = bank()
                        for j in range(31):
                            P.op("pe", lambda e, j=j, c=c, by=by: e.matmul(by.ap, lhsT=dgs[j].ap, rhs=U[c].ap[:, j:j + 512], start=(j == 0), stop=(j == 30)),
                                 reads=[dgs[j], U[c]], writes=[by])
                        P.op("dve", lambda e, c=c, by=by: e.tensor_scalar(out=Y[c].ap, in0=by.ap, scalar1=ppc(pb + 32 + c), scalar2=None, op0=ALU.add),
                             reads=[by, pp], writes=[Y[c]])
                tasks.append(([win_blk(2560 + 256 * b), win_blk(3072 + 256 * b)], comp))

            def comp_ln_attn(slots):
                bm = bank()
                mm_acc(bm.ap, bm, [(ones512.ap, Y[c].ap) for c in range(4)], [ones512] + Y)
                be = bank()
                for c in range(4):
                    sq = scratch()
                    P.op("act", lambda e, sq=sq, c=c: e.activation(out=sq.ap, in_=Y[c].ap, func=AF.Square), reads=[Y[c]], writes=[sq])
                    P.op("pe", lambda e, sq=sq, c=c: e.matmul(be.ap, lhsT=ones512.ap, rhs=sq.ap, start=(c == 0), stop=(c == 3)),
                         reads=[ones512, sq], writes=[be])
                msq = scratch()
                P.op("act", lambda e: e.activation(out=msq.ap, in_=bm.ap, func=AF.Square), reads=[bm], writes=[msq])
                var = scratch()
                P.op("dve", lambda e: e.tensor_tensor(out=var.ap, in0=be.ap, in1=msq.ap, op=ALU.subtract), reads=[be, msq], writes=[var])
                P.op("act", lambda e: e.activation(out=var.ap, in_=var.ap, func=AF.Sqrt, bias=EPS, scale=1.0), reads=[var], writes=[var])
                P.op("dve", lambda e: e.reciprocal(out=var.ap, in_=var.ap), reads=[var], writes=[var])
                for c in range(4):
                    yn = scratch()
                    P.op("dve", lambda e, yn=yn, c=c: e.tensor_tensor(out=yn.ap, in0=Y[c].ap, in1=bm.ap, op=ALU.subtract),
                         reads=[Y[c], bm], writes=[yn])
                    P.op("dve", lambda e, yn=yn: e.tensor_tensor(out=yn.ap, in0=yn.ap, in1=var.ap, op=ALU.mult), reads=[yn, var], writes=[yn])
                    P.op("act", lambda e, yn=yn, c=c: e.activation(out=sT[c].ap, in_=yn.ap, func=AF.Silu, scale=ppc(pb + 36 + c), bias=ppc(pb + 40 + c)),
                         reads=[yn, pp], writes=[sT[c]])
                nkt = 4 * g + 4
                items = [(h, kt) for h in range(8) for kt in range(nkt)]
                stg = {}

                def S_(n):
                    h, kt = items[n]
                    c, pbase = h // 2, (h % 2) * 64
                    j0 = max(kt - 4 * g, 0)
                    c0 = j0 * 128
                    bs = bank()
                    ktile = kT[c][kt // 4]
                    P.op("pe", lambda e: e.matmul(
                        bs.ap[:, c0:512], lhsT=ktile.ap[pbase:pbase + 64, (kt % 4) * 128:(kt % 4) * 128 + 128],
                        rhs=qT[c].ap[pbase:pbase + 64, c0:512], start=True, stop=True),
                        reads=[ktile, qT[c]], writes=[bs])
                    pe_t = pexp[n % 3]
                    P.op("act", lambda e: e.activation(out=pe_t.ap[:, c0:512], in_=bs.ap[:, c0:512], func=AF.Exp, scale=0.125),
                         reads=[bs], writes=[pe_t])
                    pm_t = pmsk[n % 3]
                    o0 = 4 * g + j0 - kt
                    P.op("dve", lambda e: e.tensor_tensor(
                        out=pm_t.ap[:, c0:512], in0=pe_t.ap[:, c0:512], in1=maskT.ap[:, o0 * 128:o0 * 128 + 512 - c0], op=ALU.mult),
                        reads=[pe_t, maskT], writes=[pm_t])
                    stg[n] = (pm_t, j0)

                def V_(n):
                    h, kt = items[n]
                    pm_t, j0 = stg.pop(n)
                    accb = ps[6 + (h % 2)]
                    acc = accb.ap[:, 0:260].rearrange("p (j e) -> p j e", j=4)
                    for j in range(j0, 4):
                        P.op("pe", lambda e, j=j: e.matmul(
                            acc[:, j, :], lhsT=pm_t.ap[:, j * 128:(j + 1) * 128], rhs=vA[kt].ap[:, h * 65:(h + 1) * 65],
                            start=(kt == 0 and j == 0), stop=(kt == nkt - 1 and j == 3)),
                            reads=[pm_t, vA[kt]], writes=[accb])
                    if kt == nkt - 1:
                        rc = small()
                        P.op("dve", lambda e: e.reciprocal(out=rc.ap[:, 0:4], in_=acc[:, :, 64]), reads=[accb], writes=[rc])
                        for j in range(4):
                            P.op("dve", lambda e, j=j: e.tensor_scalar(
                                out=otok[j].ap[:, h * 64:(h + 1) * 64], in0=acc[:, j, 0:64], scalar1=rc.ap[:, j:j + 1], scalar2=None, op0=ALU.mult),
                                reads=[accb, rc], writes=[otok[j]])
                LA = 2
                for n in range(min(LA, len(items))):
                    S_(n)
                for n in range(len(items)):
                    if n + LA < len(items):
                        S_(n + LA)
                    V_(n)
                for c in range(4):
                    b = bank()
                    bv = b.ap.bitcast(BF16)
                    for j in range(4):
                        P.op("pe", lambda e, bv=bv, j=j, c=c: e.transpose(out=bv[:, j * 128:(j + 1) * 128], in_=otok[j].ap[:, c * 128:(c + 1) * 128],
                                                                          identity=identb.ap), reads=[otok[j], identb], writes=[b])
                    P.op("act", lambda e, bv=bv, c=c: e.activation(out=oT[c].ap, in_=bv[:, 0:512], func=AF.Copy), reads=[b], writes=[oT[c]])
            tasks.append(([], comp_ln_attn))

            for ob in range(4):
                def comp(slots, ob=ob):
                    (s1, v1), (s2, v2), (s3, v3) = slots
                    for cc in range(2):
                        oc = 2 * ob + cc
                        sl = slice(cc * 128, (cc + 1) * 128)
                        bga = bank()
                        mm_acc(bga.ap, bga, [(v1[0][:, k, sl], hT[k].ap) for k in range(8)], [s1] + hT)
                        bgb = bank()
                        mm_acc(bgb.ap, bgb, [(v2[0][:, k, sl], hT[k].ap) for k in range(8)], [s2] + hT)
                        bya = bank()
                        mm_acc(bya.ap, bya, [(v3[0][:, k, sl], oT[k].ap) for k in range(4)], [s3] + oT)
                        byb = bank()
                        mm_acc(byb.ap, byb, [(v3[1][:, k, sl], sT[k].ap) for k in range(4)], [s3] + sT)
                        sga = scratch()
                        P.op("act", lambda e, sga=sga, bga=bga: e.activation(out=sga.ap, in_=bga.ap, func=AF.Sigmoid), reads=[bga], writes=[sga])
                        sgb = scratch()
                        P.op("act", lambda e, sgb=sgb, bgb=bgb: e.activation(out=sgb.ap, in_=bgb.ap, func=AF.Sigmoid), reads=[bgb], writes=[sgb])
                        P.op("dve", lambda e, sga=sga, bya=bya: e.tensor_tensor(out=sga.ap, in0=bya.ap, in1=sga.ap, op=ALU.mult),
                             reads=[bya, sga], writes=[sga])
                        P.op("dve", lambda e, sgb=sgb, byb=byb, oc=oc: e.scalar_tensor_tensor(out=sgb.ap, in0=byb.ap, scalar=ppc(pb + 24 + oc), in1=sgb.ap,
                                                                                            op0=ALU.add, op1=ALU.mult),
                             reads=[byb, sgb, pp], writes=[sgb])
                        P.op("dve", lambda e, sga=sga, sgb=sgb, oc=oc: e.tensor_tensor(out=mg[oc].ap, in0=sga.ap, in1=sgb.ap, op=ALU.add),
                             reads=[sga, sgb], writes=[mg[oc]])
                tasks.append(([win_blk(3584 + 256 * ob), win_blk(4608 + 256 * ob),
                               [(waol[:, :, 256 * ob:256 * ob + 256], 4, 256), (wcol[:, :, 256 * ob:256 * ob + 256], 4, 256)]], comp))

            for nb_ in range(4):
                def comp(slots, nb_=nb_):
                    (s1, v1), = slots
                    for i in range(4):
                        t = 4 * g + i
                        b = bank()
                        mm_acc(b.ap[:, 0:256], b, [(mg[k].ap[:, i * 128:(i + 1) * 128], v1[0][:, k, :]) for k in range(8)], [s1] + mg)
                        xt = X[t][nb_ // 2]
                        xs = xt.ap[:, (nb_ % 2) * 256:(nb_ % 2) * 256 + 256]
                        P.op("dve", lambda e, xs=xs, b=b: e.tensor_tensor(out=xs, in0=b.ap[:, 0:256], in1=xs, op=ALU.add),
                             reads=[b, xt], writes=[xt])
                tasks.append(([[(woutl[:, :, 256 * nb_:256 * nb_ + 256], 8, 256)]], comp))

            return tasks

        def moe_phase(l):
            pb = l * PPL
            I32 = mybir.dt.int32
            P.op("sp", lambda e: e.dma_start(out=cst.ap, in_=cst_d), writes=[cst], dkey="cst")
            P.op("pool", lambda e: e.dma_start(out=ltri.ap, in_=cst_d[:, 192:320]), writes=[ltri], dkey="cstb")
            P.op("pool", lambda e: e.memset(onesb.ap, 1.0), writes=[onesb])
            for g in range(4):
                norm_T(g, pb + 8, [h2T[c][g] for c in range(8)], xnb2)
            P.op("pool", lambda e: e.dma_start(out=wrt.ap.rearrange("p (k c) -> p k c", k=8), in_=wr_d[l].rearrange("(k p) c -> p k c", p=128)),
                 writes=[wrt], dkey="wr")
            wrv = wrt.ap.rearrange("p (k c) -> p k c", k=8)

            def route_tile(t):
                g, i = t // 4, t % 4
                b = bank()
                mm_acc(b.ap[:, 0:36], b, [(h2T[k][g].ap[:, i * 128:(i + 1) * 128], wrv[:, k, :]) for k in range(8)],
                       [wrt] + [h2T[k][g] for k in range(8)])
                lg = small()
                P.op("dve", lambda e, lg=lg, b=b: e.tensor_tensor(out=lg.ap, in0=b.ap[:, 0:36], in1=bcp.ap[:, l * 36:(l + 1) * 36], op=ALU.add),
                     reads=[b, bcp], writes=[lg])
                w1 = small()
                W = w1.ap
                P.op("dve", lambda e, lg=lg, W=W: e.tensor_reduce(out=W[:, 0:1], in_=lg.ap[:, 0:4], axis=AX.X, op=ALU.max), reads=[lg], writes=[w1])
                P.op("dve", lambda e, W=W: e.tensor_scalar(out=W[:, 1:2], in0=W[:, 0:1], scalar1=-1.0, scalar2=None, op0=ALU.mult), reads=[w1], writes=[w1])
                P.op("act", lambda e, lg=lg, W=W: e.activation(out=W[:, 8:12], in_=lg.ap[:, 0:4], func=AF.Exp, bias=W[:, 1:2], scale=1.0, accum_out=W[:, 2:3]),
                     reads=[lg, w1], writes=[w1])
                P.op("dve", lambda e, W=W: e.reciprocal(out=W[:, 3:4], in_=W[:, 2:3]), reads=[w1], writes=[w1])
                P.op("dve", lambda e, lg=lg, W=W: e.tensor_scalar(out=W[:, 4:8], in0=lg.ap[:, 0:4], scalar1=W[:, 0:1], scalar2=None, op0=ALU.is_equal),
                     reads=[lg, w1], writes=[w1])
                P.op("dve", lambda e, W=W: e.tensor_scalar(out=W[:, 4:8], in0=W[:, 4:8], scalar1=BIG, scalar2=-BIG, op0=ALU.mult, op1=ALU.add),
                     reads=[w1], writes=[w1])
                lem = small()
                for gg in range(4):
                    P.op("dve", lambda e, lem=lem, lg=lg, W=W, gg=gg: e.tensor_scalar(out=lem.ap[:, gg * 8:(gg + 1) * 8], in0=lg.ap[:, 4 + gg * 8:12 + gg * 8],
                                                                                  scalar1=W[:, 4 + gg:5 + gg], scalar2=None, op0=ALU.add),
                         reads=[lg, w1], writes=[lem])
                w2 = small()
                V = w2.ap
                oh1, oh2 = OH1[t], OH2[t]
                lem2 = small()
                P.op("dve", lambda e, lem=lem, V=V: e.tensor_reduce(out=V[:, 0:1], in_=lem.ap[:, 0:32], axis=AX.X, op=ALU.max), reads=[lem], writes=[w2])
                P.op("dve", lambda e, lem=lem, V=V, oh1=oh1: e.tensor_scalar(out=oh1.ap, in0=lem.ap[:, 0:32], scalar1=V[:, 0:1], scalar2=None, op0=ALU.is_equal),
                     reads=[lem, w2], writes=[oh1])
                P.op("dve", lambda e, lem=lem, lem2=lem2, oh1=oh1: e.scalar_tensor_tensor(out=lem2.ap[:, 0:32], in0=oh1.ap, scalar=-BIG, in1=lem.ap[:, 0:32],
                                                                                  op0=ALU.mult, op1=ALU.add), reads=[oh1, lem], writes=[lem2])
                P.op("dve", lambda e, lem2=lem2, V=V: e.tensor_reduce(out=V[:, 1:2], in_=lem2.ap[:, 0:32], axis=AX.X, op=ALU.max), reads=[lem2], writes=[w2])
                P.op("dve", lambda e, lem2=lem2, V=V, oh2=oh2: e.tensor_scalar(out=oh2.ap, in0=lem2.ap[:, 0:32], scalar1=V[:, 1:2], scalar2=None, op0=ALU.is_equal),
                     reads=[lem2, w2], writes=[oh2])
                P.op("dve", lambda e, V=V: e.tensor_tensor(out=V[:, 2:3], in0=V[:, 1:2], in1=V[:, 0:1], op=ALU.subtract), reads=[w2], writes=[w2])
                P.op("act", lambda e, V=V: e.activation(out=V[:, 3:4], in_=V[:, 2:3], func=AF.Exp), reads=[w2], writes=[w2])
                P.op("dve", lambda e, V=V: e.tensor_scalar(out=V[:, 4:5], in0=V[:, 3:4], scalar1=1.0, scalar2=None, op0=ALU.add), reads=[w2], writes=[w2])
                P.op("dve", lambda e, V=V: e.reciprocal(out=V[:, 5:6], in_=V[:, 4:5]), reads=[w2], writes=[w2])
                P.op("dve", lambda e, V=V: e.tensor_tensor(out=V[:, 6:7], in0=V[:, 3:4], in1=V[:, 5:6], op=ALU.mult), reads=[w2], writes=[w2])
                P.op("dve", lambda e, V=V, W=W, t=t: e.tensor_scalar(out=G12[t].ap, in0=V[:, 5:7], scalar1=W[:, 3:4], scalar2=None, op0=ALU.mult),
                     reads=[w2, w1], writes=[G12[t]])
                P.op("dve", lambda e, oh1=oh1, oh2=oh2, t=t: e.tensor_tensor(out=ABF[t].ap, in0=oh1.ap, in1=oh2.ap, op=ALU.add),
                     reads=[oh1, oh2], writes=[ABF[t]])

            def capture(fn, *a):
                rec = []
                P.op = lambda *args, **kw: rec.append((args, kw))
                try:
                    fn(*a)
                finally:
                    del P.op
                return rec
            RB = 3
            for t0 in range(0, NT, RB):
                recs = [capture(route_tile, t) for t in range(t0, min(t0 + RB, NT))]
                for k_ in range(max(len(r) for r in recs)):
                    for r in recs:
                        if k_ < len(r):
                            P.op(*r[k_][0], **r[k_][1])

            for t in range(NT):
                b = bank()
                for tp in range(t):
                    P.op("pe", lambda e, b=b, tp=tp: e.matmul(b.ap[:, 0:32], lhsT=onesb.ap, rhs=ABF[tp].ap, start=(tp == 0), stop=False),
                         reads=[onesb, ABF[tp]], writes=[b])
                P.op("pe", lambda e, b=b, t=t: e.matmul(b.ap[:, 0:32], lhsT=ltri.ap, rhs=ABF[t].ap, start=(t == 0), stop=True),
                     reads=[ltri, ABF[t]], writes=[b])
                P.op("act", lambda e, b=b, t=t: e.activation(out=RK[t].ap, in_=b.ap[:, 0:32], func=AF.Copy), reads=[b], writes=[RK[t]])
            bc_ = bank()
            for t in range(NT):
                P.op("pe", lambda e, t=t: e.matmul(bc_.ap[:, 0:32], lhsT=onesb.ap, rhs=ABF[t].ap, start=(t == 0), stop=(t == NT - 1)),
                     reads=[onesb, ABF[t]], writes=[bc_])
            P.op("act", lambda e: e.activation(out=cntn.ap, in_=bc_.ap[:, 0:32], func=AF.Copy), reads=[bc_], writes=[cntn])
            P.op("dve", lambda e: e.tensor_scalar(out=nn_t.ap, in0=cntn.ap, scalar1=0.0, scalar2=None, op0=ALU.is_gt), reads=[cntn], writes=[nn_t])
            for j in range(1, 16):
                P.op("dve", lambda e, j=j: e.scalar_tensor_tensor(out=nn_t.ap, in0=cntn.ap, scalar=128.0 * j, in1=nn_t.ap, op0=ALU.is_gt, op1=ALU.add),
                     reads=[cntn, nn_t], writes=[nn_t])
            P.op("dve", lambda e: e.tensor_scalar(out=np_t.ap, in0=nn_t.ap, scalar1=-2.0, scalar2=0.0, op0=ALU.add, op1=ALU.max), reads=[nn_t], writes=[np_t])
            P.op("dve", lambda e: e.tensor_copy(out=oend_t.ap[:, 0:1], in_=np_t.ap[:, 0:1]), reads=[np_t], writes=[oend_t])
            for ee in range(1, NE):
                P.op("dve", lambda e, ee=ee: e.tensor_tensor(out=oend_t.ap[:, ee:ee + 1], in0=oend_t.ap[:, ee - 1:ee], in1=np_t.ap[:, ee:ee + 1], op=ALU.add),
                     reads=[oend_t, np_t], writes=[oend_t])
            P.op("dve", lambda e: e.tensor_tensor(out=ops_t.ap, in0=oend_t.ap, in1=np_t.ap, op=ALU.subtract), reads=[oend_t, np_t], writes=[ops_t])
            P.op("dve", lambda e: e.scalar_tensor_tensor(out=q_t.ap, in0=ops_t.ap, scalar=128.0, in1=cst.ap[:, 352:384], op0=ALU.mult, op1=ALU.add),
                 reads=[ops_t, cst], writes=[q_t])
            P.op("dve", lambda e: e.tensor_scalar(out=ebacc.ap, in0=cst.ap[:, 0:64], scalar1=oend_t.ap[:, 0:1], scalar2=None, op0=ALU.is_ge),
                 reads=[cst, oend_t], writes=[ebacc])
            for ee in range(1, NE):
                P.op("dve", lambda e, ee=ee: e.scalar_tensor_tensor(out=ebacc.ap, in0=cst.ap[:, 0:64], scalar=oend_t.ap[:, ee:ee + 1], in1=ebacc.ap,
                                                                  op0=ALU.is_ge, op1=ALU.add), reads=[cst, oend_t, ebacc], writes=[ebacc])
            P.op("dve", lambda e: e.scalar_tensor_tensor(out=idxg.ap, in0=ebacc.ap, scalar=256.0, in1=cst.ap[:, 64:128], op0=ALU.mult, op1=ALU.add),
                 reads=[ebacc, cst], writes=[idxg])
            P.op("dve", lambda e: e.scalar_tensor_tensor(out=idxd.ap, in0=ebacc.ap, scalar=256.0, in1=cst.ap[:, 128:192], op0=ALU.mult, op1=ALU.add),
                 reads=[ebacc, cst], writes=[idxd])
            if _DBG == 5:
                P.op("dve", lambda e: e.tensor_copy(out=idxg.ap, in_=cst.ap[:, 64:128]), reads=[cst], writes=[idxg])
                P.op("dve", lambda e: e.tensor_copy(out=idxd.ap, in_=cst.ap[:, 128:192]), reads=[cst], writes=[idxd])
            for t in range(NT):
                tmp = small()
                sel = small()
                P.op("dve", lambda e, sel=sel, t=t: e.scalar_tensor_tensor(out=sel.ap[:, 0:32], in0=RK[t].ap, scalar=256.0, in1=q_t.ap, op0=ALU.is_ge, op1=ALU.mult),
                     reads=[RK[t], q_t], writes=[sel])
                P.op("dve", lambda e, tmp=tmp, t=t: e.tensor_tensor(out=tmp.ap[:, 0:32], in0=RK[t].ap, in1=cst.ap[:, 320:352], op=ALU.add),
                     reads=[RK[t], cst], writes=[tmp])
                P.op("dve", lambda e, tmp=tmp, sel=sel: e.tensor_tensor(out=tmp.ap[:, 0:32], in0=tmp.ap[:, 0:32], in1=sel.ap[:, 0:32], op=ALU.add),
                     reads=[tmp, sel], writes=[tmp])
                for a_, oh in enumerate((OH1[t], OH2[t])):
                    m_ = small()
                    P.op("dve", lambda e, m_=m_, tmp=tmp, oh=oh: e.tensor_tensor(out=m_.ap[:, 0:32], in0=tmp.ap[:, 0:32], in1=oh.ap, op=ALU.mult),
                         reads=[tmp, oh], writes=[m_])
                    P.op("dve", lambda e, m_=m_, t=t, a_=a_: e.tensor_reduce(out=DF[t].ap[:, a_:a_ + 1], in_=m_.ap[:, 0:32], axis=AX.X, op=ALU.add),
                         reads=[m_], writes=[DF[t]])
                P.op("dve", lambda e, t=t: e.tensor_copy(out=DI[t].ap, in_=DF[t].ap), reads=[DF[t]], writes=[DI[t]])
                P.op("dve", lambda e, t=t: e.tensor_scalar(out=DIS[t].ap, in0=DF[t].ap, scalar1=24576.0, scalar2=None, op0=ALU.add), reads=[DF[t]], writes=[DIS[t]])
            for k in range(8):
                P.op("dve", lambda e, k=k: e.tensor_scalar(out=GT.ap[:, k * 128:(k + 1) * 128], in0=onesb.ap, scalar1=ppc(pb + 168 + k), scalar2=None, op0=ALU.mult),
                     reads=[onesb, pp], writes=[GT])
            if _DBG == 6:
                dbt = scr[0]
                P.op("dve", lambda e: e.tensor_copy(out=dbt.ap[:, 0:32], in_=cntn.ap), reads=[cntn], writes=[dbt])
                P.op("dve", lambda e: e.tensor_copy(out=dbt.ap[:, 32:64], in_=nn_t.ap), reads=[nn_t], writes=[dbt])
                P.op("dve", lambda e: e.tensor_copy(out=dbt.ap[:, 64:96], in_=end_t.ap), reads=[end_t], writes=[dbt])
                P.op("dve", lambda e: e.tensor_copy(out=dbt.ap[:, 96:128], in_=psb_t.ap), reads=[psb_t], writes=[dbt])
                P.op("dve", lambda e: e.tensor_copy(out=dbt.ap[:, 128:192], in_=ebacc.ap), reads=[ebacc], writes=[dbt])
                P.op("dve", lambda e: e.tensor_copy(out=dbt.ap[:, 192:256], in_=idxg.ap), reads=[idxg], writes=[dbt])
                P.op("dve", lambda e: e.tensor_copy(out=dbt.ap[:, 256:258], in_=DF[0].ap), reads=[DF[0]], writes=[dbt])
                P.op("dve", lambda e: e.tensor_copy(out=dbt.ap[:, 258:260], in_=DI[0].ap), reads=[DI[0]], writes=[dbt])
                P.op("dve", lambda e: e.tensor_copy(out=dbt.ap[:, 260:292], in_=RK[1].ap), reads=[RK[1]], writes=[dbt])
                P.op("dve", lambda e: e.tensor_copy(out=dbt.ap[:, 292:324], in_=OH1[0].ap), reads=[OH1[0]], writes=[dbt])
                P.op("sp", lambda e: e.dma_start(out=dbg_d, in_=dbt.ap), reads=[dbt], dkey="dbg")
                ple_prep(l)
                return
            if _DBG == 1:
                ple_prep(l)
                return
            sc_tiles = []
            for t in range(NT):
                ss = row_rstd(t, junk_moe)
                xs = xs_t[t % 2]
                P.op("dve", lambda e, t=t, ss=ss, xs=xs: e.tensor_scalar(out=xs.ap, in0=xrow(t), scalar1=ss.ap[:, 2:3], scalar2=None, op0=ALU.mult),
                     reads=[X[t][0], X[t][1], ss], writes=[xs])
                for a_ in range(2):
                    dt_ = dram_tiles[("sc", t, a_)]
                    P.op("pool", lambda e, t=t, a_=a_, xs=xs: e.indirect_dma_start(
                        out=sorted_d, out_offset=bass.IndirectOffsetOnAxis(ap=DIS[t].ap[:, a_:a_ + 1], axis=0),
                        in_=xs.ap, in_offset=None, bounds_check=P.regs['b36351'], oob_is_err=False),
                        reads=[xs, DIS[t]], writes=[dt_], dkey=f"sc{t % 2}")
                    sc_tiles.append(dt_)
            ple_prep(l)
            if _DBG == 2:
                return

            gser = dram_tiles[('gser',)]

            def eload(b_, mats):
                s_ = b_ % 2
                srcs = (weg_d[l], weu_d[l], wed_d[l])
                for m_ in mats:
                    src = srcs[m_]
                    for h_, idt in enumerate((idxg, idxd)):
                        P.op("pool", lambda e, m_=m_, src=src, h_=h_, idt=idt: e.indirect_dma_start(
                            out=ew[s_][m_].ap[:, h_ * 2048:(h_ + 1) * 2048], out_offset=None, in_=src[:, :],
                            in_offset=bass.IndirectOffsetOnAxis(ap=idt.ap[:, b_:b_ + 1], axis=0),
                            bounds_check=P.regs['b8191'], oob_is_err=False),
                            reads=[idt], writes=[ew[s_][m_]], dkey=f"ew{s_}_{m_}")

            ys_tiles = []

            def views(s_):
                return (ew[s_][0].ap.rearrange("p (k c) -> p k c", k=8), ew[s_][1].ap.rearrange("p (k c) -> p k c", k=8),
                        ew[s_][2].ap.rearrange("p (k c) -> p k c", k=4))

            def stA_load(b_):
                xg = xg_t[b_ % 4]
                P.op("sp", lambda e: e.dma_start(out=xg.ap, in_=sorted_d[24576 + b_ * 128:24576 + (b_ + 1) * 128, :]), reads=sc_tiles, writes=[xg], dkey=f"xgl{b_ % 4}")

            def stA(b_):
                x2 = b_ % 2
                xg = xg_t[b_ % 4]
                bt = bank()
                btv = bt.ap.bitcast(BF16)
                xgv = xg.ap.rearrange("p (a k) -> p a k", k=8)
                for k in range(8):
                    P.op("pe", lambda e, k=k: e.transpose(out=btv[:, k * 128:(k + 1) * 128], in_=xgv[:, :, k], identity=identb.ap),
                         reads=[xg, identb], writes=[bt])
                xb = xbT[x2]
                P.op("dve", lambda e: e.tensor_tensor(out=xb.ap, in0=btv[:, 0:1024], in1=GT.ap, op=ALU.mult), reads=[bt, GT], writes=[xb])

            def stB(b_, s_):
                x2 = b_ % 2
                vg, vu, vd = views(s_)
                xb = xbT[x2]
                bg = bank()
                mm_acc(bg.ap, bg, [(xb.ap[:, k * 128:(k + 1) * 128], vg[:, k, :]) for k in range(8)], [xb, ew[s_][0]])
                bu = bank()
                mm_acc(bu.ap, bu, [(xb.ap[:, k * 128:(k + 1) * 128], vu[:, k, :]) for k in range(8)], [xb, ew[s_][1]])
                sg = scratch()
                P.op("act", lambda e: e.activation(out=sg.ap, in_=bg.ap, func=AF.Silu), reads=[bg], writes=[sg])
                hb_ = hbt[x2]
                P.op("dve", lambda e: e.tensor_tensor(out=hb_.ap, in0=bu.ap, in1=sg.ap, op=ALU.mult), reads=[bu, sg], writes=[hb_])

            def stT(b_):
                x2 = b_ % 2
                hb_ = hbt[x2]
                bh = bank()
                bhv = bh.ap.bitcast(BF16)
                for f in range(4):
                    P.op("pe", lambda e, f=f: e.transpose(out=bhv[:, f * 128:(f + 1) * 128], in_=hb_.ap[:, f * 128:(f + 1) * 128], identity=identb.ap),
                         reads=[hb_, identb], writes=[bh])
                hT_ = hbT[x2]
                P.op("act", lambda e: e.activation(out=hT_.ap, in_=bhv[:, 0:512], func=AF.Copy), reads=[bh], writes=[hT_])

            def stC(b_, s_):
                x2 = b_ % 2
                vg, vu, vd = views(s_)
                hT_ = hbT[x2]
                yo = yst[b_ % 4]
                for hh in range(2):
                    bd = bank()
                    mm_acc(bd.ap, bd, [(hT_.ap[:, f * 128:(f + 1) * 128], vd[:, f, hh * 512:(hh + 1) * 512]) for f in range(4)], [hT_, ew[s_][2]])
                    if hh == 0:
                        P.op("act", lambda e, bd=bd: e.activation(out=yo.ap[:, 0:512], in_=bd.ap, func=AF.Copy), reads=[bd], writes=[yo])
                    else:
                        P.op("dve", lambda e, bd=bd: e.tensor_copy(out=yo.ap[:, 512:1024], in_=bd.ap), reads=[bd], writes=[yo])
                yt_ = dram_tiles[("ys", b_)]
                P.op("sp", lambda e: e.dma_start(out=ys_d[b_ * 128:(b_ + 1) * 128, :], in_=yo.ap), reads=[yo], writes=[yt_], dkey=f"yst{b_ % 4}")
                ys_tiles.append(yt_)

            def sload(e_, mats):
                s_ = e_ % 2
                srcs = (weg_d[l], weu_d[l], wed_d[l])
                for m_ in mats:
                    src = srcs[m_]
                    P.op("pool", lambda e, m_=m_, src=src: e.dma_start(
                        out=ew[s_][m_].ap.rearrange("p (h c) -> p h c", h=2), in_=src[256 * e_:256 * (e_ + 1), :].rearrange("(p h) c -> p h c", h=2)),
                        writes=[ew[s_][m_]], dkey=f"ew{s_}_{m_}")

            NOVF = 28
            NB_ALL = 64 + NOVF

            def slot_of(b_):
                return (b_ // 2) % 2 if b_ < 64 else (b_ - 64) % 2

            for i in range(-6, NB_ALL):
                for e_ in range(NE):
                    if i == 2 * e_ - 4:
                        sload(e_, (0, 1))
                    if i == 2 * e_ - 2:
                        sload(e_, (2,))
                for o_ in range(NOVF):
                    b_ = 64 + o_
                    if i == b_ - 3:
                        eload(o_, (0, 1))
                    if i == b_ - 1:
                        eload(o_, (2,))
                if 0 <= i + 6 < NB_ALL:
                    stA_load(i + 6)
                if 0 <= i + 3 < NB_ALL:
                    stA(i + 3)
                if 0 <= i + 2 < NB_ALL:
                    stB(i + 2, slot_of(i + 2))
                if 0 <= i + 1 < NB_ALL:
                    stT(i + 1)
                if 0 <= i < NB_ALL:
                    stC(i, slot_of(i))
            if _DBG in (3, 4, 5):
                return
            cbufs = [ycmb[0], ycmb[1], yst[0], yst[1], yst[2], yst[3]]
            for t in range(NT):
                for a_ in range(2):
                    ci = (2 * t + a_) % 6
                    yc = cbufs[ci]
                    P.op("pool", lambda e, t=t, a_=a_, yc=yc: e.indirect_dma_start(
                        out=yc.ap, out_offset=None, in_=ys_d[:, :], in_offset=bass.IndirectOffsetOnAxis(ap=DI[t].ap[:, a_:a_ + 1], axis=0),
                        bounds_check=P.regs['b11775'], oob_is_err=False),
                        reads=ys_tiles + [DI[t]], writes=[yc], dkey=f"yc{ci}")
                    for hh in range(2):
                        xt = X[t][hh]
                        P.op("dve", lambda e, t=t, a_=a_, yc=yc, hh=hh, xt=xt: e.scalar_tensor_tensor(
                            out=xt.ap, in0=yc.ap[:, hh * 512:(hh + 1) * 512], scalar=G12[t].ap[:, a_:a_ + 1], in1=xt.ap, op0=ALU.mult, op1=ALU.add),
                            reads=[yc, G12[t], xt], writes=[xt])

        def ple_prep(l):
            P.op("pool", lambda e: e.dma_start(out=wpe.ap.rearrange("p (k c) -> p k c", k=2), in_=wpe_d[l].rearrange("(k p) c -> p k c", p=128)),
                 writes=[wpe], dkey="wpe")
            for t in range(NT):
                stt = pst[t % 2]
                P.op("sp", lambda e, t=t, stt=stt: e.dma_start(out=stt.ap, in_=p_d[l, t * 128:(t + 1) * 128, :]), writes=[stt], dkey=f"pst{t % 2}")
                b = bank()
                for kc in range(2):
                    P.op("pe", lambda e, b=b, kc=kc, stt=stt: e.transpose(out=b.ap[:, kc * 128:(kc + 1) * 128], in_=stt.ap[:, kc * 128:(kc + 1) * 128], identity=ident.ap),
                         reads=[stt, ident], writes=[b])
                for kc in range(2):
                    P.op("act", lambda e, b=b, kc=kc, t=t: e.activation(out=pT[kc].ap[:, t * 128:(t + 1) * 128], in_=b.ap[:, kc * 128:(kc + 1) * 128], func=AF.Copy),
                         reads=[b], writes=[pT[kc]])

        def ple_phase(l):
            pb = l * PPL
            wpgl = wpg_d[l].rearrange("(k p) c -> p k c", p=128)
            for i in range(2):
                P.op("pool", lambda e, i=i: e.dma_start(out=wpg[i].ap.rearrange("p (k c) -> p k c", k=8), in_=wpgl[:, :, i * 512:(i + 1) * 512]),
                     writes=[wpg[i]], dkey="wp")
            wpev = wpe.ap.rearrange("p (k c) -> p k c", k=2)

            def ple_main(g_):
                for t in range(4 * g_, 4 * g_ + 4):
                    ple_tile(t)

            def ple_tile(t):
                g, i = t // 4, t % 4
                for hh in range(2):
                    wv = wpg[hh].ap.rearrange("p (k c) -> p k c", k=8)
                    bg = bank()
                    mm_acc(bg.ap, bg, [(h2T[k][g].ap[:, i * 128:(i + 1) * 128], wv[:, k, :]) for k in range(8)], [wpg[hh]] + [h2T[k][g] for k in range(8)])
                    be = bank()
                    mm_acc(be.ap, be, [(pT[kc].ap[:, t * 128:(t + 1) * 128], wpev[:, kc, hh * 512:(hh + 1) * 512]) for kc in range(2)], [wpe] + pT)
                    sg = scratch()
                    P.op("act", lambda e, sg=sg, bg=bg: e.activation(out=sg.ap, in_=bg.ap, func=AF.Sigmoid), reads=[bg], writes=[sg])
                    P.op("dve", lambda e, sg=sg, be=be: e.tensor_tensor(out=sg.ap, in0=be.ap, in1=sg.ap, op=ALU.mult), reads=[be, sg], writes=[sg])
                    xt = X[t][hh]
                    P.op("dve", lambda e, sg=sg, xt=xt: e.tensor_tensor(out=xt.ap, in0=xt.ap, in1=sg.ap, op=ALU.add), reads=[sg, xt], writes=[xt])

            norm_T(0, pb + 16, [h2T[c][0] for c in range(8)], xnb2)
            for g in range(4):
                if g + 1 < 4:
                    norm_T(g + 1, pb + 16, [h2T[c][g + 1] for c in range(8)], xnb2)
                ple_main(g)

        def final_phase():
            P.op("sp", lambda e: e.dma_start(out=nfin.ap, in_=bc_d[:, 72:72 + D]), writes=[nfin], dkey="nf")
            for t in range(NT):
                ss = row_rstd(t, junk_moe)
                o = ost[t % 2]
                P.op("dve", lambda e, t=t, ss=ss, o=o: e.scalar_tensor_tensor(out=o.ap, in0=xrow(t), scalar=ss.ap[:, 2:3], in1=nfin.ap, op0=ALU.mult, op1=ALU.mult),
                     reads=[X[t][0], X[t][1], ss, nfin], writes=[o])
                P.op("sp", lambda e, t=t, o=o: e.dma_start(out=out_d[t * 128:(t + 1) * 128, :], in_=o.ap), reads=[o], dkey=f"ost{t % 2}")
            for i in range(2):
                P.op("sp", lambda e: e.nop(), writes=[ost[i]])

        for l in range(nl):
            for g in range(4):
                tasks = mixer_group(l, g)
                loaded = {}
                n = len(tasks)
                nxt_load = 0
                in_use = 0
                for ti in range(n):
                    while nxt_load < n and (nxt_load <= ti or in_use + len(tasks[nxt_load][0]) <= 6):
                        loaded[nxt_load] = [wload(srcs) for srcs in tasks[nxt_load][0]]
                        in_use += len(tasks[nxt_load][0])
                        nxt_load += 1
                    tasks[ti][1](loaded.pop(ti))
                    in_use -= len(tasks[ti][0])
            nbank[0] = 8
            moe_phase(l)
            ple_phase(l)
            nbank[0] = 6
        final_phase()
        P.emit()
        nops = {k: len(v) for k, v in P.ops.items()}
        print("ops per engine:", nops)
    return nc


def _host_layout(inp):
    f = np.float32
    w_in = np.asarray(inp["w_in"], f)
    swap = np.arange(512).reshape(8, 64)
    swap = np.concatenate([swap[:, 32:], swap[:, :32]], axis=1).reshape(-1)
    q, k, v = w_in[:, :, 0:512], w_in[:, :, 512:1024], w_in[:, :, 1024:1536]
    rest = w_in[:, :, 1536:]
    win = np.ascontiguousarray(np.concatenate([q, q[:, :, swap], k, k[:, :, swap], v, rest], axis=2))
    assert win.shape[2] == WIN_EXT
    wr = np.ascontiguousarray(np.concatenate([np.asarray(inp["w_route_group"], f), np.asarray(inp["w_route_expert"], f)], axis=2))
    pp = np.zeros((128, L * PPL), f)

    def cols(vec, n):
        return np.asarray(vec, f).reshape(n, 128).T
    for l in range(L):
        b = l * PPL
        pp[:, b + 0:b + 8] = cols(inp["norm_mix"][l], 8)
        pp[:, b + 8:b + 16] = cols(inp["norm_ffn"][l], 8)
        pp[:, b + 168:b + 176] = np.asarray(inp["norm_ffn"][l], f).reshape(128, 8)
        pp[:, b + 16:b + 24] = cols(inp["norm_ple"][l], 8)
        pp[:, b + 24:b + 32] = cols(inp["b_conv_out"][l], 8)
        pp[:, b + 32:b + 36] = cols(inp["b_dw"][l], 4)
        pp[:, b + 36:b + 40] = cols(inp["ln_conv_g"][l], 4)
        pp[:, b + 40:b + 44] = cols(inp["ln_conv_b"][l], 4)
        wd = np.asarray(inp["w_dw"][l], f)
        for j in range(31):
            pp[:, b + 44 + 4 * j:b + 48 + 4 * j] = cols(wd[j], 4)
    bc = np.zeros((128, 72 + D), f)
    for l in range(L):
        bc[:, l * 36:l * 36 + 4] = np.asarray(inp["b_route_group"][l], f)[None, :]
        bc[:, l * 36 + 4:l * 36 + 36] = np.asarray(inp["b_route_expert"][l], f)[None, :]
    bc[:, 72:] = np.asarray(inp["norm_final"], f)[None, :]
    inv = np.power(f(10000.0), -np.arange(0, 64, 2, dtype=f) / f(64)).astype(f)
    ang = (np.arange(S, dtype=f)[:, None] * inv[None, :]).astype(f)
    cs, sn = np.cos(ang).astype(f).T, np.sin(ang).astype(f).T
    cosT = np.concatenate([cs, cs, cs, cs], axis=0)
    sinT = np.concatenate([-sn, sn, -sn, sn], axis=0)
    kk = np.arange(128)[:, None]
    col = np.arange(S)[None, :]
    dl = col - kk
    cnt = ((dl >= 0) & (dl <= 128)).astype(f) + ((dl >= 0) & (dl <= 512) & (dl % 4 == 0)).astype(f) \
        + ((dl >= 0) & (dl <= 2048) & (dl % 16 == 0)).astype(f)
    shared = {
        "win": win, "wao": np.ascontiguousarray(inp["w_attn_out"], f), "wco": np.ascontiguousarray(inp["w_conv_out"], f),
        "wout": np.ascontiguousarray(inp["w_out"], f), "wr": wr,
        "wpg": np.ascontiguousarray(inp["w_ple_gate"], f), "wpe": np.ascontiguousarray(inp["w_ple_proj"], f),
        "pp": pp, "bc": bc, "cosT": np.ascontiguousarray(cosT), "sinT": np.ascontiguousarray(sinT),
        "maskT": np.ascontiguousarray(cnt), "ident": np.eye(128, dtype=f),
    }
    for l in range(L):
        shared[f"weg{l}"] = np.ascontiguousarray(inp["w_exp_gate"][l], f).reshape(8192, 2048)
        shared[f"weu{l}"] = np.ascontiguousarray(inp["w_exp_up"][l], f).reshape(8192, 2048)
        shared[f"wed{l}"] = np.ascontiguousarray(
            np.asarray(inp["w_exp_down"][l], f).reshape(NE, 4, 128, D).transpose(0, 2, 1, 3)).reshape(8192, 2048)
    cst = np.zeros((128, 384), f)
    cst[:, 320:352] = (256 * np.arange(32, dtype=f))[None, :]
    cst[:, 352:384] = (7936 - 256 * np.arange(32, dtype=f))[None, :]
    cst[:, 0:64] = np.arange(64, dtype=f)[None, :]
    cst[:, 64:128] = (2 * np.arange(128, dtype=f))[:, None]
    cst[:, 128:192] = (2 * np.arange(128, dtype=f) + 1)[:, None]
    cst[:, 192:320] = (np.arange(128)[:, None] < np.arange(128)[None, :]).astype(f)
    shared["cst"] = cst
    return shared


_NL = L
_DBG = 0


def kernel(**inputs):
    shared = _host_layout(inputs)
    x = np.asarray(inputs["x"], np.float32)
    p = np.asarray(inputs["p"], np.float32)
    nc = build_program(_NL)
    in_maps = []
    for b in range(8):
        m = dict(shared)
        m["x"] = np.ascontiguousarray(x[b])
        m["p"] = np.ascontiguousarray(p[:, b])
        in_maps.append(m)
    res = run_bass_kernel_spmd(nc, in_maps, core_ids=list(range(8)))
    return np.stack([r["out"] for r in res.results], axis=0).astype(np.float32)
```

```python
import contextlib
import numpy as np
import concourse.bass as bass
import concourse.mybir as mybir
from concourse.bass_utils import run_bass_kernel_spmd

F32 = mybir.dt.float32
BF16 = mybir.dt.bfloat16
ALU = mybir.AluOpType
AF = mybir.ActivationFunctionType
AX = mybir.AxisListType

S = 2048
D = 1024
L = 2
NT = 16
NE = 32
WIN_EXT = 5632
PPL = 176
EPS = 1e-6
BIG = 1.0e30

STRICT_SAME = True


class T:
    __slots__ = ("ap", "space", "lo", "hi", "w", "r", "ov", "name")

    def __init__(self, ap, space, lo, hi, name=""):
        self.ap, self.space, self.lo, self.hi, self.name = ap, space, lo, hi, name
        self.w = None
        self.r = {}
        self.ov = [self]


class Op:
    __slots__ = ("eng", "fn", "deps", "ddeps", "sig", "count", "dkey", "dval", "is_write")

    def __init__(self, eng, fn):
        self.eng, self.fn = eng, fn
        self.deps = []
        self.ddeps = {}
        self.sig = False
        self.count = None
        self.dkey = None
        self.dval = None
        self.is_write = False


class Prog:
    def __init__(self, nc):
        self.nc = nc
        self.ops = {e: [] for e in ("pe", "act", "dve", "pool", "sp")}
        self.tiles = {"sb": [], "ps": []}
        self.dma_counts = {}
        self.reg_init = {}
        self.regs = {}

    def tile(self, ap, space, lo, hi, name=""):
        t = T(ap, space, lo, hi, name)
        for o in self.tiles[space]:
            if o.lo < hi and lo < o.hi:
                o.ov.append(t)
                t.ov.append(o)
        self.tiles[space].append(t)
        return t

    def _dep_on(self, op, prod, raw=True):
        if prod is None or prod is op:
            return
        if prod.dkey is not None:
            k = prod.dkey
            if not raw and op.dkey == k and prod.dval is not None and prod.is_write:
                return
            v = self.dma_counts[k]
            if op.ddeps.get(k, 0) < v:
                op.ddeps[k] = v
            return
        if prod.eng == op.eng and op.dkey is None and (op.eng == "pe" or not STRICT_SAME):
            return
        prod.sig = True
        op.deps.append(prod)

    def op(self, eng, fn, reads=(), writes=(), dkey=None):
        o = Op(eng, fn)
        o.dkey = dkey
        o.is_write = len(writes) > 0
        for t in reads:
            for u in t.ov:
                self._dep_on(o, u.w)
        for t in writes:
            for u in t.ov:
                self._dep_on(o, u.w, raw=False)
                for rd in u.r.values():
                    self._dep_on(o, rd)
        if dkey is not None:
            self.dma_counts[dkey] = self.dma_counts.get(dkey, 0) + 16
            o.dval = self.dma_counts[dkey]
        stream = eng if dkey is None else ("dma", id(o))
        for t in reads:
            t.r[stream] = o
        for t in writes:
            t.w = o
            t.r = {}
        self.ops[eng].append(o)
        return o

    def emit(self):
        nc = self.nc
        with contextlib.ExitStack() as st:
            sems = {e: st.enter_context(nc.semaphore("s_" + e)) for e in ("pe", "act", "dve", "pool")}
            dsems = {k: st.enter_context(nc.semaphore("d_" + str(k))) for k in self.dma_counts}
            for e, lst in self.ops.items():
                c = 0
                for o in lst:
                    if o.dkey is None and o.sig:
                        c += 1
                        o.count = c
            block = st.enter_context(nc.Block())

            def run(engname, engobj):
                waited = {}
                if engname == "pool":
                    for nm, val in self.reg_init.items():
                        r = engobj.alloc_register("bnd_" + nm)
                        engobj.reg_mov(r, val)
                        self.regs[nm] = r
                for o in self.ops[engname]:
                    best = {}
                    for p in o.deps:
                        if best.get(p.eng, 0) < p.count:
                            best[p.eng] = p.count
                    for pe_, cnt in best.items():
                        if waited.get(("c", pe_), 0) < cnt:
                            engobj.wait_ge(sems[pe_], cnt)
                            waited[("c", pe_)] = cnt
                    for k, v in o.ddeps.items():
                        if waited.get(("d", k), 0) < v:
                            engobj.wait_ge(dsems[k], v)
                            waited[("d", k)] = v
                    ins = o.fn(engobj)
                    if o.dkey is not None:
                        ins.then_inc(dsems[o.dkey], 16)
                    elif o.sig:
                        ins.then_inc(sems[engname], 1)

            @block.sync
            def _(e):
                run("sp", e)

            @block.tensor
            def _(e):
                run("pe", e)

            @block.scalar
            def _(e):
                run("act", e)

            @block.vector
            def _(e):
                run("dve", e)

            @block.gpsimd
            def _(e):
                run("pool", e)


def build_program(nl=L):
    nc = bass.Bass("TRN2", target_bir_lowering=False)

    def din(name, shape):
        return nc.dram_tensor(name, list(shape), F32, kind="ExternalInput").ap()

    x_d = din("x", [S, D])
    p_d = din("p", [L, S, 256])
    win_d = din("win", [L, D, WIN_EXT])
    wao_d = din("wao", [L, 512, D])
    wco_d = din("wco", [L, 512, D])
    wout_d = din("wout", [L, D, D])
    wr_d = din("wr", [L, D, 36])
    weg_d = [nc.dram_tensor(f"weg{l}", [8192, 2048], F32, kind="ExternalInput") for l in range(L)]
    weu_d = [nc.dram_tensor(f"weu{l}", [8192, 2048], F32, kind="ExternalInput") for l in range(L)]
    wed_d = [nc.dram_tensor(f"wed{l}", [8192, 2048], F32, kind="ExternalInput") for l in range(L)]
    cst_d = din("cst", [128, 384])
    scr_d = nc.dram_tensor("moe_scr", [18432, D], F32)
    ys_d = scr_d
    sorted_d = scr_d[:, :].bitcast(BF16).rearrange("r (two c) -> (r two) c", two=2)
    wpg_d = din("wpg", [L, D, D])
    wpe_d = din("wpe", [L, 256, D])
    pp_d = din("pp", [128, L * PPL])
    bc_d = din("bc", [128, 72 + D])
    cos_d = din("cosT", [128, S])
    sin_d = din("sinT", [128, S])
    mask_d = din("maskT", [128, S])
    id_d = din("ident", [128, 128])
    out_d = nc.dram_tensor("out", [S, D], F32, kind="ExternalOutput").ap()
    dbg_d = nc.dram_tensor("dbg", [128, 512], F32, kind="ExternalOutput").ap() if _DBG == 6 else None

    NB = 212800
    with contextlib.ExitStack() as st:
        big = st.enter_context(nc.sbuf_tensor("arena", [128, NB // 4], F32))
        pbanks = [st.enter_context(nc.psum_tensor(f"pb{i}", [128, 512], F32)) for i in range(8)]
        P = Prog(nc)
        P.reg_init = {'b8191': 8191, 'b11775': 11775, 'b36351': 24576 + 11775}
        top = [0]

        def alloc(nbytes, at=None):
            nbytes = (nbytes + 31) // 32 * 32
            if at is None:
                at = top[0]
                top[0] = at + nbytes
            assert at + nbytes <= NB, ("SBUF overflow", at, nbytes)
            return at, at + nbytes

        def mk(n, dtype, name, at=None):
            es = 2 if dtype == BF16 else 4
            lo, hi = alloc(n * es, at)
            ap = big[:, lo // 4:hi // 4]
            if dtype != F32:
                ap = ap.bitcast(dtype)
            return P.tile(ap[:, 0:n], "sb", lo, hi, name)

        ps = [P.tile(pbanks[i][:, :], "ps", i * 2048, (i + 1) * 2048, f"ps{i}") for i in range(8)]
        rr = {"b": 0, "s": 0, "m": 0}

        nbank = [6]

        def bank():
            rr["b"] = (rr["b"] + 1) % nbank[0]
            return ps[rr["b"]]

        X = [[mk(512, F32, f"x{t}_{h}") for h in range(2)] for t in range(NT)]
        x0 = X[0][0].lo

        def xrow(t):
            lo = x0 + t * 4096
            return big[:, lo // 4: lo // 4 + 1024]
        ident = mk(128, F32, "ident")
        identb = mk(128, BF16, "identb")
        ones512 = mk(128, F32, "ones512")
        maskT = mk(S, BF16, "mask")
        pp = mk(L * PPL, F32, "pp")
        bcp = mk(72, F32, "bc")
        scr = [mk(512, F32, f"scr{i}") for i in range(5)]
        smalls = [mk(36, F32, f"sm{i}") for i in range(18)]
        PH0 = top[0]

        def scratch():
            rr["s"] = (rr["s"] + 1) % 5
            return scr[rr["s"]]

        def small():
            rr["m"] = (rr["m"] + 1) % 18
            return smalls[rr["m"]]

        top[0] = PH0
        kT = [[mk(512, BF16, f"kT{c}_{g}") for g in range(4)] for c in range(4)]
        vA = [mk(520, BF16, f"vA{t}") for t in range(NT)]
        hT = [mk(512, BF16, f"hT{c}") for c in range(8)]
        ring = [mk(2048, BF16, f"ring{i}") for i in range(6)]
        qT = [mk(512, BF16, f"qT{c}") for c in range(4)]
        cos2 = [mk(512, F32, f"cosg{i}") for i in range(2)]
        sin2 = [mk(512, F32, f"sing{i}") for i in range(2)]
        U = [mk(542, BF16, f"u{c}") for c in range(4)]
        dgs = [mk(128, BF16, f"dg{j}") for j in range(31)]
        junk_mix = mk(1024, BF16, "junk_mix", at=dgs[0].lo)
        Y = [mk(512, F32, f"y{c}") for c in range(4)]
        sT = [mk(512, BF16, f"sT{c}") for c in range(4)]
        pexp = [mk(512, BF16, f"pexp{i}") for i in range(3)]
        pmsk = [mk(512, BF16, f"pmsk{i}") for i in range(3)]
        otok = [mk(512, BF16, f"otok{j}") for j in range(4)]
        oT = [mk(512, BF16, f"oT{c}") for c in range(4)]
        mg = [mk(512, BF16, f"mg{c}") for c in range(8)]
        xnb = [mk(1024, BF16, f"xnb{i}", at=otok[0].lo + i * 2048) for i in range(4)]
        MIX_END = top[0]

        top[0] = PH0
        h2T = [[mk(512, BF16, f"h2T{c}_{g}") for g in range(4)] for c in range(8)]
        H2LO = h2T[0][0].lo
        xnb2 = [mk(1024, BF16, f"xnb2_{i}") for i in range(4)]
        XNLO = xnb2[0].lo
        junk_moe = mk(1024, BF16, "junk_moe")
        wrt = mk(8 * 36, BF16, "wrt")
        OH1 = [mk(32, F32, f"oh1_{t}") for t in range(NT)]
        OH2 = [mk(32, F32, f"oh2_{t}") for t in range(NT)]
        ABF = [mk(32, BF16, f"abf{t}") for t in range(NT)]
        RK = [mk(32, F32, f"rk{t}") for t in range(NT)]
        G12 = [mk(2, F32, f"g12_{t}") for t in range(NT)]
        DF = [mk(2, F32, f"df{t}") for t in range(NT)]
        DI = [mk(2, mybir.dt.int32, f"di{t}") for t in range(NT)]
        DIS = [mk(2, mybir.dt.int32, f"dis{t}") for t in range(NT)]
        cst = mk(384, F32, "cst")
        np_t = mk(32, F32, "np_t")
        oend_t = mk(32, F32, "oend")
        ops_t = mk(32, F32, "ops")
        q_t = mk(32, F32, "q_t")
        ltri = mk(128, BF16, "ltri")
        onesb = mk(128, BF16, "onesb")
        cntn = mk(32, F32, "cntn")
        nn_t = mk(32, F32, "nn")
        end_t = mk(32, F32, "end")
        psb_t = mk(32, F32, "psb")
        ebacc = mk(64, F32, "ebacc")
        idxg = mk(64, mybir.dt.int32, "idxg")
        idxd = mk(64, mybir.dt.int32, "idxd")
        GT = mk(1024, BF16, "GT")
        xs_t = [mk(1024, BF16, f"xs{i}") for i in range(2)]
        xg_t = [mk(1024, BF16, f"xg{i}") for i in range(4)]
        xbT = [mk(1024, BF16, f"xbT{i}") for i in range(2)]
        hbt = [mk(512, BF16, f"hbt{i}") for i in range(2)]
        hbT = [mk(512, BF16, f"hbT{i}") for i in range(2)]
        yst = [mk(1024, F32, f"yst{i}") for i in range(2)]
        yst.append(mk(1024, F32, "yst2", at=xs_t[0].lo))
        yst.append(mk(1024, F32, "yst3", at=OH1[0].lo))
        PH1 = top[0]
        ew = [[mk(4096, BF16, f"ew{s}_{m}", at=(H2LO + m * 8192) if s == 0 else None) for m in range(3)] for s in range(2)]
        ycmb = [mk(1024, F32, f"ycmb{i}", at=XNLO + i * 4096) for i in range(2)]
        wpe = mk(2048, BF16, "wpe")
        pT = [mk(S, BF16, f"pT{i}") for i in range(2)]
        pst = [mk(256, F32, f"pst{i}") for i in range(2)]
        MOE_END = top[0]
        top[0] = PH1
        wpg = [mk(4096, BF16, f"wpg{i}") for i in range(2)]
        ost = [mk(1024, F32, f"ost{i}") for i in range(2)]
        nfin = mk(D, F32, "nfin")
        PLE_END = top[0]
        print("SBUF bytes/partition: mixer", MIX_END, "moe", MOE_END, "ple", PLE_END, "limit", NB)

        dram_tiles = {}
        _dn = [0]
        for t_ in range(NT):
            for a_ in range(2):
                _dn[0] += 1
                dram_tiles[("sc", t_, a_)] = P.tile(None, "sb", 10 ** 9 + 10 * _dn[0], 10 ** 9 + 10 * _dn[0] + 1, "scd")
        for b_ in range(92):
            _dn[0] += 1
            dram_tiles[("ys", b_)] = P.tile(None, "sb", 10 ** 9 + 10 * _dn[0], 10 ** 9 + 10 * _dn[0] + 1, "ysd")

        dram_tiles[('gser',)] = P.tile(None, 'sb', 2 * 10 ** 9, 2 * 10 ** 9 + 1, 'gser')
        P.op("sp", lambda e: e.dma_start(out=ident.ap, in_=id_d), writes=[ident], dkey="c0")
        P.op("sp", lambda e: e.dma_start(out=pp.ap, in_=pp_d), writes=[pp], dkey="c0")
        P.op("sp", lambda e: e.dma_start(out=bcp.ap, in_=bc_d[:, 0:72]), writes=[bcp], dkey="c0")
        P.op("pool", lambda e: e.dma_start(out=maskT.ap, in_=mask_d), writes=[maskT], dkey="c1")
        P.op("pool", lambda e: e.dma_start(out=identb.ap, in_=id_d), writes=[identb], dkey="c1")
        P.op("pool", lambda e: e.memset(ones512.ap, 1.0 / 512), writes=[ones512])
        for g in range(4):
            for i in range(4):
                t = 4 * g + i
                for h in range(2):
                    P.op("sp", lambda e, t=t, h=h: e.dma_start(out=X[t][h].ap, in_=x_d[t * 128:(t + 1) * 128, h * 512:(h + 1) * 512]),
                         writes=[X[t][h]], dkey=f"xg{g}")

        def ppc(col):
            return pp.ap[:, col:col + 1]

        def row_rstd(t, junk):
            ss = small()
            P.op("act", lambda e: e.activation(out=junk.ap, in_=xrow(t), func=AF.Square, accum_out=ss.ap[:, 0:1]),
                 reads=[X[t][0], X[t][1]], writes=[ss, junk])
            P.op("act", lambda e: e.activation(out=ss.ap[:, 1:2], in_=ss.ap[:, 0:1], func=AF.Sqrt, bias=EPS, scale=1.0 / D),
                 reads=[ss], writes=[ss])
            P.op("dve", lambda e: e.reciprocal(out=ss.ap[:, 2:3], in_=ss.ap[:, 1:2]), reads=[ss], writes=[ss])
            return ss

        def norm_T(g, gcol, dst, xn_tiles):
            for i in range(4):
                t = 4 * g + i
                ss = row_rstd(t, junk_mix if xn_tiles is xnb else junk_moe)
                P.op("dve", lambda e, t=t, i=i, ss=ss: e.tensor_scalar(out=xn_tiles[i].ap, in0=xrow(t), scalar1=ss.ap[:, 2:3],
                                                                       scalar2=None, op0=ALU.mult),
                     reads=[X[t][0], X[t][1], ss], writes=[xn_tiles[i]])
            for c in range(8):
                b = bank()
                bv = b.ap.bitcast(BF16)
                for i in range(4):
                    P.op("pe", lambda e, i=i, c=c, bv=bv: e.transpose(out=bv[:, i * 128:(i + 1) * 128],
                                                                      in_=xn_tiles[i].ap[:, c * 128:(c + 1) * 128], identity=identb.ap),
                         reads=[xn_tiles[i], identb], writes=[b])
                P.op("act", lambda e, c=c, bv=bv: e.activation(out=dst[c].ap, in_=bv[:, 0:512], func=AF.Copy, scale=ppc(gcol + c)),
                     reads=[b, pp], writes=[dst[c]])

        def mm_acc(out_ap, out_t, pairs, extra_reads):
            n = len(pairs)
            for k, (lh, rh) in enumerate(pairs):
                P.op("pe", lambda e, lh=lh, rh=rh, k=k: e.matmul(out_ap, lhsT=lh, rhs=rh, start=(k == 0), stop=(k == n - 1)),
                     reads=extra_reads, writes=[out_t])

        ring_i = [0]

        def wload(srcs, key_reads=()):
            ring_i[0] = (ring_i[0] + 1) % 6
            slot = ring[ring_i[0]]
            si = ring_i[0]
            views = []
            off = 0
            for (src, k, c) in srcs:
                v = slot.ap[:, off:off + k * c].rearrange("p (k c) -> p k c", k=k)
                views.append(v)
                P.op("pool", lambda e, v=v, src=src: e.dma_start(out=v, in_=src), writes=[slot], dkey=f"ring{si}")
                off += k * c
            return slot, views

        def mixer_group(l, g):
            pb = l * PPL
            norm_T(g, pb + 0, hT, xnb)
            gi = l * 4 + g
            cosg, sing = cos2[gi % 2], sin2[gi % 2]

            def cs_load(gi_):
                g_ = gi_ % 4
                P.op("sp", lambda e: e.dma_start(out=cos2[gi_ % 2].ap, in_=cos_d[:, g_ * 512:(g_ + 1) * 512]), writes=[cos2[gi_ % 2]], dkey=f"cs{gi_ % 2}")
                P.op("sp", lambda e: e.dma_start(out=sin2[gi_ % 2].ap, in_=sin_d[:, g_ * 512:(g_ + 1) * 512]), writes=[sin2[gi_ % 2]], dkey=f"cs{gi_ % 2}")
            if g == 0:
                cs_load(gi)
            if g < 3:
                cs_load(gi + 1)
            winl = win_d[l].rearrange("(k p) c -> p k c", p=128)
            waol = wao_d[l].rearrange("(k p) c -> p k c", p=128)
            wcol = wco_d[l].rearrange("(k p) c -> p k c", p=128)
            woutl = wout_d[l].rearrange("(k p) c -> p k c", p=128)

            def win_blk(c0):
                return [(winl[:, :, c0:c0 + 256], 8, 256)]

            tasks = []

            def rope_task(base, dst_fn):
                for b in range(2):
                    def comp(slots, b=b):
                        (s1, v1), (s2, v2) = slots
                        for cc in range(2):
                            c = 2 * b + cc
                            bq = bank()
                            mm_acc(bq.ap, bq, [(v1[0][:, k, cc * 128:(cc + 1) * 128], hT[k].ap) for k in range(8)], [s1] + hT)
                            bs = bank()
                            mm_acc(bs.ap, bs, [(v2[0][:, k, cc * 128:(cc + 1) * 128], hT[k].ap) for k in range(8)], [s2] + hT)
                            t1 = scratch()
                            P.op("dve", lambda e, t1=t1, bq=bq: e.tensor_tensor(out=t1.ap, in0=bq.ap, in1=cosg.ap, op=ALU.mult),
                                 reads=[bq, cosg], writes=[t1])
                            t2 = scratch()
                            P.op("dve", lambda e, t2=t2, bs=bs: e.tensor_tensor(out=t2.ap, in0=bs.ap, in1=sing.ap, op=ALU.mult),
                                 reads=[bs, sing], writes=[t2])
                            dst = dst_fn(c)
                            P.op("dve", lambda e, t1=t1, t2=t2, dst=dst: e.tensor_tensor(out=dst.ap, in0=t1.ap, in1=t2.ap, op=ALU.add),
                                 reads=[t1, t2], writes=[dst])
                    tasks.append(([win_blk(base + 256 * b), win_blk(base + 512 + 256 * b)], comp))
            rope_task(0, lambda c: qT[c])
            rope_task(1024, lambda c: kT[c][g])

            for b in range(2):
                def comp(slots, b=b):
                    (s1, v1), = slots
                    for i in range(4):
                        t = 4 * g + i
                        bv = bank()
                        mm_acc(bv.ap[:, 0:256], bv, [(hT[k].ap[:, i * 128:(i + 1) * 128], v1[0][:, k, :]) for k in range(8)], [s1] + hT)
                        vv = vA[t].ap.rearrange("p (h e) -> p h e", h=8)
                        if b == 0:
                            P.op("dve", lambda e, vv=vv: e.memset(vv[:, :, 64:65], 1.0), writes=[vA[t]])
                        P.op("act", lambda e, vv=vv, bv=bv, b=b: e.activation(
                            out=vv[:, 4 * b:4 * b + 4, 0:64], in_=bv.ap[:, 0:256].rearrange("p (h e) -> p h e", h=4), func=AF.Copy),
                            reads=[bv], writes=[vA[t]])
                tasks.append(([win_blk(2048 + 256 * b)], comp))

            for b in range(2):
                def comp(slots, b=b):
                    (s1, v1), (s2, v2) = slots
                    for cc in range(2):
                        c = 2 * b + cc
                        bu = bank()
                        mm_acc(bu.ap, bu, [(v1[0][:, k, cc * 128:(cc + 1) * 128], hT[k].ap) for k in range(8)], [s1] + hT)
                        bg = bank()
                        mm_acc(bg.ap, bg, [(v2[0][:, k, cc * 128:(cc + 1) * 128], hT[k].ap) for k in range(8)], [s2] + hT)
                        sg = scratch()
                        P.op("act", lambda e, sg=sg, bg=bg: e.activation(out=sg.ap, in_=bg.ap, func=AF.Sigmoid), reads=[bg], writes=[sg])
                        if g == 0:
                            P.op("dve", lambda e, c=c: e.memset(U[c].ap[:, 0:30], 0.0), writes=[U[c]])
                        else:
                            P.op("dve", lambda e, c=c: e.tensor_copy(out=U[c].ap[:, 0:30], in_=U[c].ap[:, 512:542]), reads=[U[c]], writes=[U[c]])
                        P.op("dve", lambda e, c=c, bu=bu, sg=sg: e.tensor_tensor(out=U[c].ap[:, 30:542], in0=bu.ap, in1=sg.ap, op=ALU.mult),
                             reads=[bu, sg], writes=[U[c]])
                        wc0 = pb + 44
                        for j in range(31):
                            if j % 2 == 0:
                                P.op("dve", lambda e, j=j, c=c: e.tensor_scalar(out=dgs[j].ap, in0=identb.ap, scalar1=ppc(wc0 + 4 * j + c), scalar2=None,
                                                                              op0=ALU.mult), reads=[identb, pp], writes=[dgs[j]])
                            else:
                                P.op("act", lambda e, j=j, c=c: e.activation(out=dgs[j].ap, in_=identb.ap, func=AF.Copy, scale=ppc(wc0 + 4 * j + c)),
                                     reads=[identb, pp], writes=[dgs[j]])
                        by = bank()
                        for j in range(31):
                            P.op("pe", lambda e, j=j, c=c, by=by: e.matmul(by.ap, lhsT=dgs[j].ap, rhs=U[c].ap[:, j:j + 512], start=(j == 0), stop=(j == 30)),
                                 reads=[dgs[j], U[c]], writes=[by])
                        P.op("dve", lambda e, c=c, by=by: e.tensor_scalar(out=Y[c].ap, in0=by.ap, scalar1=ppc(pb + 32 + c), scalar2=None, op0=ALU.add),
                             reads=[by, pp], writes=[Y[c]])
                tasks.append(([win_blk(2560 + 256 * b), win_blk(3072 + 256 * b)], comp))

            def comp_ln_attn(slots):
                bm = bank()
                mm_acc(bm.ap, bm, [(ones512.ap, Y[c].ap) for c in range(4)], [ones512] + Y)
                be = bank()
                for c in range(4):
                    sq = scratch()
                    P.op("act", lambda e, sq=sq, c=c: e.activation(out=sq.ap, in_=Y[c].ap, func=AF.Square), reads=[Y[c]], writes=[sq])
                    P.op("pe", lambda e, sq=sq, c=c: e.matmul(be.ap, lhsT=ones512.ap, rhs=sq.ap, start=(c == 0), stop=(c == 3)),
                         reads=[ones512, sq], writes=[be])
                msq = scratch()
                P.op("act", lambda e: e.activation(out=msq.ap, in_=bm.ap, func=AF.Square), reads=[bm], writes=[msq])
                var = scratch()
                P.op("dve", lambda e: e.tensor_tensor(out=var.ap, in0=be.ap, in1=msq.ap, op=ALU.subtract), reads=[be, msq], writes=[var])
                P.op("act", lambda e: e.activation(out=var.ap, in_=var.ap, func=AF.Sqrt, bias=EPS, scale=1.0), reads=[var], writes=[var])
                P.op("dve", lambda e: e.reciprocal(out=var.ap, in_=var.ap), reads=[var], writes=[var])
                for c in range(4):
                    yn = scratch()
                    P.op("dve", lambda e, yn=yn, c=c: e.tensor_tensor(out=yn.ap, in0=Y[c].ap, in1=bm.ap, op=ALU.subtract),
                         reads=[Y[c], bm], writes=[yn])
                    P.op("dve", lambda e, yn=yn: e.tensor_tensor(out=yn.ap, in0=yn.ap, in1=var.ap, op=ALU.mult), reads=[yn, var], writes=[yn])
                    P.op("act", lambda e, yn=yn, c=c: e.activation(out=sT[c].ap, in_=yn.ap, func=AF.Silu, scale=ppc(pb + 36 + c), bias=ppc(pb + 40 + c)),
                         reads=[yn, pp], writes=[sT[c]])
                nkt = 4 * g + 4
                items = [(h, kt) for h in range(8) for kt in range(nkt)]
                stg = {}

                def S_(n):
                    h, kt = items[n]
                    c, pbase = h // 2, (h % 2) * 64
                    j0 = max(kt - 4 * g, 0)
                    c0 = j0 * 128
                    bs = bank()
                    ktile = kT[c][kt // 4]
                    P.op("pe", lambda e: e.matmul(
                        bs.ap[:, c0:512], lhsT=ktile.ap[pbase:pbase + 64, (kt % 4) * 128:(kt % 4) * 128 + 128],
                        rhs=qT[c].ap[pbase:pbase + 64, c0:512], start=True, stop=True),
                        reads=[ktile, qT[c]], writes=[bs])
                    pe_t = pexp[n % 3]
                    P.op("act", lambda e: e.activation(out=pe_t.ap[:, c0:512], in_=bs.ap[:, c0:512], func=AF.Exp, scale=0.125),
                         reads=[bs], writes=[pe_t])
                    pm_t = pmsk[n % 3]
                    o0 = 4 * g + j0 - kt
                    P.op("dve", lambda e: e.tensor_tensor(
                        out=pm_t.ap[:, c0:512], in0=pe_t.ap[:, c0:512], in1=maskT.ap[:, o0 * 128:o0 * 128 + 512 - c0], op=ALU.mult),
                        reads=[pe_t, maskT], writes=[pm_t])
                    stg[n] = (pm_t, j0)

                def V_(n):
                    h, kt = items[n]
                    pm_t, j0 = stg.pop(n)
                    accb = ps[6 + (h % 2)]
                    acc = accb.ap[:, 0:260].rearrange("p (j e) -> p j e", j=4)
                    for j in range(j0, 4):
                        P.op("pe", lambda e, j=j: e.matmul(
                            acc[:, j, :], lhsT=pm_t.ap[:, j * 128:(j + 1) * 128], rhs=vA[kt].ap[:, h * 65:(h + 1) * 65],
                            start=(kt == 0 and j == 0), stop=(kt == nkt - 1 and j == 3)),
                            reads=[pm_t, vA[kt]], writes=[accb])
                    if kt == nkt - 1:
                        rc = small()
                        P.op("dve", lambda e: e.reciprocal(out=rc.ap[:, 0:4], in_=acc[:, :, 64]), reads=[accb], writes=[rc])
                        for j in range(4):
                            P.op("dve", lambda e, j=j: e.tensor_scalar(
                                out=otok[j].ap[:, h * 64:(h + 1) * 64], in0=acc[:, j, 0:64], scalar1=rc.ap[:, j:j + 1], scalar2=None, op0=ALU.mult),
                                reads=[accb, rc], writes=[otok[j]])
                LA = 2
                for n in range(min(LA, len(items))):
                    S_(n)
                for n in range(len(items)):
                    if n + LA < len(items):
                        S_(n + LA)
                    V_(n)
                for c in range(4):
                    b = bank()
                    bv = b.ap.bitcast(BF16)
                    for j in range(4):
                        P.op("pe", lambda e, bv=bv, j=j, c=c: e.transpose(out=bv[:, j * 128:(j + 1) * 128], in_=otok[j].ap[:, c * 128:(c + 1) * 128],
                                                                          identity=identb.ap), reads=[otok[j], identb], writes=[b])
                    P.op("act", lambda e, bv=bv, c=c: e.activation(out=oT[c].ap, in_=bv[:, 0:512], func=AF.Copy), reads=[b], writes=[oT[c]])
            tasks.append(([], comp_ln_attn))

            for ob in range(4):
                def comp(slots, ob=ob):
                    (s1, v1), (s2, v2), (s3, v3) = slots
                    for cc in range(2):
                        oc = 2 * ob + cc
                        sl = slice(cc * 128, (cc + 1) * 128)
                        bga = bank()
                        mm_acc(bga.ap, bga, [(v1[0][:, k, sl], hT[k].ap) for k in range(8)], [s1] + hT)
                        bgb = bank()
                        mm_acc(bgb.ap, bgb, [(v2[0][:, k, sl], hT[k].ap) for k in range(8)], [s2] + hT)
                        bya = bank()
                        mm_acc(bya.ap, bya, [(v3[0][:, k, sl], oT[k].ap) for k in range(4)], [s3] + oT)
                        byb = bank()
                        mm_acc(byb.ap, byb, [(v3[1][:, k, sl], sT[k].ap) for k in range(4)], [s3] + sT)
                        sga = scratch()
                        P.op("act", lambda e, sga=sga, bga=bga: e.activation(out=sga.ap, in_=bga.ap, func=AF.Sigmoid), reads=[bga], writes=[sga])
                        sgb = scratch()
                        P.op("act", lambda e, sgb=sgb, bgb=bgb: e.activation(out=sgb.ap, in_=bgb.ap, func=AF.Sigmoid), reads=[bgb], writes=[sgb])
                        P.op("dve", lambda e, sga=sga, bya=bya: e.tensor_tensor(out=sga.ap, in0=bya.ap, in1=sga.ap, op=ALU.mult),
                             reads=[bya, sga], writes=[sga])
                        P.op("dve", lambda e, sgb=sgb, byb=byb, oc=oc: e.scalar_tensor_tensor(out=sgb.ap, in0=byb.ap, scalar=ppc(pb + 24 + oc), in1=sgb.ap,
                                                                                            op0=ALU.add, op1=ALU.mult),
                             reads=[byb, sgb, pp], writes=[sgb])
                        P.op("dve", lambda e, sga=sga, sgb=sgb, oc=oc: e.tensor_tensor(out=mg[oc].ap, in0=sga.ap, in1=sgb.ap, op=ALU.add),
                             reads=[sga, sgb], writes=[mg[oc]])
                tasks.append(([win_blk(3584 + 256 * ob), win_blk(4608 + 256 * ob),
                               [(waol[:, :, 256 * ob:256 * ob + 256], 4, 256), (wcol[:, :, 256 * ob:256 * ob + 256], 4, 256)]], comp))

            for nb_ in range(4):
                def comp(slots, nb_=nb_):
                    (s1, v1), = slots
                    for i in range(4):
                        t = 4 * g + i
                        b = bank()
                        mm_acc(b.ap[:, 0:256], b, [(mg[k].ap[:, i * 128:(i + 1) * 128], v1[0][:, k, :]) for k in range(8)], [s1] + mg)
                        xt = X[t][nb_ // 2]
                        xs = xt.ap[:, (nb_ % 2) * 256:(nb_ % 2) * 256 + 256]
                        P.op("dve", lambda e, xs=xs, b=b: e.tensor_tensor(out=xs, in0=b.ap[:, 0:256], in1=xs, op=ALU.add),
                             reads=[b, xt], writes=[xt])
                tasks.append(([[(woutl[:, :, 256 * nb_:256 * nb_ + 256], 8, 256)]], comp))

            return tasks

        def moe_phase(l):
            pb = l * PPL
            I32 = mybir.dt.int32
            P.op("sp", lambda e: e.dma_start(out=cst.ap, in_=cst_d), writes=[cst], dkey="cst")
            P.op("pool", lambda e: e.dma_start(out=ltri.ap, in_=cst_d[:, 192:320]), writes=[ltri], dkey="cstb")
            P.op("pool", lambda e: e.memset(onesb.ap, 1.0), writes=[onesb])
            for g in range(4):
                norm_T(g, pb + 8, [h2T[c][g] for c in range(8)], xnb2)
            P.op("pool", lambda e: e.dma_start(out=wrt.ap.rearrange("p (k c) -> p k c", k=8), in_=wr_d[l].rearrange("(k p) c -> p k c", p=128)),
                 writes=[wrt], dkey="wr")
            wrv = wrt.ap.rearrange("p (k c) -> p k c", k=8)

            def route_tile(t):
                g, i = t // 4, t % 4
                b = bank()
                mm_acc(b.ap[:, 0:36], b, [(h2T[k][g].ap[:, i * 128:(i + 1) * 128], wrv[:, k, :]) for k in range(8)],
                       [wrt] + [h2T[k][g] for k in range(8)])
                lg = small()
                P.op("dve", lambda e, lg=lg, b=b: e.tensor_tensor(out=lg.ap, in0=b.ap[:, 0:36], in1=bcp.ap[:, l * 36:(l + 1) * 36], op=ALU.add),
                     reads=[b, bcp], writes=[lg])
                w1 = small()
                W = w1.ap
                P.op("dve", lambda e, lg=lg, W=W: e.tensor_reduce(out=W[:, 0:1], in_=lg.ap[:, 0:4], axis=AX.X, op=ALU.max), reads=[lg], writes=[w1])
                P.op("dve", lambda e, W=W: e.tensor_scalar(out=W[:, 1:2], in0=W[:, 0:1], scalar1=-1.0, scalar2=None, op0=ALU.mult), reads=[w1], writes=[w1])
                P.op("act", lambda e, lg=lg, W=W: e.activation(out=W[:, 8:12], in_=lg.ap[:, 0:4], func=AF.Exp, bias=W[:, 1:2], scale=1.0, accum_out=W[:, 2:3]),
                     reads=[lg, w1], writes=[w1])
                P.op("dve", lambda e, W=W: e.reciprocal(out=W[:, 3:4], in_=W[:, 2:3]), reads=[w1], writes=[w1])
                P.op("dve", lambda e, lg=lg, W=W: e.tensor_scalar(out=W[:, 4:8], in0=lg.ap[:, 0:4], scalar1=W[:, 0:1], scalar2=None, op0=ALU.is_equal),
                     reads=[lg, w1], writes=[w1])
                P.op("dve", lambda e, W=W: e.tensor_scalar(out=W[:, 4:8], in0=W[:, 4:8], scalar1=BIG, scalar2=-BIG, op0=ALU.mult, op1=ALU.add),
                     reads=[w1], writes=[w1])
                lem = small()
                for gg in range(4):
                    P.op("dve", lambda e, lem=lem, lg=lg, W=W, gg=gg: e.tensor_scalar(out=lem.ap[:, gg * 8:(gg + 1) * 8], in0=lg.ap[:, 4 + gg * 8:12 + gg * 8],
                                                                                  scalar1=W[:, 4 + gg:5 + gg], scalar2=None, op0=ALU.add),
                         reads=[lg, w1], writes=[lem])
                w2 = small()
                V = w2.ap
                oh1, oh2 = OH1[t], OH2[t]
                lem2 = small()
                P.op("dve", lambda e, lem=lem, V=V: e.tensor_reduce(out=V[:, 0:1], in_=lem.ap[:, 0:32], axis=AX.X, op=ALU.max), reads=[lem], writes=[w2])
                P.op("dve", lambda e, lem=lem, V=V, oh1=oh1: e.tensor_scalar(out=oh1.ap, in0=lem.ap[:, 0:32], scalar1=V[:, 0:1], scalar2=None, op0=ALU.is_equal),
                     reads=[lem, w2], writes=[oh1])
                P.op("dve", lambda e, lem=lem, lem2=lem2, oh1=oh1: e.scalar_tensor_tensor(out=lem2.ap[:, 0:32], in0=oh1.ap, scalar=-BIG, in1=lem.ap[:, 0:32],
                                                                                  op0=ALU.mult, op1=ALU.add), reads=[oh1, lem], writes=[lem2])
                P.op("dve", lambda e, lem2=lem2, V=V: e.tensor_reduce(out=V[:, 1:2], in_=lem2.ap[:, 0:32], axis=AX.X, op=ALU.max), reads=[lem2], writes=[w2])
                P.op("dve", lambda e, lem2=lem2, V=V, oh2=oh2: e.tensor_scalar(out=oh2.ap, in0=lem2.ap[:, 0:32], scalar1=V[:, 1:2], scalar2=None, op0=ALU.is_equal),
                     reads=[lem2, w2], writes=[oh2])
                P.op("dve", lambda e, V=V: e.tensor_tensor(out=V[:, 2:3], in0=V[:, 1:2], in1=V[:, 0:1], op=ALU.subtract), reads=[w2], writes=[w2])
                P.op("act", lambda e, V=V: e.activation(out=V[:, 3:4], in_=V[:, 2:3], func=AF.Exp), reads=[w2], writes=[w2])
                P.op("dve", lambda e, V=V: e.tensor_scalar(out=V[:, 4:5], in0=V[:, 3:4], scalar1=1.0, scalar2=None, op0=ALU.add), reads=[w2], writes=[w2])
                P.op("dve", lambda e, V=V: e.reciprocal(out=V[:, 5:6], in_=V[:, 4:5]), reads=[w2], writes=[w2])
                P.op("dve", lambda e, V=V: e.tensor_tensor(out=V[:, 6:7], in0=V[:, 3:4], in1=V[:, 5:6], op=ALU.mult), reads=[w2], writes=[w2])
                P.op("dve", lambda e, V=V, W=W, t=t: e.tensor_scalar(out=G12[t].ap, in0=V[:, 5:7], scalar1=W[:, 3:4], scalar2=None, op0=ALU.mult),
                     reads=[w2, w1], writes=[G12[t]])
                P.op("dve", lambda e, oh1=oh1, oh2=oh2, t=t: e.tensor_tensor(out=ABF[t].ap, in0=oh1.ap, in1=oh2.ap, op=ALU.add),
                     reads=[oh1, oh2], writes=[ABF[t]])

            def capture(fn, *a):
                rec = []
                P.op = lambda *args, **kw: rec.append((args, kw))
                try:
                    fn(*a)
                finally:
                    del P.op
                return rec
            RB = 3
            for t0 in range(0, NT, RB):
                recs = [capture(route_tile, t) for t in range(t0, min(t0 + RB, NT))]
                for k_ in range(max(len(r) for r in recs)):
                    for r in recs:
                        if k_ < len(r):
                            P.op(*r[k_][0], **r[k_][1])

            for t in range(NT):
                b = bank()
                for tp in range(t):
                    P.op("pe", lambda e, b=b, tp=tp: e.matmul(b.ap[:, 0:32], lhsT=onesb.ap, rhs=ABF[tp].ap, start=(tp == 0), stop=False),
                         reads=[onesb, ABF[tp]], writes=[b])
                P.op("pe", lambda e, b=b, t=t: e.matmul(b.ap[:, 0:32], lhsT=ltri.ap, rhs=ABF[t].ap, start=(t == 0), stop=True),
                     reads=[ltri, ABF[t]], writes=[b])
                P.op("act", lambda e, b=b, t=t: e.activation(out=RK[t].ap, in_=b.ap[:, 0:32], func=AF.Copy), reads=[b], writes=[RK[t]])
            bc_ = bank()
            for t in range(NT):
                P.op("pe", lambda e, t=t: e.matmul(bc_.ap[:, 0:32], lhsT=onesb.ap, rhs=ABF[t].ap, start=(t == 0), stop=(t == NT - 1)),
                     reads=[onesb, ABF[t]], writes=[bc_])
            P.op("act", lambda e: e.activation(out=cntn.ap, in_=bc_.ap[:, 0:32], func=AF.Copy), reads=[bc_], writes=[cntn])
            P.op("dve", lambda e: e.tensor_scalar(out=nn_t.ap, in0=cntn.ap, scalar1=0.0, scalar2=None, op0=ALU.is_gt), reads=[cntn], writes=[nn_t])
            for j in range(1, 16):
                P.op("dve", lambda e, j=j: e.scalar_tensor_tensor(out=nn_t.ap, in0=cntn.ap, scalar=128.0 * j, in1=nn_t.ap, op0=ALU.is_gt, op1=ALU.add),
                     reads=[cntn, nn_t], writes=[nn_t])
            P.op("dve", lambda e: e.tensor_scalar(out=np_t.ap, in0=nn_t.ap, scalar1=-2.0, scalar2=0.0, op0=ALU.add, op1=ALU.max), reads=[nn_t], writes=[np_t])
            P.op("dve", lambda e: e.tensor_copy(out=oend_t.ap[:, 0:1], in_=np_t.ap[:, 0:1]), reads=[np_t], writes=[oend_t])
            for ee in range(1, NE):
                P.op("dve", lambda e, ee=ee: e.tensor_tensor(out=oend_t.ap[:, ee:ee + 1], in0=oend_t.ap[:, ee - 1:ee], in1=np_t.ap[:, ee:ee + 1], op=ALU.add),
                     reads=[oend_t, np_t], writes=[oend_t])
            P.op("dve", lambda e: e.tensor_tensor(out=ops_t.ap, in0=oend_t.ap, in1=np_t.ap, op=ALU.subtract), reads=[oend_t, np_t], writes=[ops_t])
            P.op("dve", lambda e: e.scalar_tensor_tensor(out=q_t.ap, in0=ops_t.ap, scalar=128.0, in1=cst.ap[:, 352:384], op0=ALU.mult, op1=ALU.add),
                 reads=[ops_t, cst], writes=[q_t])
            P.op("dve", lambda e: e.tensor_scalar(out=ebacc.ap, in0=cst.ap[:, 0:64], scalar1=oend_t.ap[:, 0:1], scalar2=None, op0=ALU.is_ge),
                 reads=[cst, oend_t], writes=[ebacc])
            for ee in range(1, NE):
                P.op("dve", lambda e, ee=ee: e.scalar_tensor_tensor(out=ebacc.ap, in0=cst.ap[:, 0:64], scalar=oend_t.ap[:, ee:ee + 1], in1=ebacc.ap,
                                                                  op0=ALU.is_ge, op1=ALU.add), reads=[cst, oend_t, ebacc], writes=[ebacc])
            P.op("dve", lambda e: e.scalar_tensor_tensor(out=idxg.ap, in0=ebacc.ap, scalar=256.0, in1=cst.ap[:, 64:128], op0=ALU.mult, op1=ALU.add),
                 reads=[ebacc, cst], writes=[idxg])
            P.op("dve", lambda e: e.scalar_tensor_tensor(out=idxd.ap, in0=ebacc.ap, scalar=256.0, in1=cst.ap[:, 128:192], op0=ALU.mult, op1=ALU.add),
                 reads=[ebacc, cst], writes=[idxd])
            if _DBG == 5:
                P.op("dve", lambda e: e.tensor_copy(out=idxg.ap, in_=cst.ap[:, 64:128]), reads=[cst], writes=[idxg])
                P.op("dve", lambda e: e.tensor_copy(out=idxd.ap, in_=cst.ap[:, 128:192]), reads=[cst], writes=[idxd])
            for t in range(NT):
                tmp = small()
                sel = small()
                P.op("dve", lambda e, sel=sel, t=t: e.scalar_tensor_tensor(out=sel.ap[:, 0:32], in0=RK[t].ap, scalar=256.0, in1=q_t.ap, op0=ALU.is_ge, op1=ALU.mult),
                     reads=[RK[t], q_t], writes=[sel])
                P.op("dve", lambda e, tmp=tmp, t=t: e.tensor_tensor(out=tmp.ap[:, 0:32], in0=RK[t].ap, in1=cst.ap[:, 320:352], op=ALU.add),
                     reads=[RK[t], cst], writes=[tmp])
                P.op("dve", lambda e, tmp=tmp, sel=sel: e.tensor_tensor(out=tmp.ap[:, 0:32], in0=tmp.ap[:, 0:32], in1=sel.ap[:, 0:32], op=ALU.add),
                     reads=[tmp, sel], writes=[tmp])
                for a_, oh in enumerate((OH1[t], OH2[t])):
                    m_ = small()
                    P.op("dve", lambda e, m_=m_, tmp=tmp, oh=oh: e.tensor_tensor(out=m_.ap[:, 0:32], in0=tmp.ap[:, 0:32], in1=oh.ap, op=ALU.mult),
                         reads=[tmp, oh], writes=[m_])
                    P.op("dve", lambda e, m_=m_, t=t, a_=a_: e.tensor_reduce(out=DF[t].ap[:, a_:a_ + 1], in_=m_.ap[:, 0:32], axis=AX.X, op=ALU.add),
                         reads=[m_], writes=[DF[t]])
                P.op("dve", lambda e, t=t: e.tensor_copy(out=DI[t].ap, in_=DF[t].ap), reads=[DF[t]], writes=[DI[t]])
                P.op("dve", lambda e, t=t: e.tensor_scalar(out=DIS[t].ap, in0=DF[t].ap, scalar1=24576.0, scalar2=None, op0=ALU.add), reads=[DF[t]], writes=[DIS[t]])
            for k in range(8):
                P.op("dve", lambda e, k=k: e.tensor_scalar(out=GT.ap[:, k * 128:(k + 1) * 128], in0=onesb.ap, scalar1=ppc(pb + 168 + k), scalar2=None, op0=ALU.mult),
                     reads=[onesb, pp], writes=[GT])
            if _DBG == 6:
                dbt = scr[0]
                P.op("dve", lambda e: e.tensor_copy(out=dbt.ap[:, 0:32], in_=cntn.ap), reads=[cntn], writes=[dbt])
                P.op("dve", lambda e: e.tensor_copy(out=dbt.ap[:, 32:64], in_=nn_t.ap), reads=[nn_t], writes=[dbt])
                P.op("dve", lambda e: e.tensor_copy(out=dbt.ap[:, 64:96], in_=end_t.ap), reads=[end_t], writes=[dbt])
                P.op("dve", lambda e: e.tensor_copy(out=dbt.ap[:, 96:128], in_=psb_t.ap), reads=[psb_t], writes=[dbt])
                P.op("dve", lambda e: e.tensor_copy(out=dbt.ap[:, 128:192], in_=ebacc.ap), reads=[ebacc], writes=[dbt])
                P.op("dve", lambda e: e.tensor_copy(out=dbt.ap[:, 192:256], in_=idxg.ap), reads=[idxg], writes=[dbt])
                P.op("dve", lambda e: e.tensor_copy(out=dbt.ap[:, 256:258], in_=DF[0].ap), reads=[DF[0]], writes=[dbt])
                P.op("dve", lambda e: e.tensor_copy(out=dbt.ap[:, 258:260], in_=DI[0].ap), reads=[DI[0]], writes=[dbt])
                P.op("dve", lambda e: e.tensor_copy(out=dbt.ap[:, 260:292], in_=RK[1].ap), reads=[RK[1]], writes=[dbt])
                P.op("dve", lambda e: e.tensor_copy(out=dbt.ap[:, 292:324], in_=OH1[0].ap), reads=[OH1[0]], writes=[dbt])
                P.op("sp", lambda e: e.dma_start(out=dbg_d, in_=dbt.ap), reads=[dbt], dkey="dbg")
                ple_prep(l)
                return
            if _DBG == 1:
                ple_prep(l)
                return
            sc_tiles = []
            for t in range(NT):
                ss = row_rstd(t, junk_moe)
                xs = xs_t[t % 2]
                P.op("dve", lambda e, t=t, ss=ss, xs=xs: e.tensor_scalar(out=xs.ap, in0=xrow(t), scalar1=ss.ap[:, 2:3], scalar2=None, op0=ALU.mult),
                     reads=[X[t][0], X[t][1], ss], writes=[xs])
                for a_ in range(2):
                    dt_ = dram_tiles[("sc", t, a_)]
                    P.op("pool", lambda e, t=t, a_=a_, xs=xs: e.indirect_dma_start(
                        out=sorted_d, out_offset=bass.IndirectOffsetOnAxis(ap=DIS[t].ap[:, a_:a_ + 1], axis=0),
                        in_=xs.ap, in_offset=None, bounds_check=P.regs['b36351'], oob_is_err=False),
                        reads=[xs, DIS[t]], writes=[dt_], dkey=f"sc{t % 2}")
                    sc_tiles.append(dt_)
            ple_prep(l)
            if _DBG == 2:
                return

            gser = dram_tiles[('gser',)]

            def eload(b_, mats):
                s_ = b_ % 2
                srcs = (weg_d[l], weu_d[l], wed_d[l])
                for m_ in mats:
                    src = srcs[m_]
                    for h_, idt in enumerate((idxg, idxd)):
                        P.op("pool", lambda e, m_=m_, src=src, h_=h_, idt=idt: e.indirect_dma_start(
                            out=ew[s_][m_].ap[:, h_ * 2048:(h_ + 1) * 2048], out_offset=None, in_=src[:, :],
                            in_offset=bass.IndirectOffsetOnAxis(ap=idt.ap[:, b_:b_ + 1], axis=0),
                            bounds_check=P.regs['b8191'], oob_is_err=False),
                            reads=[idt], writes=[ew[s_][m_]], dkey=f"ew{s_}_{m_}")

            ys_tiles = []

            def views(s_):
                return (ew[s_][0].ap.rearrange("p (k c) -> p k c", k=8), ew[s_][1].ap.rearrange("p (k c) -> p k c", k=8),
                        ew[s_][2].ap.rearrange("p (k c) -> p k c", k=4))

            def stA_load(b_):
                xg = xg_t[b_ % 4]
                P.op("sp", lambda e: e.dma_start(out=xg.ap, in_=sorted_d[24576 + b_ * 128:24576 + (b_ + 1) * 128, :]), reads=sc_tiles, writes=[xg], dkey=f"xgl{b_ % 4}")

            def stA(b_):
                x2 = b_ % 2
                xg = xg_t[b_ % 4]
                bt = bank()
                btv = bt.ap.bitcast(BF16)
                xgv = xg.ap.rearrange("p (a k) -> p a k", k=8)
                for k in range(8):
                    P.op("pe", lambda e, k=k: e.transpose(out=btv[:, k * 128:(k + 1) * 128], in_=xgv[:, :, k], identity=identb.ap),
                         reads=[xg, identb], writes=[bt])
                xb = xbT[x2]
                P.op("dve", lambda e: e.tensor_tensor(out=xb.ap, in0=btv[:, 0:1024], in1=GT.ap, op=ALU.mult), reads=[bt, GT], writes=[xb])

            def stB(b_, s_):
                x2 = b_ % 2
                vg, vu, vd = views(s_)
                xb = xbT[x2]
                bg = bank()
                mm_acc(bg.ap, bg, [(xb.ap[:, k * 128:(k + 1) * 128], vg[:, k, :]) for k in range(8)], [xb, ew[s_][0]])
                bu = bank()
                mm_acc(bu.ap, bu, [(xb.ap[:, k * 128:(k + 1) * 128], vu[:, k, :]) for k in range(8)], [xb, ew[s_][1]])
                sg = scratch()
                P.op("act", lambda e: e.activation(out=sg.ap, in_=bg.ap, func=AF.Silu), reads=[bg], writes=[sg])
                hb_ = hbt[x2]
                P.op("dve", lambda e: e.tensor_tensor(out=hb_.ap, in0=bu.ap, in1=sg.ap, op=ALU.mult), reads=[bu, sg], writes=[hb_])

            def stT(b_):
                x2 = b_ % 2
                hb_ = hbt[x2]
                bh = bank()
                bhv = bh.ap.bitcast(BF16)
                for f in range(4):
                    P.op("pe", lambda e, f=f: e.transpose(out=bhv[:, f * 128:(f + 1) * 128], in_=hb_.ap[:, f * 128:(f + 1) * 128], identity=identb.ap),
                         reads=[hb_, identb], writes=[bh])
                hT_ = hbT[x2]
                P.op("act", lambda e: e.activation(out=hT_.ap, in_=bhv[:, 0:512], func=AF.Copy), reads=[bh], writes=[hT_])

            def stC(b_, s_):
                x2 = b_ % 2
                vg, vu, vd = views(s_)
                hT_ = hbT[x2]
                yo = yst[b_ % 4]
                for hh in range(2):
                    bd = bank()
                    mm_acc(bd.ap, bd, [(hT_.ap[:, f * 128:(f + 1) * 128], vd[:, f, hh * 512:(hh + 1) * 512]) for f in range(4)], [hT_, ew[s_][2]])
                    if hh == 0:
                        P.op("act", lambda e, bd=bd: e.activation(out=yo.ap[:, 0:512], in_=bd.ap, func=AF.Copy), reads=[bd], writes=[yo])
                    else:
                        P.op("dve", lambda e, bd=bd: e.tensor_copy(out=yo.ap[:, 512:1024], in_=bd.ap), reads=[bd], writes=[yo])
                yt_ = dram_tiles[("ys", b_)]
                P.op("sp", lambda e: e.dma_start(out=ys_d[b_ * 128:(b_ + 1) * 128, :], in_=yo.ap), reads=[yo], writes=[yt_], dkey=f"yst{b_ % 4}")
                ys_tiles.append(yt_)

            def sload(e_, mats):
                s_ = e_ % 2
                srcs = (weg_d[l], weu_d[l], wed_d[l])
                for m_ in mats:
                    src = srcs[m_]
                    P.op("pool", lambda e, m_=m_, src=src: e.dma_start(
                        out=ew[s_][m_].ap.rearrange("p (h c) -> p h c", h=2), in_=src[256 * e_:256 * (e_ + 1), :].rearrange("(p h) c -> p h c", h=2)),
                        writes=[ew[s_][m_]], dkey=f"ew{s_}_{m_}")

            NOVF = 28
            NB_ALL = 64 + NOVF

            def slot_of(b_):
                return (b_ // 2) % 2 if b_ < 64 else (b_ - 64) % 2

            for i in range(-6, NB_ALL):
                for e_ in range(NE):
                    if i == 2 * e_ - 4:
                        sload(e_, (0, 1))
                    if i == 2 * e_ - 2:
                        sload(e_, (2,))
                for o_ in range(NOVF):
                    b_ = 64 + o_
                    if i == b_ - 3:
                        eload(o_, (0, 1))
                    if i == b_ - 1:
                        eload(o_, (2,))
                if 0 <= i + 6 < NB_ALL:
                    stA_load(i + 6)
                if 0 <= i + 3 < NB_ALL:
                    stA(i + 3)
                if 0 <= i + 2 < NB_ALL:
                    stB(i + 2, slot_of(i + 2))
                if 0 <= i + 1 < NB_ALL:
                    stT(i + 1)
                if 0 <= i < NB_ALL:
                    stC(i, slot_of(i))
            if _DBG in (3, 4, 5):
                return
            cbufs = [ycmb[0], ycmb[1], yst[0], yst[1], yst[2], yst[3]]
            for t in range(NT):
                for a_ in range(2):
                    ci = (2 * t + a_) % 6
                    yc = cbufs[ci]
                    P.op("pool", lambda e, t=t, a_=a_, yc=yc: e.indirect_dma_start(
                        out=yc.ap, out_offset=None, in_=ys_d[:, :], in_offset=bass.IndirectOffsetOnAxis(ap=DI[t].ap[:, a_:a_ + 1], axis=0),
                        bounds_check=P.regs['b11775'], oob_is_err=False),
                        reads=ys_tiles + [DI[t]], writes=[yc], dkey=f"yc{ci}")
                    for hh in range(2):
                        xt = X[t][hh]
                        P.op("dve", lambda e, t=t, a_=a_, yc=yc, hh=hh, xt=xt: e.scalar_tensor_tensor(
                            out=xt.ap, in0=yc.ap[:, hh * 512:(hh + 1) * 512], scalar=G12[t].ap[:, a_:a_ + 1], in1=xt.ap, op0=ALU.mult, op1=ALU.add),
                            reads=[yc, G12[t], xt], writes=[xt])

        def ple_prep(l):
            P.op("pool", lambda e: e.dma_start(out=wpe.ap.rearrange("p (k c) -> p k c", k=2), in_=wpe_d[l].rearrange("(k p) c -> p k c", p=128)),
                 writes=[wpe], dkey="wpe")
            for t in range(NT):
                stt = pst[t % 2]
                P.op("sp", lambda e, t=t, stt=stt: e.dma_start(out=stt.ap, in_=p_d[l, t * 128:(t + 1) * 128, :]), writes=[stt], dkey=f"pst{t % 2}")
                b = bank()
                for kc in range(2):
                    P.op("pe", lambda e, b=b, kc=kc, stt=stt: e.transpose(out=b.ap[:, kc * 128:(kc + 1) * 128], in_=stt.ap[:, kc * 128:(kc + 1) * 128], identity=ident.ap),
                         reads=[stt, ident], writes=[b])
                for kc in range(2):
                    P.op("act", lambda e, b=b, kc=kc, t=t: e.activation(out=pT[kc].ap[:, t * 128:(t + 1) * 128], in_=b.ap[:, kc * 128:(kc + 1) * 128], func=AF.Copy),
                         reads=[b], writes=[pT[kc]])

        def ple_phase(l):
            pb = l * PPL
            wpgl = wpg_d[l].rearrange("(k p) c -> p k c", p=128)
            for i in range(2):
                P.op("pool", lambda e, i=i: e.dma_start(out=wpg[i].ap.rearrange("p (k c) -> p k c", k=8), in_=wpgl[:, :, i * 512:(i + 1) * 512]),
                     writes=[wpg[i]], dkey="wp")
            wpev = wpe.ap.rearrange("p (k c) -> p k c", k=2)

            def ple_main(g_):
                for t in range(4 * g_, 4 * g_ + 4):
                    ple_tile(t)

            def ple_tile(t):
                g, i = t // 4, t % 4
                for hh in range(2):
                    wv = wpg[hh].ap.rearrange("p (k c) -> p k c", k=8)
                    bg = bank()
                    mm_acc(bg.ap, bg, [(h2T[k][g].ap[:, i * 128:(i + 1) * 128], wv[:, k, :]) for k in range(8)], [wpg[hh]] + [h2T[k][g] for k in range(8)])
                    be = bank()
                    mm_acc(be.ap, be, [(pT[kc].ap[:, t * 128:(t + 1) * 128], wpev[:, kc, hh * 512:(hh + 1) * 512]) for kc in range(2)], [wpe] + pT)
                    sg = scratch()
                    P.op("act", lambda e, sg=sg, bg=bg: e.activation(out=sg.ap, in_=bg.ap, func=AF.Sigmoid), reads=[bg], writes=[sg])
                    P.op("dve", lambda e, sg=sg, be=be: e.tensor_tensor(out=sg.ap, in0=be.ap, in1=sg.ap, op=ALU.mult), reads=[be, sg], writes=[sg])
                    xt = X[t][hh]
                    P.op("dve", lambda e, sg=sg, xt=xt: e.tensor_tensor(out=xt.ap, in0=xt.ap, in1=sg.ap, op=ALU.add), reads=[sg, xt], writes=[xt])

            norm_T(0, pb + 16, [h2T[c][0] for c in range(8)], xnb2)
            for g in range(4):
                if g + 1 < 4:
                    norm_T(g + 1, pb + 16, [h2T[c][g + 1] for c in range(8)], xnb2)
                ple_main(g)

        def final_phase():
            P.op("sp", lambda e: e.dma_start(out=nfin.ap, in_=bc_d[:, 72:72 + D]), writes=[nfin], dkey="nf")
            for t in range(NT):
                ss = row_rstd(t, junk_moe)
                o = ost[t % 2]
                P.op("dve", lambda e, t=t, ss=ss, o=o: e.scalar_tensor_tensor(out=o.ap, in0=xrow(t), scalar=ss.ap[:, 2:3], in1=nfin.ap, op0=ALU.mult, op1=ALU.mult),
                     reads=[X[t][0], X[t][1], ss, nfin], writes=[o])
                P.op("sp", lambda e, t=t, o=o: e.dma_start(out=out_d[t * 128:(t + 1) * 128, :], in_=o.ap), reads=[o], dkey=f"ost{t % 2}")
            for i in range(2):
                P.op("sp", lambda e: e.nop(), writes=[ost[i]])

        for l in range(nl):
            for g in range(4):
                tasks = mixer_group(l, g)
                loaded = {}
                n = len(tasks)
                nxt_load = 0
                in_use = 0
                for ti in range(n):
                    while nxt_load < n and (nxt_load <= ti or in_use + len(tasks[nxt_load][0]) <= 6):
                        loaded[nxt_load] = [wload(srcs) for srcs in tasks[nxt_load][0]]
                        in_use += len(tasks[nxt_load][0])
                        nxt_load += 1
                    tasks[ti][1](loaded.pop(ti))
                    in_use -= len(tasks[ti][0])
            nbank[0] = 8
            moe_phase(l)
            ple_phase(l)
            nbank[0] = 6
        final_phase()
        P.emit()
        nops = {k: len(v) for k, v in P.ops.items()}
        print("ops per engine:", nops)
    return nc


def _host_layout(inp):
    f = np.float32
    w_in = np.asarray(inp["w_in"], f)
    swap = np.arange(512).reshape(8, 64)
    swap = np.concatenate([swap[:, 32:], swap[:, :32]], axis=1).reshape(-1)
    q, k, v = w_in[:, :, 0:512], w_in[:, :, 512:1024], w_in[:, :, 1024:1536]
    rest = w_in[:, :, 1536:]
    win = np.ascontiguousarray(np.concatenate([q, q[:, :, swap], k, k[:, :, swap], v, rest], axis=2))
    assert win.shape[2] == WIN_EXT
    wr = np.ascontiguousarray(np.concatenate([np.asarray(inp["w_route_group"], f), np.asarray(inp["w_route_expert"], f)], axis=2))
    pp = np.zeros((128, L * PPL), f)

    def cols(vec, n):
        return np.asarray(vec, f).reshape(n, 128).T
    for l in range(L):
        b = l * PPL
        pp[:, b + 0:b + 8] = cols(inp["norm_mix"][l], 8)
        pp[:, b + 8:b + 16] = cols(inp["norm_ffn"][l], 8)
        pp[:, b + 168:b + 176] = np.asarray(inp["norm_ffn"][l], f).reshape(128, 8)
        pp[:, b + 16:b + 24] = cols(inp["norm_ple"][l], 8)
        pp[:, b + 24:b + 32] = cols(inp["b_conv_out"][l], 8)
        pp[:, b + 32:b + 36] = cols(inp["b_dw"][l], 4)
        pp[:, b + 36:b + 40] = cols(inp["ln_conv_g"][l], 4)
        pp[:, b + 40:b + 44] = cols(inp["ln_conv_b"][l], 4)
        wd = np.asarray(inp["w_dw"][l], f)
        for j in range(31):
            pp[:, b + 44 + 4 * j:b + 48 + 4 * j] = cols(wd[j], 4)
    bc = np.zeros((128, 72 + D), f)
    for l in range(L):
        bc[:, l * 36:l * 36 + 4] = np.asarray(inp["b_route_group"][l], f)[None, :]
        bc[:, l * 36 + 4:l * 36 + 36] = np.asarray(inp["b_route_expert"][l], f)[None, :]
    bc[:, 72:] = np.asarray(inp["norm_final"], f)[None, :]
    inv = np.power(f(10000.0), -np.arange(0, 64, 2, dtype=f) / f(64)).astype(f)
    ang = (np.arange(S, dtype=f)[:, None] * inv[None, :]).astype(f)
    cs, sn = np.cos(ang).astype(f).T, np.sin(ang).astype(f).T
    cosT = np.concatenate([cs, cs, cs, cs], axis=0)
    sinT = np.concatenate([-sn, sn, -sn, sn], axis=0)
    kk = np.arange(128)[:, None]
    col = np.arange(S)[None, :]
    dl = col - kk
    cnt = ((dl >= 0) & (dl <= 128)).astype(f) + ((dl >= 0) & (dl <= 512) & (dl % 4 == 0)).astype(f) \
        + ((dl >= 0) & (dl <= 2048) & (dl % 16 == 0)).astype(f)
    shared = {
        "win": win, "wao": np.ascontiguousarray(inp["w_attn_out"], f), "wco": np.ascontiguousarray(inp["w_conv_out"], f),
        "wout": np.ascontiguousarray(inp["w_out"], f), "wr": wr,
        "wpg": np.ascontiguousarray(inp["w_ple_gate"], f), "wpe": np.ascontiguousarray(inp["w_ple_proj"], f),
        "pp": pp, "bc": bc, "cosT": np.ascontiguousarray(cosT), "sinT": np.ascontiguousarray(sinT),
        "maskT": np.ascontiguousarray(cnt), "ident": np.eye(128, dtype=f),
    }
    for l in range(L):
        shared[f"weg{l}"] = np.ascontiguousarray(inp["w_exp_gate"][l], f).reshape(8192, 2048)
        shared[f"weu{l}"] = np.ascontiguousarray(inp["w_exp_up"][l], f).reshape(8192, 2048)
        shared[f"wed{l}"] = np.ascontiguousarray(
            np.asarray(inp["w_exp_down"][l], f).reshape(NE, 4, 128, D).transpose(0, 2, 1, 3)).reshape(8192, 2048)
    cst = np.zeros((128, 384), f)
    cst[:, 320:352] = (256 * np.arange(32, dtype=f))[None, :]
    cst[:, 352:384] = (7936 - 256 * np.arange(32, dtype=f))[None, :]
    cst[:, 0:64] = np.arange(64, dtype=f)[None, :]
    cst[:, 64:128] = (2 * np.arange(128, dtype=f))[:, None]
    cst[:, 128:192] = (2 * np.arange(128, dtype=f) + 1)[:, None]
    cst[:, 192:320] = (np.arange(128)[:, None] < np.arange(128)[None, :]).astype(f)
    shared["cst"] = cst
    return shared


_NL = L
_DBG = 0


def kernel(**inputs):
    shared = _host_layout(inputs)
    x = np.asarray(inputs["x"], np.float32)
    p = np.asarray(inputs["p"], np.float32)
    nc = build_program(_NL)
    in_maps = []
    for b in range(8):
        m = dict(shared)
        m["x"] = np.ascontiguousarray(x[b])
        m["p"] = np.ascontiguousarray(p[:, b])
        in_maps.append(m)
    res = run_bass_kernel_spmd(nc, in_maps, core_ids=list(range(8)))
    return np.stack([r["out"] for r in res.results], axis=0).astype(np.float32)
```
